# Optimizing a Trainium2 kernel written in Bass

```python
import jax, jax.numpy as jnp
from jax import lax
import numpy as np

D_MODEL = 1024
BATCH = 8
SEQ = 4096
DEPTH = 1

GRID_W = 64
CTX_LEN = 256
HEAD_DIM = 64
N_Q_HEADS = 8
N_KV_HEADS = 2
Q_PER_KV = N_Q_HEADS // N_KV_HEADS
ATTN_DIM = N_Q_HEADS * HEAD_DIM
KV_DIM = N_KV_HEADS * HEAD_DIM
WINDOW = 128
ATTN_BLOCK = 128
ATTN_SCALE = HEAD_DIM ** -0.5
ROPE_THETA = 10000.0
POOL_WINDOWS = (2, 4, 8, 16)
N_POOL_GROUPS = len(POOL_WINDOWS)
POOL_DIM = D_MODEL // 2
POOL_GROUP_DIM = POOL_DIM // N_POOL_GROUPS
Q_OFF = 0
K_OFF = Q_OFF + ATTN_DIM
V_OFF = K_OFF + KV_DIM
P_OFF = V_OFF + KV_DIM
GA_OFF = P_OFF + POOL_DIM
GP_OFF = GA_OFF + D_MODEL
IN_DIM = GP_OFF + D_MODEL
N_EXPERTS = 256
TOP_K = 8
N_EXPERT_GROUPS = 8
TOPK_GROUPS = 4
EXPERT_DIM = 256
SHARED_DIM = 256
ROUTED_SCALE = 2.5
EXPERT_BLOCK = 128
N_ADA = 6
NORM_EPS = 1e-6
NEG_INF = -1e30

kernel_name = "hybrid_gated_window_gqa_pool_moe_dit_block"


def _rmsnorm(x, g):
    x32 = x.astype(jnp.float32)
    y = x32 * lax.rsqrt(jnp.mean(x32 * x32, axis=-1, keepdims=True) + NORM_EPS)
    return y.astype(x.dtype) * g


def _axial_rope(seq_len):
    rows = seq_len // GRID_W
    row = jnp.repeat(jnp.arange(rows), GRID_W).astype(jnp.float32)
    col = jnp.tile(jnp.arange(GRID_W), rows).astype(jnp.float32)
    n_freq = HEAD_DIM // 4
    inv = ROPE_THETA ** (-jnp.arange(n_freq, dtype=jnp.float32) / n_freq)
    ang = jnp.stack([row[:, None] * inv, col[:, None] * inv], axis=1)
    return jnp.cos(ang), jnp.sin(ang)


def _apply_rope(x, cos, sin):
    shp = x.shape
    nf = HEAD_DIM // 4
    xr = x.reshape(shp[:-1] + (2, 2, nf))
    bshape = (shp[1],) + (1,) * (x.ndim - 3) + (2, nf)
    c = cos.reshape(bshape).astype(x.dtype)
    s = sin.reshape(bshape).astype(x.dtype)
    a, b = xr[..., 0, :], xr[..., 1, :]
    return jnp.stack([a * c - b * s, b * c + a * s], axis=-2).reshape(shp)


def _window_attention(q, k, v, kc, vc, sink):
    B, S = q.shape[:2]
    C = kc.shape[1]
    nb = S // ATTN_BLOCK
    n_win = 3 * ATTN_BLOCK
    qb = q.reshape(B, nb, ATTN_BLOCK, N_KV_HEADS, Q_PER_KV, HEAD_DIM)

    def band(t):
        tp = jnp.pad(t, ((0, 0), (ATTN_BLOCK, ATTN_BLOCK), (0, 0), (0, 0)))
        tp = tp.reshape(B, nb + 2, ATTN_BLOCK, N_KV_HEADS, HEAD_DIM)
        return jnp.concatenate([tp[:, :-2], tp[:, 1:-1], tp[:, 2:]], axis=2)

    kw, vw = band(k), band(v)
    s_loc = jnp.einsum('bnqkgd,bnjkd->bnkgqj', qb, kw, preferred_element_type=jnp.float32) * ATTN_SCALE
    blk = jnp.arange(nb)[:, None, None]
    qpos = blk * ATTN_BLOCK + jnp.arange(ATTN_BLOCK)[None, :, None]
    kpos = (blk - 1) * ATTN_BLOCK + jnp.arange(n_win)[None, None, :]
    valid = (jnp.abs(qpos - kpos) <= WINDOW) & (kpos >= 0) & (kpos < S)
    s_loc = jnp.where(valid[None, :, None, None], s_loc, NEG_INF)
    s_ctx = jnp.einsum('bnqkgd,bckd->bnkgqc', qb, kc, preferred_element_type=jnp.float32) * ATTN_SCALE
    s_sink = jnp.broadcast_to(sink.astype(jnp.float32)[None, None, :, :, None, None], s_ctx.shape[:-1] + (1,))
    p = jax.nn.softmax(jnp.concatenate([s_loc, s_ctx, s_sink], axis=-1), axis=-1).astype(q.dtype)
    o = (jnp.einsum('bnkgqj,bnjkd->bnqkgd', p[..., :n_win], vw)
         + jnp.einsum('bnkgqc,bckd->bnqkgd', p[..., n_win:n_win + C], vc))
    return o.reshape(B, S, ATTN_DIM)


def _context_attention(qc, kc, vc, sink):
    B, C = qc.shape[:2]
    s = jnp.einsum('bckgd,bjkd->bkgcj', qc, kc, preferred_element_type=jnp.float32) * ATTN_SCALE
    s_sink = jnp.broadcast_to(sink.astype(jnp.float32)[None, :, :, None, None], s.shape[:-1] + (1,))
    p = jax.nn.softmax(jnp.concatenate([s, s_sink], axis=-1), axis=-1)[..., :-1].astype(vc.dtype)
    o = jnp.einsum('bkgcj,bjkd->bckgd', p, vc)
    return o.reshape(B, C, ATTN_DIM)


def _pool_mixer(u, w_pool, pool_scale):
    B, L, _ = u.shape
    u32 = u.astype(jnp.float32)
    cs = jnp.concatenate([jnp.zeros((B, 1, POOL_DIM), jnp.float32), jnp.cumsum(u32, axis=1)], axis=1)
    t = jnp.arange(L)
    groups = []
    for g, w in enumerate(POOL_WINDOWS):
        lo = jnp.clip(t - w // 2, 0, L)
        hi = jnp.clip(t + w // 2, 0, L)
        sl = slice(g * POOL_GROUP_DIM, (g + 1) * POOL_GROUP_DIM)
        csg = cs[:, :, sl]
        mean = (jnp.take(csg, hi, axis=1) - jnp.take(csg, lo, axis=1)) / (hi - lo).astype(jnp.float32)[None, :, None]
        groups.append(mean - u32[:, :, sl])
    d = jnp.stack(groups, axis=2).astype(u.dtype)
    y = jnp.einsum('blgc,gcd->blgd', d, w_pool).reshape(B, L, POOL_DIM)
    return y * pool_scale


def _merge_branches(attn, pool, gate_cols, w_up_attn, w_up_pool, w_out):
    ga, gp = gate_cols[..., :D_MODEL], gate_cols[..., D_MODEL:]
    y = jax.nn.sigmoid(ga) * (attn @ w_up_attn) + jax.nn.sigmoid(gp) * (pool @ w_up_pool)
    return y @ w_out


def _swiglu(x, wg, wu, wd):
    return (jax.nn.silu(x @ wg) * (x @ wu)) @ wd


def _route(h, w_router, router_bias):
    T = h.shape[0]
    s = jax.nn.sigmoid(jnp.einsum('td,de->te', h, w_router, preferred_element_type=jnp.float32))
    sel = s + router_bias.astype(jnp.float32)
    sel_g = sel.reshape(T, N_EXPERT_GROUPS, N_EXPERTS // N_EXPERT_GROUPS)
    group_score = lax.top_k(sel_g, 2)[0].sum(axis=-1)
    _, gidx = lax.top_k(group_score, TOPK_GROUPS)
    gmask = jax.nn.one_hot(gidx, N_EXPERT_GROUPS, dtype=jnp.float32).sum(axis=-2) > 0
    emask = jnp.repeat(gmask, N_EXPERTS // N_EXPERT_GROUPS, axis=-1)
    _, eidx = lax.top_k(jnp.where(emask, sel, NEG_INF), TOP_K)
    w = jnp.take_along_axis(s, eidx, axis=-1)
    w = w / jnp.sum(w, axis=-1, keepdims=True) * ROUTED_SCALE
    return eidx, w


def _moe(h, w_router, router_bias, w_exp_gate, w_exp_up, w_exp_down, w_sh_gate, w_sh_up, w_sh_down):
    T, D = h.shape
    eidx, w = _route(h, w_router, router_bias)
    A = T * TOP_K
    flat_e = eidx.reshape(A)
    flat_tok = jnp.repeat(jnp.arange(T, dtype=jnp.int32), TOP_K)
    flat_w = w.reshape(A)
    order = jnp.argsort(flat_e)
    sorted_e = flat_e[order]
    counts = jnp.bincount(flat_e, length=N_EXPERTS)
    starts = jnp.cumsum(counts) - counts
    padded = (counts + EXPERT_BLOCK - 1) // EXPERT_BLOCK * EXPERT_BLOCK
    pends = jnp.cumsum(padded)
    pstarts = pends - padded
    dest = pstarts[sorted_e] + jnp.arange(A) - starts[sorted_e]
    n_blocks = (A + N_EXPERTS * (EXPERT_BLOCK - 1) + EXPERT_BLOCK - 1) // EXPERT_BLOCK
    n_slots = n_blocks * EXPERT_BLOCK
    slot_tok = jnp.full((n_slots,), T, dtype=jnp.int32).at[dest].set(flat_tok[order])
    slot_w = jnp.zeros((n_slots,), h.dtype).at[dest].set(flat_w[order].astype(h.dtype))
    block_e = jnp.minimum(jnp.searchsorted(pends, jnp.arange(n_blocks) * EXPERT_BLOCK, side='right'), N_EXPERTS - 1)
    h_pad = jnp.concatenate([h, jnp.zeros((1, D), h.dtype)], axis=0)

    def run_block(args):
        tok, wt, e = args
        return _swiglu(h_pad[tok], w_exp_gate[e], w_exp_up[e], w_exp_down[e]) * wt[:, None]

    y = lax.map(run_block, (slot_tok.reshape(n_blocks, EXPERT_BLOCK),
                            slot_w.reshape(n_blocks, EXPERT_BLOCK), block_e))
    routed = jax.ops.segment_sum(y.reshape(n_slots, D), slot_tok, num_segments=T + 1)[:T]
    return routed + _swiglu(h, w_sh_gate, w_sh_up, w_sh_down)


def setup_inputs(seed: int = 0) -> dict:
    key = jax.random.key(seed)
    ks = jax.random.split(key, 24)
    D, E, F = D_MODEL, N_EXPERTS, EXPERT_DIM
    nrm = jax.random.normal
    return {
        "x": nrm(ks[0], (BATCH, SEQ, D), jnp.float32),
        "c": nrm(ks[1], (BATCH, D), jnp.float32),
        "ctx": nrm(ks[2], (BATCH, CTX_LEN, D), jnp.float32),
        "c_ctx": nrm(ks[3], (D,), jnp.float32),
        "w_ada": nrm(ks[4], (DEPTH, D, N_ADA * D), jnp.float32) * (0.5 * D ** -0.5),
        "b_ada": nrm(ks[5], (DEPTH, N_ADA * D), jnp.float32) * 0.02,
        "norm1_g": 1.0 + 0.02 * nrm(ks[6], (DEPTH, D), jnp.float32),
        "w_in": nrm(ks[7], (DEPTH, D, IN_DIM), jnp.float32) * D ** -0.5,
        "attn_sink": nrm(ks[8], (DEPTH, N_Q_HEADS), jnp.float32) * 0.5,
        "w_pool": nrm(ks[9], (DEPTH, N_POOL_GROUPS, POOL_GROUP_DIM, POOL_GROUP_DIM), jnp.float32) * POOL_GROUP_DIM ** -0.5,
        "pool_scale": 1.0 + 0.02 * nrm(ks[10], (DEPTH, POOL_DIM), jnp.float32),
        "w_up_attn": nrm(ks[11], (DEPTH, ATTN_DIM, D), jnp.float32) * ATTN_DIM ** -0.5,
        "w_up_pool": nrm(ks[12], (DEPTH, POOL_DIM, D), jnp.float32) * POOL_DIM ** -0.5,
        "w_out": nrm(ks[13], (DEPTH, D, D), jnp.float32) * D ** -0.5,
        "norm2_g": 1.0 + 0.02 * nrm(ks[14], (DEPTH, D), jnp.float32),
        "w_router": nrm(ks[15], (DEPTH, D, E), jnp.float32) * D ** -0.5,
        "router_bias": nrm(ks[16], (DEPTH, E), jnp.float32) * 0.01,
        "w_exp_gate": nrm(ks[17], (DEPTH, E, D, F), jnp.float32) * D ** -0.5,
        "w_exp_up": nrm(ks[18], (DEPTH, E, D, F), jnp.float32) * D ** -0.5,
        "w_exp_down": nrm(ks[19], (DEPTH, E, F, D), jnp.float32) * F ** -0.5,
        "w_sh_gate": nrm(ks[20], (DEPTH, D, SHARED_DIM), jnp.float32) * D ** -0.5,
        "w_sh_up": nrm(ks[21], (DEPTH, D, SHARED_DIM), jnp.float32) * D ** -0.5,
        "w_sh_down": nrm(ks[22], (DEPTH, SHARED_DIM, D), jnp.float32) * SHARED_DIM ** -0.5,
        "final_g": 1.0 + 0.02 * nrm(ks[23], (D,), jnp.float32),
    }


def reference(x, c, ctx, c_ctx, w_ada, b_ada, norm1_g, w_in, attn_sink, w_pool, pool_scale,
              w_up_attn, w_up_pool, w_out, norm2_g, w_router, router_bias,
              w_exp_gate, w_exp_up, w_exp_down, w_sh_gate, w_sh_up, w_sh_down, final_g):
    B, S, D = x.shape
    C = ctx.shape[1]
    cos, sin = _axial_rope(S)
    for l in range(DEPTH):
        last = l == DEPTH - 1
        mod = (jax.nn.silu(c) @ w_ada[l] + b_ada[l])[:, None, :]
        mod_c = jax.nn.silu(c_ctx) @ w_ada[l] + b_ada[l]
        sh1, sc1, g1, sh2, sc2, g2 = jnp.split(mod, N_ADA, axis=-1)
        sh1c, sc1c, g1c, sh2c, sc2c, g2c = jnp.split(mod_c, N_ADA, axis=-1)

        h = _rmsnorm(x, norm1_g[l]) * (1 + sc1) + sh1
        hc = _rmsnorm(ctx, norm1_g[l]) * (1 + sc1c) + sh1c
        proj = h @ w_in[l]
        q = _apply_rope(proj[..., Q_OFF:K_OFF].reshape(B, S, N_KV_HEADS, Q_PER_KV, HEAD_DIM), cos, sin)
        k = _apply_rope(proj[..., K_OFF:V_OFF].reshape(B, S, N_KV_HEADS, HEAD_DIM), cos, sin)
        v = proj[..., V_OFF:P_OFF].reshape(B, S, N_KV_HEADS, HEAD_DIM)
        kvc = hc @ w_in[l][:, K_OFF:P_OFF]
        kc = kvc[..., :KV_DIM].reshape(B, C, N_KV_HEADS, HEAD_DIM)
        vc = kvc[..., KV_DIM:].reshape(B, C, N_KV_HEADS, HEAD_DIM)
        sink = attn_sink[l].reshape(N_KV_HEADS, Q_PER_KV)
        attn = _window_attention(q, k, v, kc, vc, sink)
        pool = _pool_mixer(proj[..., P_OFF:GA_OFF], w_pool[l], pool_scale[l])
        mix = _merge_branches(attn, pool, proj[..., GA_OFF:IN_DIM], w_up_attn[l], w_up_pool[l], w_out[l])
        x = x + g1 * mix

        h2 = _rmsnorm(x, norm2_g[l]) * (1 + sc2) + sh2
        moe_args = (w_router[l], router_bias[l], w_exp_gate[l], w_exp_up[l], w_exp_down[l],
                    w_sh_gate[l], w_sh_up[l], w_sh_down[l])
        if last:
            f = _moe(h2.reshape(B * S, D), *moe_args).reshape(B, S, D)
        else:
            qc = (hc @ w_in[l][:, Q_OFF:K_OFF]).reshape(B, C, N_KV_HEADS, Q_PER_KV, HEAD_DIM)
            restc = hc @ w_in[l][:, P_OFF:IN_DIM]
            attn_c = _context_attention(qc, kc, vc, sink)
            pool_c = _pool_mixer(restc[..., :POOL_DIM], w_pool[l], pool_scale[l])
            mix_c = _merge_branches(attn_c, pool_c, restc[..., POOL_DIM:], w_up_attn[l], w_up_pool[l], w_out[l])
            ctx = ctx + g1c * mix_c
            h2c = _rmsnorm(ctx, norm2_g[l]) * (1 + sc2c) + sh2c
            fa = _moe(jnp.concatenate([h2.reshape(B * S, D), h2c.reshape(B * C, D)], axis=0), *moe_args)
            f = fa[:B * S].reshape(B, S, D)
            ctx = ctx + g2c * fa[B * S:].reshape(B, C, D)
        x = x + g2 * f
    return _rmsnorm(x, final_g)
```

```python
import numpy as np
import concourse.bass as bass
import concourse.mybir as mybir
from concourse.bass_utils import run_bass_kernel_spmd

ENGS = ("sp", "act", "dve", "pool", "pe")
SB_LO = 16512
SB_HI = 229376


class Ev:
    __slots__ = ("op", "shared")

    def __init__(self, op, shared=False):
        self.op = op
        self.shared = shared


class Buf:
    def __init__(self, S, name, t=None, is_dram=False):
        self.S = S
        self.name = name
        self.t = t
        self.is_dram = is_dram
        self.writers = {}
        self.readers = {}
        self.shared_keys = set()
        self.wsem = None
        self.rsem = None
        if S.epoch_op is not None:
            self.writers[S.epoch_op.semkey()] = S.epoch_op
        S.bufs.append(self)

    def __getitem__(self, k):
        return self.t[k]


class Op:
    __slots__ = ("eng", "fn", "deps", "is_dma", "sem", "val", "marked", "idx", "raw_same")

    def __init__(self, eng, fn, is_dma):
        self.eng = eng
        self.fn = fn
        self.deps = []
        self.is_dma = is_dma
        self.sem = None
        self.val = None
        self.marked = False

    def semkey(self):
        return ("dma", id(self.sem)) if self.is_dma else ("eng", self.eng)


class DmaSem:
    def __init__(self, S, name):
        self.h = S.nc.alloc_semaphore(name)
        self.count = 0


class Sched:
    def __init__(self, nc):
        self.nc = nc
        self.ops = {e: [] for e in ENGS}
        self.bufs = []
        self.epoch_op = None
        self.sb_ptr = SB_LO
        self.nsem = 0
        self.eng_sem = {}
        self.uid = 0
        self.psum_used = 0

    def sb(self, name, shape, dtype, buf=True):
        nbytes = 1
        for s in shape[1:]:
            nbytes *= s
        nbytes *= mybir.dt.size(dtype)
        nbytes = (nbytes + 31) // 32 * 32
        off = self.sb_ptr
        assert off + nbytes <= SB_HI, f"SBUF overflow allocating {name}: {off}+{nbytes}"
        self.sb_ptr += nbytes
        self.uid += 1
        t = self.nc.alloc_sbuf_tensor_at(f"{name}_{self.uid}", list(shape), dtype, offset=off)
        return Buf(self, name, t) if buf else t

    def ps(self, name, shape, dtype):
        self.uid += 1
        t = self.nc.alloc_psum_tensor(f"{name}_{self.uid}", list(shape), dtype)
        return Buf(self, name, t)

    def dram(self, name):
        return Buf(self, name, None, True)

    def reg(self, e, val):
        if not hasattr(self, "_regs"):
            self._regs = {}
        if val not in self._regs:
            self._regs[val] = e.to_reg(val)
        return self._regs[val]

    def newsem(self, name):
        self.nsem += 1
        return DmaSem(self, f"{name}_{self.nsem}")

    def _add(self, eng, fn, reads, writes, wshared, is_dma, sem):
        op = Op(eng, fn, is_dma)
        op.sem = sem
        deps = {}

        def add(d, kind):
            if d is op:
                return
            if (not d.is_dma) and (not is_dma) and d.eng == eng and kind != "raw":
                return
            if (not d.is_dma) and (not is_dma) and d.eng == eng == "pe":
                return
            k = d.semkey()
            cur = deps.get(k)
            if cur is None or d.idx > cur.idx:
                deps[k] = d

        for b in reads:
            for d in b.writers.values():
                add(d, "raw")
        for b in writes:
            for d in b.writers.values():
                add(d, "waw")
            for d in b.readers.values():
                add(d, "war")
        for b in wshared:
            for kk, d in b.writers.items():
                if kk not in b.shared_keys:
                    add(d, "waw")
            for d in b.readers.values():
                add(d, "war")
        op.deps = list(deps.values())
        op.idx = len(self.ops[eng])
        if is_dma:
            sem.count += 16
            op.val = sem.count
        if is_dma:
            op.idx = op.val
        self.ops[eng].append(op)
        k = op.semkey()
        for b in reads:
            b.readers[k] = op
        for b in writes:
            b.writers = {k: op}
            b.readers = {}
            b.shared_keys = set()
        for b in wshared:
            b.writers[k] = op
            b.shared_keys.add(k)
        return op

    def op(self, eng, fn, reads=(), writes=(), wshared=()):
        return self._add(eng, fn, list(reads), list(writes), list(wshared), False, None)

    def dma(self, eng, out_ap, in_ap, reads=(), writes=(), wshared=(), sem=None, cast=False, **kw):
        reads, writes, wshared = list(reads), list(writes), list(wshared)
        if sem is None:
            sem = self._pick_sem(reads, writes, wshared)

        def fn(e, out_ap=out_ap, in_ap=in_ap, kw=kw):
            return e.dma_start(out=out_ap, in_=in_ap, **kw)

        return self._add(eng, fn, reads, writes, wshared, True, sem)

    def _pick_sem(self, reads, writes, wshared):
        for b in writes + wshared:
            if not b.is_dram:
                if b.wsem is None:
                    b.wsem = self.newsem("w" + b.name)
                return b.wsem
        for b in reads:
            if not b.is_dram:
                if b.rsem is None:
                    b.rsem = self.newsem("r" + b.name)
                return b.rsem
        raise ValueError("dma needs explicit sem")

    def dma_fn(self, eng, fn, reads=(), writes=(), wshared=(), sem=None):
        reads, writes, wshared = list(reads), list(writes), list(wshared)
        if sem is None:
            sem = self._pick_sem(reads, writes, wshared)
        return self._add(eng, fn, reads, writes, wshared, True, sem)

    def barrier(self, eng="pool", fn=None):
        assert fn is not None
        op = self._add(eng, fn, [], list(self.bufs), [], False, None)
        self.epoch_op = op
        return op

    def finalize_and_emit(self):
        nc = self.nc
        for e in ENGS:
            for op in self.ops[e]:
                for d in op.deps:
                    d.marked = True
        for e in ENGS:
            c = 0
            for op in self.ops[e]:
                if not op.is_dma and op.marked:
                    c += 1
                    op.val = c
        for e in ENGS:
            self.eng_sem[e] = nc.alloc_semaphore(f"eng_{e}")
        engobj = {"sp": None, "act": None, "dve": None, "pool": None, "pe": None}
        S = self
        nwaits = {e: 0 for e in ENGS}

        def run(ename, eng):
            seen = {}
            mysems = {}
            for op in S.ops[ename]:
                for d in op.deps:
                    if d.is_dma:
                        key, h, v = ("d", id(d.sem)), d.sem.h, d.val
                    else:
                        key, h, v = ("e", d.eng), S.eng_sem[d.eng], d.val
                    if seen.get(key, 0) >= v:
                        continue
                    seen[key] = v
                    eng.wait_ge(h, v)
                    nwaits[ename] += 1
                ins = op.fn(eng)
                if op.is_dma:
                    ins.then_inc(op.sem.h, 16)
                    mysems[id(op.sem)] = op.sem
                elif op.marked:
                    ins.then_inc(S.eng_sem[ename], 1)
            for sm in mysems.values():
                if seen.get(("d", id(sm)), 0) < sm.count:
                    eng.wait_ge(sm.h, sm.count)

        with nc.Block() as block:
            @block.sync
            def _(e):
                run("sp", e)

            @block.scalar
            def _(e):
                run("act", e)

            @block.vector
            def _(e):
                run("dve", e)

            @block.gpsimd
            def _(e):
                run("pool", e)

            @block.tensor
            def _(e):
                run("pe", e)
        self.nwaits = nwaits

F32 = mybir.dt.float32
BF16 = mybir.dt.bfloat16
I32 = mybir.dt.int32
U32 = mybir.dt.uint32
AF = mybir.ActivationFunctionType
ALU = mybir.AluOpType
AX = mybir.AxisListType

T = 4096
D = 1024
NT = T // 128
CH = 256
NCH = T // CH
NXC = 3968
E = 256
NBLK = 511
NSLOT = NBLK * 128
EPS = 1e-6

QB, QR, KB, KR, UB, GA, GP, VB = 0, 4, 8, 9, 10, 14, 22, 30


def build(nc, stage="full"):
    S = Sched(nc)
    dt_in = lambda name, shape, dt=F32: nc.dram_tensor(name, list(shape), dt, kind="ExternalInput").ap()
    x_d = dt_in("x", [T, D]); ctx_d = dt_in("ctx", [256, D]); csin_d = dt_in("csin", [128, 16])
    wada_d = dt_in("wada", [D, 6144]); bada_d = dt_in("bada", [1, 6144]); badafm_d = dt_in("badafm", [128, 48])
    n1fm_d = dt_in("n1fm", [128, 8]); n2row_d = dt_in("n2row", [1, D]); fgrow_d = dt_in("fgrow", [1, D])
    wx_d = dt_in("wx", [D, NXC]); ropec_d = dt_in("ropec", [128, T]); ropes_d = dt_in("ropes", [128, T])
    sink_d = dt_in("sink", [1, 8]); wpool_d = dt_in("wpool", [128, 512]); pscale_d = dt_in("pscale", [128, 4])
    wua_d = dt_in("wua", [128, 4096]); wup_d = dt_in("wup", [128, 4096]); wout_d = dt_in("wout", [128, 8192])
    ident_d = dt_in("ident", [128, 128]); masks_d = dt_in("masks", [128, 1024]); ustrict_d = dt_in("ustrict", [128, 128])
    invc_d = dt_in("invc", [128, 64]); iotae_d = dt_in("iotae", [128, 256]); iotap_d = dt_in("iotap", [128, 1])
    iotab_d = dt_in("iotab", [128, 512])
    full = stage == "full"
    if full:
        wr_d = dt_in("wr", [128, 2048]); rbias_d = dt_in("rbias", [1, 256])
        wsg_d = dt_in("wsg", [128, 2048]); wsu_d = dt_in("wsu", [128, 2048]); wsd_d = dt_in("wsd", [128, 2048])
        weg_d = dt_in("weg", [E * 128, 2048]); weu_d = dt_in("weu", [E * 128, 2048]); wed_d = dt_in("wed", [E * 128, 2048])
    out_d = nc.dram_tensor("out", [T, D], F32, kind="ExternalOutput").ap()
    xmid_t = nc.dram_tensor("xmid", [T, D], F32, kind="Internal")
    xmid_d = xmid_t.ap()
    xmid_B = S.dram("xmid")
    out_B = S.dram("outd")

    def bcast(ap_row, n):
        return ap_row.partition_broadcast(128) if hasattr(ap_row, "partition_broadcast") else ap_row

    ident_f = S.sb("identf", [128, 128], F32); ident_b = S.sb("identb", [128, 128], BF16)
    masks = S.sb("masks", [128, 2, 512], BF16)
    ustrict = S.sb("ustrict", [128, 128], BF16); ones_b = S.sb("onesb", [128, 128], BF16)
    iotae = S.sb("iotae", [128, 256], F32); iotap = S.sb("iotap", [128, 1], F32)
    g1row = S.sb("g1row", [128, D], F32)
    rowsave_t = nc.dram_tensor("rowsave", [3, D], F32, kind="Internal")
    rowsave_d = rowsave_t.ap()
    rowsave_B = S.dram("rowsave")
    S.dma("sp", ident_f[:], ident_d, writes=[ident_f])
    S.dma("pool", ident_b[:], ident_d, writes=[ident_b])
    S.dma("pool", masks[:].rearrange("p a b -> p (a b)"), masks_d, writes=[masks])
    S.dma("pool", ustrict[:], ustrict_d, writes=[ustrict])
    S.dma("sp", iotae[:], iotae_d, writes=[iotae]); S.dma("sp", iotap[:], iotap_d, writes=[iotap])
    S.op("pool", lambda e: e.memset(ones_b[:], 1.0), writes=[ones_b])
    neghalf = S.sb("neghalf", [128, 1], F32)
    S.op("pool", lambda e: e.memset(neghalf[:], -0.5), writes=[neghalf])
    P2_BASE = S.sb_ptr

    PB = [S.ps(f"pb{i}", [128, 512], F32) for i in range(7)]
    PTb = S.ps("ptb", [128, 1024], BF16)

    cs_in = S.sb("csin", [128, 16], F32); cs = S.sb("cs", [128, 16], F32)
    badafm = S.sb("badafm", [128, 48], F32); n1fm = S.sb("n1fm", [128, 8], F32)
    modx = S.sb("modx", [128, 16], F32); modc = S.sb("modc", [128, 16], F32)
    a1x = S.sb("a1x", [128, 8], F32); a1c = S.sb("a1c", [128, 8], F32)
    P0_KEEP = S.sb_ptr
    WX = S.sb("WX", [128, 8, NXC], BF16)
    wxv = wx_d.rearrange("(k p) n -> p k n", p=128)
    for k in range(8):
        for hh in range(2):
            S.dma("pool", WX[:, k, hh * 1984:(hh + 1) * 1984], wxv[:, k, hh * 1984:(hh + 1) * 1984], wshared=[WX])
    wua = S.sb("wua", [128, 4, D], BF16); wup = S.sb("wup", [128, 4, D], BF16); wout = S.sb("wout", [128, 8, D], BF16)
    wpool = S.sb("wpool", [128, 4, 128], BF16); pscale = S.sb("pscale", [128, 4], F32)
    invc = S.sb("invc", [128, 4, 2, 8], F32)
    for g in range(4):
        S.dma("pool", wua[:, g, :], wua_d[:, g * 1024:(g + 1) * 1024], wshared=[wua])
        S.dma("pool", wup[:, g, :], wup_d[:, g * 1024:(g + 1) * 1024], wshared=[wup])
    for g in range(8):
        S.dma("pool", wout[:, g, :], wout_d[:, g * 1024:(g + 1) * 1024], wshared=[wout])
    S.dma("pool", wpool[:].rearrange("p a b -> p (a b)"), wpool_d, writes=[wpool])
    S.dma("sp", pscale[:], pscale_d, writes=[pscale])
    S.dma("sp", invc[:].rearrange("p a b c -> p (a b c)"), invc_d, writes=[invc])
    sink_sb = S.sb("sink", [1, 8], F32); esink1 = S.sb("esink1", [1, 8], F32); esink = S.sb("esink", [1, 2, 512], BF16)
    sinksel = S.sb("sinksel", [1, 2, 128], BF16)
    S.dma("sp", sink_sb[:], sink_d, writes=[sink_sb])
    S.op("act", lambda e: e.activation(out=esink1[:], in_=sink_sb[:], func=AF.Exp), reads=[sink_sb], writes=[esink1])
    for h in range(8):
        S.op("dve", lambda e, h=h: e.tensor_copy(out=esink[0:1, h // 4, (h % 4) * 128:(h % 4 + 1) * 128],
                                                 in_=esink1[0:1, h:h + 1].to_broadcast([1, 128])),
             reads=[esink1], writes=[esink])
    S.op("pool", lambda e: e.memset(sinksel[:], 0.0), writes=[sinksel])
    S.op("pool", lambda e: e.memset(sinksel[0:1, 0, 64:128], 1.0), writes=[sinksel])
    S.op("pool", lambda e: e.memset(sinksel[0:1, 1, 0:64], 1.0), writes=[sinksel])

    P2_ACT = S.sb_ptr
    csrep = S.sb("csrep", [128, 8, 128], F32)
    wadab = [S.sb(f"wada{i}", [128, 8, 512], F32) for i in range(2)]
    brow = [S.sb(f"brow{i}", [128, D], F32) for i in range(2)]
    n2row = S.sb("n2row", [128, D], F32)
    sh2row = S.sb("sh2row", [128, D], F32); a2row = S.sb("a2row", [128, D], F32); g2row = S.sb("g2row", [128, D], F32)
    if full:
        xs_t = nc.dram_tensor("xs", [NSLOT, D], BF16, kind="Internal"); xs_d = xs_t.ap(); xs_B = S.dram("xs")
    S.dma("sp", cs_in[:], csin_d, writes=[cs_in])
    S.dma("sp", badafm[:], badafm_d, writes=[badafm]); S.dma("sp", n1fm[:], n1fm_d, writes=[n1fm])
    S.dma("act", n2row[:], n2row_d.partition_broadcast(128), writes=[n2row])
    S.op("act", lambda e: e.activation(out=cs[:], in_=cs_in[:], func=AF.Silu), reads=[cs_in], writes=[cs])
    csv = cs[:].rearrange("p (k t) -> p k t", t=2)
    for k in range(8):
        S.op("dve", lambda e, k=k: e.tensor_copy(out=csrep[:, k, :], in_=cs[:, 2 * k:2 * k + 1].to_broadcast([128, 128])),
             reads=[cs], writes=[csrep])
    wview = wada_d.rearrange("(k p) n -> p k n", p=128)
    rows_dst = {2: g1row, 3: sh2row, 4: a2row, 5: g2row}
    for m2 in range(12):
        m, hf = m2 // 2, m2 % 2
        wb = wadab[m2 % 2]
        for k in range(8):
            S.dma("sp" if k % 2 == 0 else "act", wb[:, k, :], wview[:, k, m2 * 512:(m2 + 1) * 512], wshared=[wb])
        if m < 2:
            pa = PB[m2 % 2]
            for jj in range(4):
                for k in range(8):
                    S.op("pe", lambda e, pa=pa, wb=wb, jj=jj, k=k: e.matmul(
                        out=pa[:, 2 * jj:2 * jj + 2], lhsT=wb[:, k, jj * 128:(jj + 1) * 128],
                        rhs=cs[:, 2 * k:2 * k + 2], start=(k == 0), stop=(k == 7)),
                        reads=[wb, cs], writes=[pa])
            j0_ = m * 8 + hf * 4
            S.op("dve", lambda e, pa=pa, j0_=j0_: e.tensor_tensor(out=modx[:, j0_:j0_ + 4], in0=pa[:, 0:8].rearrange("p (j t) -> p j t", t=2)[:, :, 0],
                                                                  in1=badafm[:, j0_:j0_ + 4], op=ALU.add),
                 reads=[pa, badafm], wshared=[modx])
            S.op("dve", lambda e, pa=pa, j0_=j0_: e.tensor_tensor(out=modc[:, j0_:j0_ + 4], in0=pa[:, 0:8].rearrange("p (j t) -> p j t", t=2)[:, :, 1],
                                                                  in1=badafm[:, j0_:j0_ + 4], op=ALU.add),
                 reads=[pa, badafm], wshared=[modc])
        else:
            br = brow[m % 2]
            if hf == 0:
                S.dma("act", br[:], bada_d[:, m * 1024:(m + 1) * 1024].partition_broadcast(128), writes=[br])
            dst = rows_dst[m]
            pa = PB[2 + hf]
            for k in range(8):
                S.op("pe", lambda e, pa=pa, wb=wb, k=k: e.matmul(
                    out=pa[:], lhsT=csrep[:, k, :], rhs=wb[:, k, :],
                    start=(k == 0), stop=(k == 7)), reads=[wb, csrep], writes=[pa])
            S.op("dve", lambda e, pa=pa, hf=hf, dst=dst, br=br: e.tensor_tensor(
                out=dst[:, hf * 512:(hf + 1) * 512], in0=pa[:], in1=br[:, hf * 512:(hf + 1) * 512], op=ALU.add),
                reads=[pa, br], wshared=[dst])
            if m == 4 and hf == 1:
                S.op("dve", lambda e: e.scalar_tensor_tensor(out=a2row[:], in0=a2row[:], scalar=1.0, in1=n2row[:],
                                                             op0=ALU.add, op1=ALU.mult),
                     reads=[a2row, n2row], writes=[a2row])
    S.op("dve", lambda e: e.scalar_tensor_tensor(out=a1x[:], in0=modx[:, 8:16], scalar=1.0, in1=n1fm[:],
                                                 op0=ALU.add, op1=ALU.mult), reads=[modx, n1fm], writes=[a1x])
    S.op("dve", lambda e: e.scalar_tensor_tensor(out=a1c[:], in0=modc[:, 8:16], scalar=1.0, in1=n1fm[:],
                                                 op0=ALU.add, op1=ALU.mult), reads=[modc, n1fm], writes=[a1c])

    if stage == "P0":
        S.dma("sp", out_d[0:128, :], g1row[:], reads=[g1row], wshared=[out_B])
        S.dma("sp", out_d[128:256, :], a2row[:], reads=[a2row], wshared=[out_B])
        S.dma("sp", out_d[256:384, 0:16], modx[:], reads=[modx], wshared=[out_B])
        S.dma("sp", out_d[256:384, 16:32], modc[:], reads=[modc], wshared=[out_B])
        S.dma("sp", out_d[256:384, 32:40], a1x[:], reads=[a1x], wshared=[out_B])
        S.finalize_and_emit()
        return nc
    for ri, rr in enumerate((sh2row, a2row, g2row)):
        S.dma("sp", rowsave_d[ri:ri + 1, :], rr[0:1, :], reads=[rr], wshared=[rowsave_B])
    S.barrier("pool", lambda e: e.memset(ones_b[:, 0:1], 1.0))
    S.sb_ptr = P2_ACT

    import os
    if os.environ.get("DBG_STOP", "") == "p0end":
        S.finalize_and_emit()
        return nc
    xt = [S.sb(f"xt{i}", [128, D], F32) for i in range(2)]
    xnb = [S.sb(f"xnb{i}", [128, D], BF16) for i in range(2)]
    ssq = [S.sb(f"ssq{i}", [128, 1], F32) for i in range(2)]
    sdv = [S.sb(f"sdv{i}", [128, 1], F32) for i in range(2)]
    rstd = [S.sb(f"rstd{i}", [128, 1], F32) for i in range(2)]
    hT = [S.sb(f"hT{i}", [128, 8, CH], BF16) for i in range(2)]
    hcT = S.sb("hcT", [128, 8, 256], BF16)
    kcT = S.sb("kcT", [128, 256], BF16)
    vctx = S.sb("vctx", [128, 2, 2, 128], BF16)
    kring = [S.sb(f"kring{i}", [128, CH], BF16) for i in range(3)]
    vring = [S.sb(f"vring{i}", [128, 2, 2, 128], BF16) for i in range(3)]
    uring = [S.sb(f"uring{i}", [128, 4, CH + 16], F32) for i in range(2)]
    qT = S.sb("qT", [128, 4, CH], BF16)
    sg = S.sb("sg", [128, 16, CH], BF16)
    ropeC = [S.sb(f"ropeC{i}", [128, CH], F32) for i in range(2)]
    ropeS = [S.sb(f"ropeS{i}", [128, CH], F32) for i in range(2)]
    rt1 = S.sb("rt1", [128, CH], F32); rt2 = S.sb("rt2", [128, CH], F32)
    pT = [S.sb(f"pT{i}", [128, 512], BF16) for i in range(5)]
    recA = [S.sb(f"recA{i}", [128, 512], F32) for i in range(2)]; recB = [S.sb(f"recB{i}", [128, 512], F32) for i in range(2)]
    attnT = S.sb("attnT", [128, 4, CH], BF16)
    ps2 = S.sb("ps2", [128, CH + 16], F32); ps4 = S.sb("ps4", [128, CH + 16], F32); ps8 = S.sb("ps8", [128, CH + 16], F32)
    ps16 = S.sb("ps16", [128, CH + 16], F32)
    dT = S.sb("dT", [128, 4, CH], BF16); poolT = S.sb("poolT", [128, 4, CH], BF16)
    etmp = S.sb("etmp", [128, 8], F32)
    yT = S.sb("yT", [128, 8, CH], BF16); mt1 = S.sb("mt1", [128, CH], F32); mt2 = S.sb("mt2", [128, CH], F32)
    xr = [S.sb(f"xr{i}", [128, D], F32) for i in range(1)] * 2
    xmo = [S.sb(f"xmo{i}", [128, D], F32) for i in range(2)]
    for t in vring + [vctx]:
        S.op("pool", lambda e, t=t: e.memset(t[:], 1.0), writes=[t])
    for u in uring:
        S.op("pool", lambda e, u=u: e.memset(u[:], 0.0), writes=[u])

    if full:
        wedb_t = nc.dram_tensor("wedb", [E * 128, 2048], BF16, kind="Internal"); wedb_d = wedb_t.ap(); wedb_B = S.dram("wedb")
        conv_sem = S.newsem("convwd")

    def conv_wd(c):
        if not full:
            return
        per = E // NCH
        for ex in range(c * per, (c + 1) * per):
            S.dma("pool", wedb_d[ex * 128:(ex + 1) * 128, :], wed_d[ex * 128:(ex + 1) * 128, :], wshared=[wedb_B], sem=conv_sem)

    tile_ctr = [0]
    import os
    if os.environ.get("DBG_STOP", "") == "setup":
        S.finalize_and_emit()
        return nc

    def make_hT(src_rows_ap, avec, shvec, dst, dst_off):
        i = tile_ctr[0] % 2
        tile_ctr[0] += 1
        x_, xn_, ss_, sd_, rs_ = xt[i], xnb[i], ssq[i], sdv[i], rstd[i]
        S.dma("sp", x_[:], src_rows_ap, writes=[x_])
        S.op("act", lambda e: e.activation(out=xn_[:], in_=x_[:], func=AF.Square, accum_out=ss_[:]),
             reads=[x_], writes=[xn_, ss_])
        S.op("pool", lambda e: e.tensor_scalar(out=sd_[:], in0=ss_[:], scalar1=1.0 / D, scalar2=EPS, op0=ALU.mult, op1=ALU.add),
             reads=[ss_], writes=[sd_])
        S.op("pool", lambda e: e.tensor_tensor(out=rs_[:], in0=sd_[:], in1=neghalf[:], op=ALU.pow), reads=[sd_, neghalf], writes=[rs_])
        S.op("dve", lambda e: e.tensor_scalar(out=xn_[:], in0=x_[:], scalar1=rs_[:], scalar2=None, op0=ALU.mult),
             reads=[x_, rs_], writes=[xn_])
        if os.environ.get("DBG_STOP", "") == "h1":
            return
        for k in range(8):
            S.op("pe", lambda e, k=k: e.transpose(out=PTb[:, k * 128:(k + 1) * 128], in_=xn_[:, k * 128:(k + 1) * 128],
                                                  identity=ident_b[:]), reads=[xn_, ident_b], writes=[PTb])
        if os.environ.get("DBG_STOP", "") == "h2":
            return
        EV = os.environ.get("DBG_EVAC", "")
        for k in range(8):
            if i == 0:
                S.op("act", lambda e, k=k: e.activation(out=dst[:, k, dst_off:dst_off + 128], in_=PTb[:, k * 128:(k + 1) * 128],
                                                        func=AF.Identity, scale=avec[:, k:k + 1], bias=shvec[:, k:k + 1]),
                     reads=[PTb, avec, shvec], wshared=[dst])
            else:
                S.op("dve", lambda e, k=k: e.tensor_scalar(out=dst[:, k, dst_off:dst_off + 128], in0=PTb[:, k * 128:(k + 1) * 128],
                                                           scalar1=avec[:, k:k + 1], scalar2=shvec[:, k:k + 1],
                                                           op0=ALU.mult, op1=ALU.add),
                     reads=[PTb, avec, shvec], wshared=[dst])

    pb_rr = [0]

    def next_pb():
        p = PB[pb_rr[0] % 2]
        pb_rr[0] += 1
        return p

    def proj_fm(hsrc, blk, ncols):
        p = next_pb()
        for k in range(8):
            S.op("pe", lambda e, k=k, p=p: e.matmul(out=p[:, 0:ncols], lhsT=WX[:, k, blk * 128:(blk + 1) * 128],
                                                    rhs=hsrc[:, k, 0:ncols], start=(k == 0), stop=(k == 7)),
                 reads=[WX, hsrc], writes=[p])
        return p

    for t in range(2):
        make_hT(ctx_d[t * 128:(t + 1) * 128, :], a1c, modc, hcT, t * 128)
    if os.environ.get("DBG_STOP", "") in ("h1", "h2", "h3"):
        S.finalize_and_emit()
        return nc
    p = proj_fm(hcT, KB, 256)
    S.op("act", lambda e, p=p: e.activation(out=kcT[:], in_=p[:, 0:256], func=AF.Copy), reads=[p], writes=[kcT])

    def proj_v(hsrc, tok_off, dst4, blk):
        p = next_pb()
        for k in range(8):
            S.op("pe", lambda e, k=k, p=p: e.matmul(out=p[:, 0:128], lhsT=hsrc[:, k, tok_off:tok_off + 128],
                                                    rhs=WX[:, k, VB * 128:(VB + 1) * 128], start=(k == 0), stop=(k == 7)),
                 reads=[WX, hsrc], writes=[p])
        S.op("act", lambda e, p=p: e.activation(out=dst4[:, blk, 0, 0:64], in_=p[:, 0:64], func=AF.Copy),
             reads=[p], wshared=[dst4])
        S.op("act", lambda e, p=p: e.activation(out=dst4[:, blk, 1, 64:128], in_=p[:, 64:128], func=AF.Copy),
             reads=[p], wshared=[dst4])

    for t in range(2):
        proj_v(hcT, t * 128, vctx, t)

    def stage_H(c):
        for t in range(2):
            make_hT(x_d[c * CH + t * 128:c * CH + (t + 1) * 128, :], a1x, modx, hT[c % 2], t * 128)

    def load_rope(c):
        S.dma("sp", ropeC[c % 2][:], ropec_d[:, c * CH:(c + 1) * CH], writes=[ropeC[c % 2]])
        S.dma("sp", ropeS[c % 2][:], ropes_d[:, c * CH:(c + 1) * CH], writes=[ropeS[c % 2]])

    def rope_evac(pa, pr, c, dst_ap, dstB):
        rc, rs_ = ropeC[c % 2], ropeS[c % 2]
        S.op("dve", lambda e: e.tensor_tensor(out=rt1[:], in0=pa[:, 0:CH], in1=rc[:], op=ALU.mult),
             reads=[pa, rc], writes=[rt1])
        S.op("dve", lambda e: e.tensor_tensor(out=rt2[:], in0=pr[:, 0:CH], in1=rs_[:], op=ALU.mult),
             reads=[pr, rs_], writes=[rt2])
        S.op("pool", lambda e: e.tensor_tensor(out=dst_ap, in0=rt1[:], in1=rt2[:], op=ALU.add),
             reads=[rt1, rt2], wshared=[dstB])

    def kvu_units(c):
        h = hT[c % 2]
        u = uring[c % 2]
        units = []

        def unit_k():
            pa = proj_fm(h, KB, CH); pr = proj_fm(h, KR, CH)
            rope_evac(pa, pr, c, kring[c % 3][:], kring[c % 3])
        units.append(unit_k)
        for t in range(2):
            units.append(lambda t=t: proj_v(h, t * 128, vring[c % 3], t))
        for g in range(4):
            def unit_u(g=g):
                p = proj_fm(h, UB + g, CH)
                S.op("act", lambda e, p=p, g=g: e.activation(out=u[:, g, 8:8 + CH], in_=p[:, 0:CH], func=AF.Copy),
                     reads=[p], wshared=[u])
            units.append(unit_u)

        def unit_halo():
            if c > 0:
                up = uring[(c - 1) % 2]
                S.op("pool", lambda e: e.tensor_copy(out=up[:, :, 8 + CH:16 + CH], in_=u[:, :, 8:16]), reads=[u], wshared=[up])
                S.op("pool", lambda e: e.tensor_copy(out=u[:, :, 0:8], in_=up[:, :, CH:8 + CH]), reads=[up], wshared=[u])
            else:
                S.op("pool", lambda e: e.memset(u[:, :, 0:8], 0.0), wshared=[u])
            if c == NCH - 1:
                S.op("pool", lambda e: e.memset(u[:, :, 8 + CH:16 + CH], 0.0), wshared=[u])
        units.append(unit_halo)
        return units

    def stage_KVU(c):
        for f in kvu_units(c):
            f()

    def stage_QG(c):
        h = hT[c % 2]
        for q in range(4):
            pa = proj_fm(h, QB + q, CH); pr = proj_fm(h, QR + q, CH)
            rope_evac(pa, pr, c, qT[:, q, :], qT)
        for j in range(16):
            p = proj_fm(h, GA + j, CH)
            S.op("act", lambda e, p=p, j=j: e.activation(out=sg[:, j, :], in_=p[:, 0:CH], func=AF.Sigmoid),
                 reads=[p], wshared=[sg])

    pt_rr = [0]
    PSC = [PB[2], PB[3], PB[6], PB[0]]

    def attention(c):
        steps = []
        groups = []
        for i in range(2):
            n = 2 * c + i
            for g in range(2):
                gs = slice(g * 64, (g + 1) * 64)
                keys = []
                if n > 0:
                    cc, bb = (n - 1) // 2, (n - 1) % 2
                    keys.append((kring[cc % 3], kring[cc % 3][gs, bb * 128:(bb + 1) * 128], vring[cc % 3], vring[cc % 3][:, bb, g, :], 0))
                keys.append((kring[c % 3], kring[c % 3][gs, i * 128:(i + 1) * 128], vring[c % 3], vring[c % 3][:, i, g, :], None))
                if n < NT - 1:
                    cc, bb = (n + 1) // 2, (n + 1) % 2
                    keys.append((kring[cc % 3], kring[cc % 3][gs, bb * 128:(bb + 1) * 128], vring[cc % 3], vring[cc % 3][:, bb, g, :], 1))
                for t in range(2):
                    keys.append((kcT, kcT[gs, t * 128:(t + 1) * 128], vctx, vctx[:, t, g, :], None))
                gi = len(groups)
                groups.append(dict(i=i, g=g, po=PB[4 + gi % 2], nk=len(keys), qap=qT[gs, :, i * 128:(i + 1) * 128],
                                   rA=recA[gi % 2], rB=recB[gi % 2]))
                for ki, key in enumerate(keys):
                    steps.append((gi, ki, key))
        bufs = {}

        def emit_qk_exp(s):
            gi, ki, (kB, kap, vB, vap, mk) = steps[s]
            psc = PSC[pt_rr[0] % len(PSC)]
            pt = pT[pt_rr[0] % len(pT)]
            pt_rr[0] += 1
            bufs[s] = pt
            qap = groups[gi]["qap"]
            S.op("pe", lambda e, psc=psc, kap=kap, qap=qap, mk=mk: e.matmul(out=psc[:].rearrange("p (a b) -> p a b", a=4), lhsT=kap, rhs=qap,
                                                                            start=True, stop=(mk is None)), reads=[kB, qT], writes=[psc])
            if mk is not None:
                S.op("pe", lambda e, psc=psc, mk=mk: e.matmul(out=psc[:], lhsT=ident_b[:], rhs=masks[:, mk, :], start=False, stop=True),
                     reads=[ident_b, masks], writes=[psc])
            S.op("act", lambda e, psc=psc, pt=pt: e.activation(out=pt[:], in_=psc[:], func=AF.Exp, scale=0.125),
                 reads=[psc], writes=[pt])

        def emit_pv(s):
            gi, ki, (kB, kap, vB, vap, mk) = steps[s]
            G_ = groups[gi]; po = G_["po"]; pt = bufs[s]; g = G_["g"]
            S.op("pe", lambda e, po=po, vap=vap, pt=pt, ki=ki: e.matmul(out=po[:], lhsT=vap, rhs=pt[:], start=(ki == 0), stop=False),
                 reads=[vB, pt], writes=[po])
            if ki == G_["nk"] - 1:
                S.op("pe", lambda e, po=po, g=g: e.matmul(out=po[:], lhsT=sinksel[0:1, g, :], rhs=esink[0:1, g, :], start=False, stop=True),
                     reads=[sinksel, esink], writes=[po])

        def emit_norm_a(gi):
            G_ = groups[gi]; po = G_["po"]; rA = G_["rA"]
            ds = slice(64, 128) if G_["g"] == 0 else slice(0, 64)
            S.op("dve", lambda e, po=po, ds=ds, rA=rA: e.reciprocal(out=rA[ds, :], in_=po[ds, :]), reads=[po], writes=[rA])

        def emit_norm_b(gi):
            G_ = groups[gi]; po = G_["po"]; rA = G_["rA"]; rB = G_["rB"]; i = G_["i"]
            ns = slice(0, 64) if G_["g"] == 0 else slice(64, 128)
            ds = slice(64, 128) if G_["g"] == 0 else slice(0, 64)
            S.op("act", lambda e, ns=ns, ds=ds, rA=rA, rB=rB: e.activation(out=rB[ns, :], in_=rA[ds, :], func=AF.Copy),
                 reads=[rA], writes=[rB])
            S.op("dve", lambda e, po=po, ns=ns, i=i, rB=rB: e.tensor_tensor(
                out=attnT[ns, :, i * 128:(i + 1) * 128], in0=po[ns, :].rearrange("p (a b) -> p a b", a=4),
                in1=rB[ns, :].rearrange("p (a b) -> p a b", a=4), op=ALU.mult), reads=[po, rB], wshared=[attnT])

        nsteps = len(steps)
        pend = []
        LA = 3
        for s0 in range(min(LA, nsteps)):
            emit_qk_exp(s0)
        for s in range(nsteps):
            if s + LA < nsteps:
                emit_qk_exp(s + LA)
            emit_pv(s)
            gi, ki, _ = steps[s]
            if ki == groups[gi]["nk"] - 1:
                emit_norm_a(gi)
                pend.append((s + 3, gi))
            while pend and pend[0][0] <= s:
                emit_norm_b(pend.pop(0)[1])
        for _, gi in pend:
            emit_norm_b(gi)

    def poolmix_a(c):
        u = uring[c % 2]
        W = CH + 16
        for g in range(4):
            ug = u[:, g, :]
            S.op("pool", lambda e, ug=ug: e.tensor_tensor(out=ps2[:, 1:W], in0=ug[:, 0:W - 1], in1=ug[:, 1:W], op=ALU.add),
                 reads=[u], writes=[ps2])
            src = ps2
            if g >= 1:
                S.op("pool", lambda e: e.tensor_tensor(out=ps4[:, 2:W - 1], in0=ps2[:, 1:W - 2], in1=ps2[:, 3:W], op=ALU.add),
                     reads=[ps2], writes=[ps4])
                src = ps4
            if g >= 2:
                S.op("pool", lambda e: e.tensor_tensor(out=ps8[:, 4:W - 3], in0=ps4[:, 2:W - 5], in1=ps4[:, 6:W - 1], op=ALU.add),
                     reads=[ps4], writes=[ps8])
                src = ps8
            if g >= 3:
                S.op("pool", lambda e: e.tensor_tensor(out=ps16[:, 8:W - 7], in0=ps8[:, 4:W - 11], in1=ps8[:, 12:W - 3], op=ALU.add),
                     reads=[ps8], writes=[ps16])
                src = ps16
            w = 2 ** (g + 1)
            S.op("dve", lambda e, src=src, g=g, w=w, ug=ug: e.scalar_tensor_tensor(
                out=dT[:, g, :], in0=src[:, 8:8 + CH], scalar=1.0 / w, in1=ug[:, 8:8 + CH], op0=ALU.mult, op1=ALU.subtract),
                reads=[src, u], wshared=[dT])
            for (cond, side, col) in ((c == 0, 0, 0), (c == NCH - 1, 1, CH - 8)):
                if cond:
                    S.op("dve", lambda e, src=src, g=g, side=side, col=col: e.tensor_tensor(
                        out=etmp[:], in0=src[:, 8 + col:16 + col], in1=invc[:, g, side, :], op=ALU.mult),
                        reads=[src, invc], writes=[etmp])
                    S.op("dve", lambda e, g=g, col=col, ug=ug: e.tensor_tensor(
                        out=dT[:, g, col:col + 8], in0=etmp[:], in1=ug[:, 8 + col:16 + col], op=ALU.subtract),
                        reads=[etmp, u, dT], wshared=[dT])

    def poolmix_b(c):
        for g in range(4):
            p = next_pb()
            S.op("pe", lambda e, p=p, g=g: e.matmul(out=p[:, 0:CH], lhsT=wpool[:, g, :], rhs=dT[:, g, :], start=True, stop=True),
                 reads=[wpool, dT], writes=[p])
            S.op("act", lambda e, p=p, g=g: e.activation(out=poolT[:, g, :], in_=p[:, 0:CH], func=AF.Identity, scale=pscale[:, g:g + 1]),
                 reads=[p, pscale], wshared=[poolT])

    def merge(c, extra_units=()):
        for oc in range(8):
            pa = PB[2 + oc % 2]; pb_ = (PB[6], PB[0], PB[1])[oc % 3]
            for q in range(4):
                S.op("pe", lambda e, q=q, oc=oc, pa=pa: e.matmul(out=pa[:, 0:CH], lhsT=wua[:, q, oc * 128:(oc + 1) * 128], rhs=attnT[:, q, :],
                                                          start=(q == 0), stop=(q == 3)), reads=[wua, attnT], writes=[pa])
            for g in range(4):
                S.op("pe", lambda e, g=g, oc=oc, pb_=pb_: e.matmul(out=pb_[:, 0:CH], lhsT=wup[:, g, oc * 128:(oc + 1) * 128], rhs=poolT[:, g, :],
                                                                   start=(g == 0), stop=(g == 3)), reads=[wup, poolT], writes=[pb_])
            ma, mb = (mt1, mt2) if oc % 2 == 0 else (rt1, rt2)
            S.op("dve", lambda e, oc=oc, pa=pa, ma=ma: e.tensor_tensor(out=ma[:], in0=pa[:, 0:CH], in1=sg[:, oc, :], op=ALU.mult),
                 reads=[pa, sg], writes=[ma])
            S.op("dve", lambda e, oc=oc, pb_=pb_, mb=mb: e.tensor_tensor(out=mb[:], in0=pb_[:, 0:CH], in1=sg[:, 8 + oc, :], op=ALU.mult),
                 reads=[pb_, sg], writes=[mb])
            S.op("pool", lambda e, oc=oc, ma=ma, mb=mb: e.tensor_tensor(out=yT[:, oc, :], in0=ma[:], in1=mb[:], op=ALU.add),
                 reads=[ma, mb], wshared=[yT])
        mix_units = []
        for t in range(2):
            tok0 = c * CH + t * 128
            xr_ = xr[t]; xo_ = xmo[t]
            for n in range(2):
                def unit_mm(t=t, n=n, xr_=xr_, xo_=xo_, tok0=tok0):
                    if n == 0:
                        S.dma("sp", xr_[:], x_d[tok0:tok0 + 128, :], writes=[xr_])
                    pm = PB[4 + n]
                    for oc in range(8):
                        S.op("pe", lambda e, pm=pm, oc=oc, t=t, n=n: e.matmul(out=pm[:], lhsT=yT[:, oc, t * 128:(t + 1) * 128],
                                                                              rhs=wout[:, oc, n * 512:(n + 1) * 512], start=(oc == 0), stop=(oc == 7)),
                             reads=[yT, wout], writes=[pm])
                    S.op("dve", lambda e, pm=pm, n=n, xo_=xo_: e.tensor_tensor(out=xo_[:, n * 512:(n + 1) * 512], in0=pm[:],
                                                                              in1=g1row[:, n * 512:(n + 1) * 512], op=ALU.mult),
                         reads=[pm, g1row], wshared=[xo_])
                mix_units.append(unit_mm)

            def unit_st(xr_=xr_, xo_=xo_, tok0=tok0):
                S.op("dve", lambda e, xo_=xo_, xr_=xr_: e.tensor_tensor(out=xo_[:], in0=xo_[:], in1=xr_[:], op=ALU.add),
                     reads=[xo_, xr_], writes=[xo_])
                dstd = out_d if stage == "A" else xmid_d
                S.dma("sp", dstd[tok0:tok0 + 128, :], xo_[:], reads=[xo_], wshared=[out_B if stage == "A" else xmid_B])
            mix_units.append(unit_st)
        extra = list(extra_units)
        while mix_units or extra:
            if mix_units:
                mix_units.pop(0)()
            if extra:
                extra.pop(0)()
            if extra and len(extra) > len(mix_units):
                extra.pop(0)()

    stage_H(0); load_rope(0); stage_KVU(0)
    if NCH > 1:
        stage_H(1); load_rope(1); stage_KVU(1)
    for c in range(NCH):
        stage_QG(c)
        poolmix_a(c)
        conv_wd(c)
        attention(c)
        poolmix_b(c)
        extra = ()
        if c + 2 < NCH:
            stage_H(c + 2); load_rope(c + 2)
            extra = kvu_units(c + 2)
        merge(c, extra)

    if stage == "A":
        S.finalize_and_emit()
        return nc
    PHASE3(S, locals())
    S.finalize_and_emit()
    return nc


def PHASE3(S, ns):
    nc = S.nc
    LB = 43
    PB = ns["PB"]; PTb = ns["PTb"]
    ident_f = ns["ident_f"]; ident_b = ns["ident_b"]; ustrict = ns["ustrict"]; ones_b = ns["ones_b"]
    iotae = ns["iotae"]; iotap = ns["iotap"]
    rowsave_d = ns["rowsave_d"]; rowsave_B = ns["rowsave_B"]; fgrow_d = ns["fgrow_d"]
    wr_d = ns["wr_d"]; rbias_d = ns["rbias_d"]; wsg_d = ns["wsg_d"]; wsu_d = ns["wsu_d"]; wsd_d = ns["wsd_d"]
    weg_d = ns["weg_d"]; weu_d = ns["weu_d"]; wed_d = ns["wed_d"]
    xmid_d = ns["xmid_d"]; xmid_B = ns["xmid_B"]; out_d = ns["out_d"]; out_B = ns["out_B"]
    xs_d = ns["xs_d"]; xs_B = ns["xs_B"]
    ys_t = nc.dram_tensor("ys", [NSLOT, D], BF16, kind="Internal"); ys_d = ys_t.ap(); ys_B = S.dram("ys")
    x2_t = nc.dram_tensor("x2", [T, D], F32, kind="Internal"); x2_d = x2_t.ap(); x2_B = S.dram("x2")

    S.barrier("pool", lambda e: e.memset(ones_b[:, 0:1], 1.0))
    S.sb_ptr = ns["P0_KEEP"]
    g2row = S.sb("g2row3", [128, D], F32); fgrow = S.sb("fgrow", [128, D], F32)
    w8all = S.sb("w8all", [128, NT, 8], F32); sloti = S.sb("sloti", [128, NT, 8], I32)
    idxall = S.sb("idxall", [128, 512], I32)
    P3_KEEP = S.sb_ptr
    sh2row = S.sb("sh2row3", [128, D], F32); a2row = S.sb("a2row3", [128, D], F32)
    S.dma("sp", sh2row[:], rowsave_d[0:1, :].partition_broadcast(128), reads=[rowsave_B], writes=[sh2row])
    S.dma("sp", a2row[:], rowsave_d[1:2, :].partition_broadcast(128), reads=[rowsave_B], writes=[a2row])
    S.dma("sp", g2row[:], rowsave_d[2:3, :].partition_broadcast(128), reads=[rowsave_B], writes=[g2row])
    S.dma("sp", fgrow[:], fgrow_d.partition_broadcast(128), writes=[fgrow])
    rbias = S.sb("rbias", [128, 256], F32)
    S.dma("act", rbias[:], rbias_d.partition_broadcast(128), writes=[rbias])
    wr = S.sb("wr", [128, 8, 256], F32)
    S.dma("sp", wr[:].rearrange("p a b -> p (a b)"), wr_d, writes=[wr])
    wsg = S.sb("wsg", [128, 8, 256], BF16); wsu = S.sb("wsu", [128, 8, 256], BF16); wsd = S.sb("wsd", [128, 2, 1024], BF16)
    S.dma("pool", wsg[:].rearrange("p a b -> p (a b)"), wsg_d, writes=[wsg])
    S.dma("pool", wsu[:].rearrange("p a b -> p (a b)"), wsu_d, writes=[wsu])
    S.dma("pool", wsd[:].rearrange("p a b -> p (a b)"), wsd_d, writes=[wsd])
    maskall = S.sb("maskall", [128, NT, 256], BF16)
    h2ball = S.sb("h2ball", [128, NT, D], BF16)
    eidxu = S.sb("eidxu", [128, NT, 8], U32); eidxf = S.sb("eidxf", [128, NT, 8], F32); slotf = S.sb("slotf", [128, NT, 8], F32)
    xmb = [S.sb(f"xm{i}", [128, D], F32) for i in range(2)]
    h2fs = [S.sb(f"h2f{j}", [128, D], F32) for j in range(2)]
    h2T32s = [S.sb(f"h2T32{j}", [128, 8, 128], F32) for j in range(2)]
    h2Tb = S.sb("h2Tb", [128, 8, 128], BF16)
    iotab = S.sb("iotab", [128, 512], F32); pendsT = S.sb("pendsT", [128, 2], F32)
    Mh = [S.sb(f"Mh{j}", [128, 512], BF16) for j in range(2)]
    S.dma("sp", iotab[:], ns["iotab_d"], writes=[iotab])
    ss = S.sb("ss3", [128, 1], F32); sd = S.sb("sd3", [128, 1], F32); rs = S.sb("rs3", [128, 1], F32)
    sv = S.sb("sv", [128, 256], F32); sel = S.sb("sel", [128, 256], F32); selm = S.sb("selm", [128, 256], F32)
    sw = S.sb("sw", [128, 256], F32); G = S.sb("G", [128, 256], F32); junk256 = S.sb("junk256", [128, 256], F32)
    top8g = S.sb("top8g", [128, 8, 8], F32); gs = S.sb("gs", [128, 8], F32); gsort = S.sb("gsort", [128, 8], F32)
    gmask = S.sb("gmask", [128, 8], F32); negb = S.sb("negb", [128, 8], F32); top8 = S.sb("top8", [128, 8], F32)
    ssum = S.sb("ssum", [128, 1], F32); rs2 = S.sb("rs2", [128, 1], F32)
    sgs = S.sb("sgs", [128, 256], F32); actsh = S.sb("actsh", [128, 256], BF16)
    x2t = [S.sb(f"x2t{i}", [128, D], F32) for i in range(2)]
    cnt = S.sb("cnt", [128, 256], F32); tq = S.sb("tq", [128, 256], F32); nbi = S.sb("nbi", [128, 256], I32)
    padded = S.sb("padded", [128, 256], F32); fix = S.sb("fix", [128, 256], F32)
    pends = S.sb("pends", [128, 256], F32); pstart = S.sb("pstart", [128, 256], F32); ones256 = S.sb("ones256", [128, 256], F32)
    eb = S.sb("eb", [128, 512], F32); idxf = S.sb("idxf", [128, 512], F32)
    ebs = S.sb("ebs", [128, 512], F32); same = S.sb("same", [128, 512], F32)
    macc = S.sb("macc", [128, 256], BF16); slotm = S.sb("slotm", [128, 256], F32)
    S.op("pool", lambda e: e.memset(macc[:], 0.0), writes=[macc])
    S.op("pool", lambda e: e.memset(ones256[:], 1.0), writes=[ones256])
    S.op("pool", lambda e: e.memset(eb[:], 256.0), writes=[eb])

    neghalf = ns["neghalf"]

    def load_xm(i):
        S.dma("sp", xmb[i % 2][:], xmid_d[i * 128:(i + 1) * 128, :], reads=[xmid_B], writes=[xmb[i % 2]])

    def stage_A(i):
        xm = xmb[i % 2]; h2f = h2fs[i % 2]; h2T32 = h2T32s[i % 2]
        if i + 1 < NT:
            load_xm(i + 1)
        S.op("act", lambda e: e.activation(out=h2f[:], in_=xm[:], func=AF.Square, accum_out=ss[:]), reads=[xm], writes=[h2f, ss])
        S.op("pool", lambda e: e.tensor_scalar(out=sd[:], in0=ss[:], scalar1=1.0 / D, scalar2=EPS, op0=ALU.mult, op1=ALU.add), reads=[ss], writes=[sd])
        S.op("pool", lambda e: e.tensor_tensor(out=rs[:], in0=sd[:], in1=neghalf[:], op=ALU.pow), reads=[sd, neghalf], writes=[rs])
        S.op("dve", lambda e: e.scalar_tensor_tensor(out=h2f[:], in0=xm[:], scalar=rs[:, 0:1], in1=a2row[:], op0=ALU.mult, op1=ALU.mult),
             reads=[xm, rs, a2row], writes=[h2f])
        S.op("dve", lambda e: e.tensor_tensor(out=h2f[:], in0=h2f[:], in1=sh2row[:], op=ALU.add), reads=[h2f, sh2row], writes=[h2f])
        S.op("act", lambda e: e.activation(out=h2ball[:, i, :], in_=h2f[:], func=AF.Copy), reads=[h2f], wshared=[h2ball])
        for k in range(8):
            pb = PB[0] if k < 4 else PB[1]
            S.op("pe", lambda e, k=k, pb=pb: e.transpose(out=pb[:, (k % 4) * 128:(k % 4 + 1) * 128], in_=h2f[:, k * 128:(k + 1) * 128], identity=ident_f[:]),
                 reads=[h2f, ident_f], writes=[pb])
        S.op("act", lambda e: e.activation(out=h2T32[:, 0:4, :].rearrange("p a b -> p (a b)"), in_=PB[0][:], func=AF.Copy), reads=[PB[0]], wshared=[h2T32])
        S.op("act", lambda e: e.activation(out=h2T32[:, 4:8, :].rearrange("p a b -> p (a b)"), in_=PB[1][:], func=AF.Copy), reads=[PB[1]], wshared=[h2T32])

    def stage_B1(i):
        h2T32 = h2T32s[i % 2]
        for k in range(8):
            S.op("pe", lambda e, k=k: e.matmul(out=PB[2][:, 0:256], lhsT=h2T32[:, k, :], rhs=wr[:, k, :], start=(k == 0), stop=(k == 7)),
                 reads=[h2T32, wr], writes=[PB[2]])
        S.op("act", lambda e: e.activation(out=sv[:], in_=PB[2][:, 0:256], func=AF.Sigmoid), reads=[PB[2]], writes=[sv])

    def stage_B(i):
        S.op("dve", lambda e: e.tensor_tensor(out=sel[:], in0=sv[:], in1=rbias[:], op=ALU.add), reads=[sv, rbias], writes=[sel])
        for g in range(8):
            S.op("dve", lambda e, g=g: e.max(out=top8g[:, g, :], in_=sel[:, g * 32:(g + 1) * 32]), reads=[sel], wshared=[top8g])
        S.op("dve", lambda e: e.tensor_tensor(out=gs[:], in0=top8g[:, :, 0], in1=top8g[:, :, 1], op=ALU.add), reads=[top8g], writes=[gs])
        S.op("dve", lambda e: e.max(out=gsort[:], in_=gs[:]), reads=[gs], writes=[gsort])
        S.op("dve", lambda e: e.tensor_scalar(out=gmask[:], in0=gs[:], scalar1=gsort[:, 3:4], scalar2=None, op0=ALU.is_ge), reads=[gs, gsort], writes=[gmask])
        S.op("dve", lambda e: e.tensor_scalar(out=negb[:], in0=gmask[:], scalar1=-1.0, scalar2=1e30, op0=ALU.add, op1=ALU.mult), reads=[gmask], writes=[negb])
        for g in range(8):
            S.op("dve", lambda e, g=g: e.tensor_scalar(out=selm[:, g * 32:(g + 1) * 32], in0=sel[:, g * 32:(g + 1) * 32],
                                                       scalar1=gmask[:, g:g + 1], scalar2=negb[:, g:g + 1], op0=ALU.mult, op1=ALU.add),
                 reads=[sel, gmask, negb], wshared=[selm])
        S.op("dve", lambda e: e.max(out=top8[:], in_=selm[:]), reads=[selm], writes=[top8])
        S.op("dve", lambda e: e.tensor_scalar(out=maskall[:, i, :], in0=selm[:], scalar1=top8[:, 7:8], scalar2=None, op0=ALU.is_ge),
             reads=[selm, top8], wshared=[maskall])
        S.op("dve", lambda e: e.scalar_tensor_tensor(out=sw[:], in0=sv[:], scalar=1.0, in1=maskall[:, i, :], op0=ALU.mult, op1=ALU.mult, accum_out=ssum[:]),
             reads=[sv, maskall], writes=[sw, ssum])
        S.op("dve", lambda e: e.reciprocal(out=rs2[:], in_=ssum[:]), reads=[ssum], writes=[rs2])
        S.op("dve", lambda e: e.tensor_scalar(out=G[:], in0=sw[:], scalar1=rs2[:, 0:1], scalar2=2.5, op0=ALU.mult, op1=ALU.mult), reads=[sw, rs2], writes=[G])
        S.op("dve", lambda e: e.max(out=w8all[:, i, :], in_=G[:]), reads=[G], wshared=[w8all])
        S.op("dve", lambda e: e.max_index(out=eidxu[:, i, :], in_max=w8all[:, i, :], in_values=G[:]), reads=[G, w8all], wshared=[eidxu])
        S.op("pe", lambda e: e.matmul(out=PB[3][:, 0:256], lhsT=ones_b[:], rhs=maskall[:, i, :], start=(i == 0), stop=(i == NT - 1)),
             reads=[ones_b, maskall], writes=[PB[3]])

    load_xm(0)
    stage_A(0)
    for i in range(NT):
        stage_B1(i)
        if i + 1 < NT:
            stage_A(i + 1)
        stage_B(i)

    S.op("dve", lambda e: e.tensor_copy(out=cnt[:], in_=PB[3][:, 0:256]), reads=[PB[3]], writes=[cnt])
    S.op("dve", lambda e: e.tensor_scalar(out=tq[:], in0=cnt[:], scalar1=127.0, scalar2=1.0 / 128, op0=ALU.add, op1=ALU.mult), reads=[cnt], writes=[tq])
    S.op("dve", lambda e: e.tensor_scalar(out=nbi[:], in0=tq[:], scalar1=-0.49609375, scalar2=None, op0=ALU.add), reads=[tq], writes=[nbi])
    S.op("dve", lambda e: e.tensor_copy(out=padded[:], in_=nbi[:]), reads=[nbi], writes=[padded])
    S.op("dve", lambda e: e.tensor_scalar(out=padded[:], in0=padded[:], scalar1=128.0, scalar2=None, op0=ALU.mult), reads=[padded], writes=[padded])
    S.op("dve", lambda e: e.tensor_tensor(out=fix[:], in0=padded[:], in1=cnt[:], op=ALU.is_lt), reads=[padded, cnt], writes=[fix])
    S.op("dve", lambda e: e.scalar_tensor_tensor(out=padded[:], in0=fix[:], scalar=128.0, in1=padded[:], op0=ALU.mult, op1=ALU.add), reads=[fix, padded], writes=[padded])
    S.op("dve", lambda e: e.tensor_scalar(out=tq[:], in0=padded[:], scalar1=-128.0, scalar2=None, op0=ALU.add), reads=[padded], writes=[tq])
    S.op("dve", lambda e: e.tensor_tensor(out=fix[:], in0=tq[:], in1=cnt[:], op=ALU.is_ge), reads=[tq, cnt], writes=[fix])
    S.op("dve", lambda e: e.scalar_tensor_tensor(out=padded[:], in0=fix[:], scalar=-128.0, in1=padded[:], op0=ALU.mult, op1=ALU.add), reads=[fix, padded], writes=[padded])
    S.op("dve", lambda e: e.tensor_tensor_scan(out=pends[:], data0=ones256[:], data1=padded[:], initial=0.0, op0=ALU.mult, op1=ALU.add),
         reads=[ones256, padded], writes=[pends])
    S.op("dve", lambda e: e.tensor_tensor(out=pstart[:], in0=pends[:], in1=padded[:], op=ALU.subtract), reads=[pends, padded], writes=[pstart])
    for h in range(2):
        S.op("pe", lambda e, h=h: e.transpose(out=PB[4][:, h * 128:(h + 1) * 128], in_=pends[:, h * 128:(h + 1) * 128], identity=ident_f[:]),
             reads=[pends, ident_f], writes=[PB[4]])
    S.op("dve", lambda e: e.tensor_copy(out=pendsT[:], in_=PB[4][:, 0:256].rearrange("p (h c) -> p h c", h=2)[:, :, 0]), reads=[PB[4]], writes=[pendsT])
    for h in range(2):
        S.op("dve", lambda e, h=h: e.tensor_scalar(out=Mh[h][:], in0=iotab[:], scalar1=pendsT[:, h:h + 1], scalar2=None, op0=ALU.is_ge),
             reads=[iotab, pendsT], writes=[Mh[h]])
        S.op("pe", lambda e, h=h: e.matmul(out=PB[5][:], lhsT=ones_b[:], rhs=Mh[h][:], start=(h == 0), stop=(h == 1)),
             reads=[ones_b, Mh[h]], writes=[PB[5]])
    S.op("dve", lambda e: e.tensor_copy(out=eb[:], in_=PB[5][:]), reads=[PB[5]], writes=[eb])
    S.op("dve", lambda e: e.memset(eb[:, 511:512], 256.0), reads=[eb], writes=[eb])
    S.op("pool", lambda e: e.memset(ebs[:, 0:1], -1.0), wshared=[ebs])
    S.op("dve", lambda e: e.tensor_copy(out=ebs[:, 1:512], in_=eb[:, 0:511]), reads=[eb], wshared=[ebs])
    S.op("dve", lambda e: e.tensor_tensor(out=same[:], in0=eb[:], in1=ebs[:], op=ALU.is_equal), reads=[eb, ebs], writes=[same])
    for l0 in range(0, 512, LB):
        S.op("dve", lambda e, l0=l0: e.memset(same[:, l0:l0 + 1], 0.0), reads=[same], writes=[same])
    S.op("dve", lambda e: e.tensor_scalar(out=idxf[:], in0=eb[:], scalar1=128.0, scalar2=iotap[:, 0:1], op0=ALU.mult, op1=ALU.add), reads=[eb, iotap], writes=[idxf])
    S.op("dve", lambda e: e.scalar_tensor_tensor(out=idxf[:], in0=same[:], scalar=1.0e6, in1=idxf[:], op0=ALU.mult, op1=ALU.add), reads=[same, idxf], writes=[idxf])
    S.op("dve", lambda e: e.tensor_copy(out=idxall[:], in_=idxf[:]), reads=[idxf], writes=[idxall])

    h2Tb2 = S.sb("h2Tb2", [128, D], BF16)
    tsh = S.sb("tsh", [128, 256], F32)
    load_xm(0)
    for i in range(NT):
        xm = xmb[i % 2]
        if i + 1 < NT:
            load_xm(i + 1)
        S.op("pe", lambda e, i=i: e.matmul(out=PB[0][:, 0:256], lhsT=ustrict[:], rhs=maskall[:, i, :], start=True, stop=False), reads=[ustrict, maskall], writes=[PB[0]])
        S.op("pe", lambda e: e.matmul(out=PB[0][:, 0:256], lhsT=ones_b[:], rhs=macc[:], start=False, stop=True), reads=[ones_b, macc], writes=[PB[0]])
        S.op("dve", lambda e: e.tensor_tensor(out=slotm[:], in0=PB[0][:, 0:256], in1=pstart[:], op=ALU.add), reads=[PB[0], pstart], writes=[slotm])
        S.op("dve", lambda e, i=i: e.tensor_tensor(out=macc[:], in0=macc[:], in1=maskall[:, i, :], op=ALU.add), reads=[macc, maskall], writes=[macc])
        S.op("dve", lambda e, i=i: e.tensor_copy(out=eidxf[:, i, :], in_=eidxu[:, i, :]), reads=[eidxu], wshared=[eidxf])
        for k in range(8):
            S.op("dve", lambda e, i=i, k=k: e.scalar_tensor_tensor(out=junk256[:], in0=iotae[:], scalar=eidxf[:, i, k:k + 1], in1=slotm[:],
                                                                   op0=ALU.is_equal, op1=ALU.mult, accum_out=slotf[:, i, k:k + 1]),
                 reads=[iotae, eidxf, slotm], writes=[junk256], wshared=[slotf])
        S.op("dve", lambda e, i=i: e.tensor_copy(out=sloti[:, i, :], in_=slotf[:, i, :]), reads=[slotf], wshared=[sloti])
        for k in range(8):
            S.dma_fn("pool", lambda e, i=i, k=k: e.indirect_dma_start(
                out=xs_d, out_offset=bass.IndirectOffsetOnAxis(ap=sloti[:, i, k:k + 1], axis=0),
                in_=h2ball[:, i, :], in_offset=None, bounds_check=S.reg(e, NSLOT - 1), oob_is_err=False),
                reads=[h2ball, sloti], wshared=[xs_B])
        for k in range(8):
            S.op("pe", lambda e, i=i, k=k: e.transpose(out=PTb[:, k * 128:(k + 1) * 128], in_=h2ball[:, i, k * 128:(k + 1) * 128], identity=ident_b[:]),
                 reads=[h2ball, ident_b], writes=[PTb])
        S.op("act", lambda e: e.activation(out=h2Tb2[:], in_=PTb[:], func=AF.Copy), reads=[PTb], writes=[h2Tb2])
        for fc in range(2):
            for (wsrc, off) in ((wsg, 0), (wsu, 256)):
                for k in range(8):
                    S.op("pe", lambda e, fc=fc, wsrc=wsrc, off=off, k=k: e.matmul(out=PB[4][:, off + fc * 128:off + (fc + 1) * 128],
                                                                                   lhsT=wsrc[:, k, fc * 128:(fc + 1) * 128], rhs=h2Tb2[:, k * 128:(k + 1) * 128],
                                                                                   start=(k == 0), stop=(k == 7)),
                         reads=[wsrc, h2Tb2], writes=[PB[4]])
        S.op("act", lambda e: e.activation(out=sgs[:], in_=PB[4][:, 0:256], func=AF.Sigmoid), reads=[PB[4]], writes=[sgs])
        S.op("dve", lambda e: e.tensor_tensor(out=tsh[:], in0=sgs[:], in1=PB[4][:, 0:256], op=ALU.mult), reads=[sgs, PB[4]], writes=[tsh])
        S.op("dve", lambda e: e.tensor_tensor(out=actsh[:], in0=tsh[:], in1=PB[4][:, 256:512], op=ALU.mult), reads=[tsh, PB[4]], writes=[actsh])
        xo = x2t[i % 2]
        for n in range(2):
            for fc in range(2):
                S.op("pe", lambda e, n=n, fc=fc: e.matmul(out=PB[5 + n][:], lhsT=actsh[:, fc * 128:(fc + 1) * 128], rhs=wsd[:, fc, n * 512:(n + 1) * 512],
                                                          start=(fc == 0), stop=(fc == 1)), reads=[actsh, wsd], writes=[PB[5 + n]])
            S.op("dve", lambda e, n=n, xo=xo: e.tensor_tensor(out=xo[:, n * 512:(n + 1) * 512], in0=PB[5 + n][:], in1=g2row[:, n * 512:(n + 1) * 512], op=ALU.mult),
                 reads=[PB[5 + n], g2row], wshared=[xo])
        S.op("dve", lambda e, xo=xo, xm=xm: e.tensor_tensor(out=xo[:], in0=xo[:], in1=xm[:], op=ALU.add), reads=[xo, xm], writes=[xo])
        S.dma("sp", x2_d[i * 128:(i + 1) * 128, :], xo[:], reads=[xo], wshared=[x2_B])

    S.barrier("pool", lambda e: e.memset(ones_b[:, 0:1], 1.0))
    S.sb_ptr = P3_KEEP
    NL = 12
    wg = [S.sb(f"wg{j}", [128, 2048], BF16, buf=False) for j in range(NL)]
    wu = [S.sb(f"wu{j}", [128, 2048], BF16, buf=False) for j in range(NL)]
    wd = [S.sb(f"wd{j}", [128, 2048], BF16, buf=False) for j in range(NL)]
    wset = [Buf(S, f"wset{j}") for j in range(NL)]
    xsb = [S.sb(f"xsb{j}", [128, D], BF16) for j in range(3)]
    xsT = [S.sb(f"xsT{j}", [128, D], BF16) for j in range(2)]
    sgt = S.sb("sgt", [128, 256], F32)
    actT = [S.sb(f"actT{j}", [128, 256], BF16) for j in range(2)]
    ysb = [S.sb(f"ysb{j}", [128, D], BF16) for j in range(2)]
    order = [l * LB + m for m in range(LB) for l in range(NL) if l * LB + m < NBLK]

    def load_xs(n):
        b = order[n]
        S.dma("sp", xsb[n % 3][:], xs_d[b * 128:(b + 1) * 128, :], reads=[xs_B], writes=[xsb[n % 3]])

    def load_w(b):
        l = b // LB
        for (wt, src, extra) in ((wg[l], weg_d, []), (wu[l], weu_d, []), (wd[l], ns["wedb_d"], [ns["wedb_B"]])):
            S.dma_fn("pool", lambda e, wt=wt, src=src, b=b: e.indirect_dma_start(
                out=wt[:], out_offset=None, in_=src, in_offset=bass.IndirectOffsetOnAxis(ap=idxall[:, b:b + 1], axis=0),
                bounds_check=S.reg(e, E * 128 - 1), oob_is_err=False), reads=[idxall] + extra, wshared=[wset[l]])

    for l in range(NL):
        load_w(l * LB)
    load_xs(0); load_xs(1)
    for n, b in enumerate(order):
        l = b // LB
        if n + 2 < len(order):
            load_xs(n + 2)
        xb = xsb[n % 3]; xT = xsT[n % 2]; aT = actT[n % 2]; yb = ysb[n % 2]
        for k in range(8):
            S.op("pe", lambda e, k=k, xb=xb: e.transpose(out=PTb[:, k * 128:(k + 1) * 128], in_=xb[:, k * 128:(k + 1) * 128], identity=ident_b[:]),
                 reads=[xb, ident_b], writes=[PTb])
        if n % 2 == 0:
            S.op("act", lambda e, xT=xT: e.activation(out=xT[:], in_=PTb[:], func=AF.Copy), reads=[PTb], writes=[xT])
        else:
            S.op("dve", lambda e, xT=xT: e.tensor_copy(out=xT[:], in_=PTb[:]), reads=[PTb], writes=[xT])
        pg = PB[n % 2]
        for fc in range(2):
            for (wt, off) in ((wg[l], 0), (wu[l], 256)):
                for k in range(8):
                    S.op("pe", lambda e, fc=fc, wt=wt, off=off, k=k, pg=pg, xT=xT: e.matmul(
                        out=pg[:, off + fc * 128:off + (fc + 1) * 128], lhsT=wt[:, k * 256 + fc * 128:k * 256 + (fc + 1) * 128],
                        rhs=xT[:, k * 128:(k + 1) * 128], start=(k == 0), stop=(k == 7)), reads=[wset[l], xT], writes=[pg])
        S.op("act", lambda e, pg=pg: e.activation(out=sgt[:], in_=pg[:, 0:256], func=AF.Silu), reads=[pg], writes=[sgt])
        S.op("dve", lambda e, pg=pg, aT=aT: e.tensor_tensor(out=aT[:], in0=sgt[:], in1=pg[:, 256:512], op=ALU.mult), reads=[sgt, pg], writes=[aT])
        for nn in range(2):
            py = PB[2 + 2 * (n % 2) + nn]
            for fc in range(2):
                S.op("pe", lambda e, nn=nn, fc=fc, py=py, aT=aT, l=l: e.matmul(out=py[:], lhsT=aT[:, fc * 128:(fc + 1) * 128],
                                                                              rhs=wd[l][:, fc * 1024 + nn * 512:fc * 1024 + (nn + 1) * 512],
                                                                              start=(fc == 0), stop=(fc == 1)), reads=[aT, wset[l]], writes=[py])
            S.op("dve", lambda e, py=py, yb=yb, nn=nn: e.tensor_tensor(out=yb[:, nn * 512:(nn + 1) * 512], in0=py[:], in1=g2row[:, nn * 512:(nn + 1) * 512], op=ALU.mult),
                 reads=[py, g2row], wshared=[yb])
        S.dma("sp", ys_d[b * 128:(b + 1) * 128, :], yb[:], reads=[yb], wshared=[ys_B])
        if b + 1 < NBLK and (b + 1) % LB != 0:
            load_w(b + 1)

    S.barrier("pool", lambda e: e.memset(ones_b[:, 0:1], 1.0))
    S.sb_ptr = P3_KEEP
    gk = [S.sb(f"gk{j}", [128, 8, D], BF16) for j in range(2)]
    x2l = [S.sb(f"x2l{j}", [128, D], F32) for j in range(2)]
    acc = [S.sb(f"acc{j}", [128, D], F32) for j in range(2)]
    ot = [S.sb(f"ot{j}", [128, D], F32) for j in range(2)]
    ss4 = S.sb("ss4", [128, 1], F32); sd4 = S.sb("sd4", [128, 1], F32); rs4 = S.sb("rs4", [128, 1], F32)
    def load_c(i):
        gkt = gk[i % 2]; xl = x2l[i % 2]
        for k in range(8):
            S.dma_fn("pool", lambda e, i=i, k=k, gkt=gkt: e.indirect_dma_start(
                out=gkt[:, k, :], out_offset=None, in_=ys_d, in_offset=bass.IndirectOffsetOnAxis(ap=sloti[:, i, k:k + 1], axis=0),
                bounds_check=S.reg(e, NSLOT - 1), oob_is_err=False), reads=[ys_B, sloti], wshared=[gkt])
        S.dma("sp", xl[:], x2_d[i * 128:(i + 1) * 128, :], reads=[x2_B], writes=[xl])
    load_c(0)
    for i in range(NT):
        gkt = gk[i % 2]; xl = x2l[i % 2]; ac = acc[i % 2]; o_ = ot[i % 2]
        if i + 1 < NT:
            load_c(i + 1)
        S.op("dve", lambda e, i=i, gkt=gkt, ac=ac, xl=xl: e.scalar_tensor_tensor(out=ac[:], in0=gkt[:, 0, :], scalar=w8all[:, i, 0:1], in1=xl[:],
                                                                            op0=ALU.mult, op1=ALU.add), reads=[gkt, w8all, xl], writes=[ac])
        for k in range(1, 8):
            S.op("dve", lambda e, i=i, k=k, gkt=gkt, ac=ac: e.scalar_tensor_tensor(out=ac[:], in0=gkt[:, k, :], scalar=w8all[:, i, k:k + 1], in1=ac[:],
                                                                                   op0=ALU.mult, op1=ALU.add), reads=[gkt, w8all, ac], writes=[ac])
        S.op("act", lambda e, ac=ac, o_=o_: e.activation(out=o_[:], in_=ac[:], func=AF.Square, accum_out=ss4[:]), reads=[ac], writes=[o_, ss4])
        S.op("pool", lambda e: e.tensor_scalar(out=sd4[:], in0=ss4[:], scalar1=1.0 / D, scalar2=EPS, op0=ALU.mult, op1=ALU.add), reads=[ss4], writes=[sd4])
        S.op("pool", lambda e: e.tensor_tensor(out=rs4[:], in0=sd4[:], in1=neghalf[:], op=ALU.pow), reads=[sd4, neghalf], writes=[rs4])
        S.op("dve", lambda e, ac=ac, o_=o_: e.scalar_tensor_tensor(out=o_[:], in0=ac[:], scalar=rs4[:, 0:1], in1=fgrow[:], op0=ALU.mult, op1=ALU.mult),
             reads=[ac, rs4, fgrow], writes=[o_])
        S.dma("sp", out_d[i * 128:(i + 1) * 128, :], o_[:], reads=[o_], wshared=[out_B])

def _fm(v, nk):
    return np.ascontiguousarray(v.reshape(nk, 128).T)


def _kp(w):
    K = w.shape[0] // 128
    return np.ascontiguousarray(w.reshape(K, 128, w.shape[1]).transpose(1, 0, 2).reshape(128, K * w.shape[1]))


def _consts():
    c = {}
    c["ident"] = np.eye(128, dtype=np.float32)
    j = np.arange(128)[:, None]; i = np.arange(128)[None, :]
    NEG = np.float32(-240000.0)
    mprev = np.where(j >= i, np.float32(0.0), NEG).astype(np.float32); mnext = np.where(j <= i, np.float32(0.0), NEG).astype(np.float32)
    c["masks"] = np.concatenate([np.tile(mprev, (1, 4)), np.tile(mnext, (1, 4))], axis=1)
    c["ustrict"] = (j < i).astype(np.float32)
    L = T
    invc = np.zeros((4, 2, 8), np.float32)
    for g, w in enumerate((2, 4, 8, 16)):
        for side in range(2):
            for jj in range(8):
                t = jj if side == 0 else L - 8 + jj
                lo = min(max(t - w // 2, 0), L); hi = min(max(t + w // 2, 0), L)
                invc[g, side, jj] = 1.0 / float(hi - lo)
    c["invc"] = np.tile(invc.reshape(1, 64), (128, 1))
    c["iotae"] = np.tile(np.arange(256, dtype=np.float32)[None, :], (128, 1))
    c["iotap"] = np.arange(128, dtype=np.float32)[:, None].copy()
    c["iotab"] = np.tile((128.0 * np.arange(512, dtype=np.float32))[None, :], (128, 1))
    rows = T // 64
    row = np.repeat(np.arange(rows), 64).astype(np.float32)
    col = np.tile(np.arange(64), rows).astype(np.float32)
    nf = 16
    inv = (np.float32(10000.0) ** (-np.arange(nf, dtype=np.float32) / np.float32(nf))).astype(np.float32)
    ang = np.stack([row[:, None] * inv, col[:, None] * inv], axis=1)
    cs_, sn_ = np.cos(ang).astype(np.float32), np.sin(ang).astype(np.float32)
    C = np.zeros((64, T), np.float32); Sg = np.zeros((64, T), np.float32)
    for d in range(64):
        axis, ab, f = d // 32, (d % 32) // 16, d % 16
        C[d] = cs_[:, axis, f]
        Sg[d] = (-sn_[:, axis, f]) if ab == 0 else sn_[:, axis, f]
    c["ropec"] = np.concatenate([C, C], axis=0); c["ropes"] = np.concatenate([Sg, Sg], axis=0)
    return c


def _partner(cols):
    d = cols % 64
    ab = (d % 32) // 16
    return np.where(ab == 0, cols + 16, cols - 16)


def _prep_shared(inp, full=True):
    sh = dict(_consts())
    w_in = inp["w_in"][0]
    qcols = np.concatenate([np.concatenate([np.arange(c * 64, (c + 1) * 64), np.arange((4 + c) * 64, (5 + c) * 64)]) for c in range(4)])
    kcols = np.arange(512, 640)
    cols = np.concatenate([qcols, (_partner(qcols)), kcols, 512 + _partner(kcols - 512),
                           np.arange(768, 1280), np.arange(1280, 3328), np.arange(640, 768)])
    assert cols.shape[0] == NXC
    sh["wx"] = np.ascontiguousarray(w_in[:, cols])
    sh["wada"] = inp["w_ada"][0]; sh["bada"] = inp["b_ada"][0][None, :].copy(); sh["badafm"] = _fm(inp["b_ada"][0], 48)
    sh["n1fm"] = _fm(inp["norm1_g"][0], 8); sh["n2row"] = inp["norm2_g"][0][None, :].copy(); sh["fgrow"] = inp["final_g"][None, :].copy()
    sh["sink"] = inp["attn_sink"][0][None, :].copy()
    sh["wpool"] = np.ascontiguousarray(inp["w_pool"][0].transpose(1, 0, 2).reshape(128, 512))
    sh["pscale"] = _fm(inp["pool_scale"][0], 4)
    wua = inp["w_up_attn"][0]
    rows_ = np.concatenate([np.concatenate([np.arange(c * 64, (c + 1) * 64), np.arange((4 + c) * 64, (5 + c) * 64)]) for c in range(4)])
    sh["wua"] = _kp(wua[rows_]); sh["wup"] = _kp(inp["w_up_pool"][0]); sh["wout"] = _kp(inp["w_out"][0])
    if full:
        sh["wr"] = _kp(inp["w_router"][0]); sh["rbias"] = inp["router_bias"][0][None, :].copy()
        sh["wsg"] = _kp(inp["w_sh_gate"][0]); sh["wsu"] = _kp(inp["w_sh_up"][0]); sh["wsd"] = _kp(inp["w_sh_down"][0])
        def ek(w):
            Ee, R, N = w.shape
            K = R // 128
            return np.ascontiguousarray(w.reshape(Ee, K, 128, N).transpose(0, 2, 1, 3)).reshape(Ee * 128, K * N)
        sh["weg"] = ek(inp["w_exp_gate"][0]); sh["weu"] = ek(inp["w_exp_up"][0]); sh["wed"] = ek(inp["w_exp_down"][0])
    return sh


def _prep_core(inp, b):
    m = {}
    m["x"] = np.ascontiguousarray(inp["x"][b]); m["ctx"] = np.ascontiguousarray(inp["ctx"][b])
    cs = np.stack([_fm(inp["c"][b], 8), _fm(inp["c_ctx"], 8)], axis=2)
    m["csin"] = np.ascontiguousarray(cs.reshape(128, 16))
    return m


def kernel(**inputs):
    inp = {k: np.asarray(v, dtype=np.float32) for k, v in inputs.items()}
    nc = bass.Bass("TRN2", target_bir_lowering=False)
    build(nc, "full")
    sh = _prep_shared(inp, True)
    in_maps = []
    for b in range(8):
        m = dict(sh); m.update(_prep_core(inp, b)); in_maps.append(m)
    res = run_bass_kernel_spmd(nc, in_maps, core_ids=list(range(8)))
    return np.stack([res.results[b]["out"] for b in range(8)], axis=0).astype(np.float32)
```

```python
import numpy as np
import concourse.bass as bass
import concourse.mybir as mybir
from concourse.bass_utils import run_bass_kernel_spmd

ENGS = ("sp", "act", "dve", "pool", "pe")
SB_LO = 16512
SB_HI = 229376


class Ev:
    __slots__ = ("op", "shared")

    def __init__(self, op, shared=False):
        self.op = op
        self.shared = shared


class Buf:
    def __init__(self, S, name, t=None, is_dram=False):
        self.S = S
        self.name = name
        self.t = t
        self.is_dram = is_dram
        self.writers = {}
        self.readers = {}
        self.shared_keys = set()
        self.wsem = None
        self.rsem = None
        if S.epoch_op is not None:
            self.writers[S.epoch_op.semkey()] = S.epoch_op
        S.bufs.append(self)

    def __getitem__(self, k):
        return self.t[k]


class Op:
    __slots__ = ("eng", "fn", "deps", "is_dma", "sem", "val", "marked", "idx", "raw_same")

    def __init__(self, eng, fn, is_dma):
        self.eng = eng
        self.fn = fn
        self.deps = []
        self.is_dma = is_dma
        self.sem = None
        self.val = None
        self.marked = False

    def semkey(self):
        return ("dma", id(self.sem)) if self.is_dma else ("eng", self.eng)


class DmaSem:
    def __init__(self, S, name):
        self.h = S.nc.alloc_semaphore(name)
        self.count = 0


class Sched:
    def __init__(self, nc):
        self.nc = nc
        self.ops = {e: [] for e in ENGS}
        self.bufs = []
        self.epoch_op = None
        self.sb_ptr = SB_LO
        self.nsem = 0
        self.eng_sem = {}
        self.uid = 0
        self.psum_used = 0

    def sb(self, name, shape, dtype, buf=True):
        nbytes = 1
        for s in shape[1:]:
            nbytes *= s
        nbytes *= mybir.dt.size(dtype)
        nbytes = (nbytes + 31) // 32 * 32
        off = self.sb_ptr
        assert off + nbytes <= SB_HI, f"SBUF overflow allocating {name}: {off}+{nbytes}"
        self.sb_ptr += nbytes
        self.uid += 1
        t = self.nc.alloc_sbuf_tensor_at(f"{name}_{self.uid}", list(shape), dtype, offset=off)
        return Buf(self, name, t) if buf else t

    def ps(self, name, shape, dtype):
        self.uid += 1
        t = self.nc.alloc_psum_tensor(f"{name}_{self.uid}", list(shape), dtype)
        return Buf(self, name, t)

    def dram(self, name):
        return Buf(self, name, None, True)

    def reg(self, e, val):
        if not hasattr(self, "_regs"):
            self._regs = {}
        if val not in self._regs:
            self._regs[val] = e.to_reg(val)
        return self._regs[val]

    def newsem(self, name):
        self.nsem += 1
        return DmaSem(self, f"{name}_{self.nsem}")

    def _add(self, eng, fn, reads, writes, wshared, is_dma, sem):
        op = Op(eng, fn, is_dma)
        op.sem = sem
        deps = {}

        def add(d, kind):
            if d is op:
                return
            if (not d.is_dma) and (not is_dma) and d.eng == eng and kind != "raw":
                return
            if (not d.is_dma) and (not is_dma) and d.eng == eng == "pe":
                return
            k = d.semkey()
            cur = deps.get(k)
            if cur is None or d.idx > cur.idx:
                deps[k] = d

        for b in reads:
            for d in b.writers.values():
                add(d, "raw")
        for b in writes:
            for d in b.writers.values():
                add(d, "waw")
            for d in b.readers.values():
                add(d, "war")
        for b in wshared:
            for kk, d in b.writers.items():
                if kk not in b.shared_keys:
                    add(d, "waw")
            for d in b.readers.values():
                add(d, "war")
        op.deps = list(deps.values())
        op.idx = len(self.ops[eng])
        if is_dma:
            sem.count += 16
            op.val = sem.count
        if is_dma:
            op.idx = op.val
        self.ops[eng].append(op)
        k = op.semkey()
        for b in reads:
            b.readers[k] = op
        for b in writes:
            b.writers = {k: op}
            b.readers = {}
            b.shared_keys = set()
        for b in wshared:
            b.writers[k] = op
            b.shared_keys.add(k)
        return op

    def op(self, eng, fn, reads=(), writes=(), wshared=()):
        return self._add(eng, fn, list(reads), list(writes), list(wshared), False, None)

    def dma(self, eng, out_ap, in_ap, reads=(), writes=(), wshared=(), sem=None, cast=False, **kw):
        reads, writes, wshared = list(reads), list(writes), list(wshared)
        if sem is None:
            sem = self._pick_sem(reads, writes, wshared)

        def fn(e, out_ap=out_ap, in_ap=in_ap, kw=kw):
            return e.dma_start(out=out_ap, in_=in_ap, **kw)

        return self._add(eng, fn, reads, writes, wshared, True, sem)

    def _pick_sem(self, reads, writes, wshared):
        for b in writes + wshared:
            if not b.is_dram:
                if b.wsem is None:
                    b.wsem = self.newsem("w" + b.name)
                return b.wsem
        for b in reads:
            if not b.is_dram:
                if b.rsem is None:
                    b.rsem = self.newsem("r" + b.name)
                return b.rsem
        raise ValueError("dma needs explicit sem")

    def dma_fn(self, eng, fn, reads=(), writes=(), wshared=(), sem=None):
        reads, writes, wshared = list(reads), list(writes), list(wshared)
        if sem is None:
            sem = self._pick_sem(reads, writes, wshared)
        return self._add(eng, fn, reads, writes, wshared, True, sem)

    def barrier(self, eng="pool", fn=None):
        assert fn is not None
        op = self._add(eng, fn, [], list(self.bufs), [], False, None)
        self.epoch_op = op
        return op

    def finalize_and_emit(self):
        nc = self.nc
        for e in ENGS:
            for op in self.ops[e]:
                for d in op.deps:
                    d.marked = True
        for e in ENGS:
            c = 0
            for op in self.ops[e]:
                if not op.is_dma and op.marked:
                    c += 1
                    op.val = c
        for e in ENGS:
            self.eng_sem[e] = nc.alloc_semaphore(f"eng_{e}")
        engobj = {"sp": None, "act": None, "dve": None, "pool": None, "pe": None}
        S = self
        nwaits = {e: 0 for e in ENGS}

        def run(ename, eng):
            seen = {}
            mysems = {}
            for op in S.ops[ename]:
                for d in op.deps:
                    if d.is_dma:
                        key, h, v = ("d", id(d.sem)), d.sem.h, d.val
                    else:
                        key, h, v = ("e", d.eng), S.eng_sem[d.eng], d.val
                    if seen.get(key, 0) >= v:
                        continue
                    seen[key] = v
                    eng.wait_ge(h, v)
                    nwaits[ename] += 1
                ins = op.fn(eng)
                if op.is_dma:
                    ins.then_inc(op.sem.h, 16)
                    mysems[id(op.sem)] = op.sem
                elif op.marked:
                    ins.then_inc(S.eng_sem[ename], 1)
            for sm in mysems.values():
                if seen.get(("d", id(sm)), 0) < sm.count:
                    eng.wait_ge(sm.h, sm.count)

        with nc.Block() as block:
            @block.sync
            def _(e):
                run("sp", e)

            @block.scalar
            def _(e):
                run("act", e)

            @block.vector
            def _(e):
                run("dve", e)

            @block.gpsimd
            def _(e):
                run("pool", e)

            @block.tensor
            def _(e):
                run("pe", e)
        self.nwaits = nwaits

F32 = mybir.dt.float32
BF16 = mybir.dt.bfloat16
I32 = mybir.dt.int32
U32 = mybir.dt.uint32
AF = mybir.ActivationFunctionType
ALU = mybir.AluOpType
AX = mybir.AxisListType

T = 4096
D = 1024
NT = T // 128
CH = 256
NCH = T // CH
NXC = 3968
E = 256
NBLK = 511
NSLOT = NBLK * 128
EPS = 1e-6

QB, QR, KB, KR, UB, GA, GP, VB = 0, 4, 8, 9, 10, 14, 22, 30


def build(nc, stage="full"):
    S = Sched(nc)
    dt_in = lambda name, shape, dt=F32: nc.dram_tensor(name, list(shape), dt, kind="ExternalInput").ap()
    x_d = dt_in("x", [T, D]); ctx_d = dt_in("ctx", [256, D]); csin_d = dt_in("csin", [128, 16])
    wada_d = dt_in("wada", [D, 6144]); bada_d = dt_in("bada", [1, 6144]); badafm_d = dt_in("badafm", [128, 48])
    n1fm_d = dt_in("n1fm", [128, 8]); n2row_d = dt_in("n2row", [1, D]); fgrow_d = dt_in("fgrow", [1, D])
    wx_d = dt_in("wx", [D, NXC]); ropec_d = dt_in("ropec", [128, T]); ropes_d = dt_in("ropes", [128, T])
    sink_d = dt_in("sink", [1, 8]); wpool_d = dt_in("wpool", [128, 512]); pscale_d = dt_in("pscale", [128, 4])
    wua_d = dt_in("wua", [128, 4096]); wup_d = dt_in("wup", [128, 4096]); wout_d = dt_in("wout", [128, 8192])
    ident_d = dt_in("ident", [128, 128]); masks_d = dt_in("masks", [128, 1024]); ustrict_d = dt_in("ustrict", [128, 128])
    invc_d = dt_in("invc", [128, 64]); iotae_d = dt_in("iotae", [128, 256]); iotap_d = dt_in("iotap", [128, 1])
    iotab_d = dt_in("iotab", [128, 512])
    full = stage == "full"
    if full:
        wr_d = dt_in("wr", [128, 2048]); rbias_d = dt_in("rbias", [1, 256])
        wsg_d = dt_in("wsg", [128, 2048]); wsu_d = dt_in("wsu", [128, 2048]); wsd_d = dt_in("wsd", [128, 2048])
        weg_d = dt_in("weg", [E * 128, 2048]); weu_d = dt_in("weu", [E * 128, 2048]); wed_d = dt_in("wed", [E * 128, 2048])
    out_d = nc.dram_tensor("out", [T, D], F32, kind="ExternalOutput").ap()
    xmid_t = nc.dram_tensor("xmid", [T, D], F32, kind="Internal")
    xmid_d = xmid_t.ap()
    xmid_B = S.dram("xmid")
    out_B = S.dram("outd")

    def bcast(ap_row, n):
        return ap_row.partition_broadcast(128) if hasattr(ap_row, "partition_broadcast") else ap_row

    ident_f = S.sb("identf", [128, 128], F32); ident_b = S.sb("identb", [128, 128], BF16)
    masks = S.sb("masks", [128, 2, 512], BF16)
    ustrict = S.sb("ustrict", [128, 128], BF16); ones_b = S.sb("onesb", [128, 128], BF16)
    iotae = S.sb("iotae", [128, 256], F32); iotap = S.sb("iotap", [128, 1], F32)
    g1row = S.sb("g1row", [128, D], F32)
    rowsave_t = nc.dram_tensor("rowsave", [3, D], F32, kind="Internal")
    rowsave_d = rowsave_t.ap()
    rowsave_B = S.dram("rowsave")
    S.dma("sp", ident_f[:], ident_d, writes=[ident_f])
    S.dma("pool", ident_b[:], ident_d, writes=[ident_b])
    S.dma("pool", masks[:].rearrange("p a b -> p (a b)"), masks_d, writes=[masks])
    S.dma("pool", ustrict[:], ustrict_d, writes=[ustrict])
    S.dma("sp", iotae[:], iotae_d, writes=[iotae]); S.dma("sp", iotap[:], iotap_d, writes=[iotap])
    S.op("pool", lambda e: e.memset(ones_b[:], 1.0), writes=[ones_b])
    neghalf = S.sb("neghalf", [128, 1], F32)
    S.op("pool", lambda e: e.memset(neghalf[:], -0.5), writes=[neghalf])
    P2_BASE = S.sb_ptr

    PB = [S.ps(f"pb{i}", [128, 512], F32) for i in range(7)]
    PTb = S.ps("ptb", [128, 1024], BF16)

    cs_in = S.sb("csin", [128, 16], F32); cs = S.sb("cs", [128, 16], F32)
    badafm = S.sb("badafm", [128, 48], F32); n1fm = S.sb("n1fm", [128, 8], F32)
    modx = S.sb("modx", [128, 16], F32); modc = S.sb("modc", [128, 16], F32)
    a1x = S.sb("a1x", [128, 8], F32); a1c = S.sb("a1c", [128, 8], F32)
    P0_KEEP = S.sb_ptr
    WX = S.sb("WX", [128, 8, NXC], BF16)
    wxv = wx_d.rearrange("(k p) n -> p k n", p=128)
    for k in range(8):
        for hh in range(2):
            S.dma("pool", WX[:, k, hh * 1984:(hh + 1) * 1984], wxv[:, k, hh * 1984:(hh + 1) * 1984], wshared=[WX])
    wua = S.sb("wua", [128, 4, D], BF16); wup = S.sb("wup", [128, 4, D], BF16); wout = S.sb("wout", [128, 8, D], BF16)
    wpool = S.sb("wpool", [128, 4, 128], BF16); pscale = S.sb("pscale", [128, 4], F32)
    invc = S.sb("invc", [128, 4, 2, 8], F32)
    for g in range(4):
        S.dma("pool", wua[:, g, :], wua_d[:, g * 1024:(g + 1) * 1024], wshared=[wua])
        S.dma("pool", wup[:, g, :], wup_d[:, g * 1024:(g + 1) * 1024], wshared=[wup])
    for g in range(8):
        S.dma("pool", wout[:, g, :], wout_d[:, g * 1024:(g + 1) * 1024], wshared=[wout])
    S.dma("pool", wpool[:].rearrange("p a b -> p (a b)"), wpool_d, writes=[wpool])
    S.dma("sp", pscale[:], pscale_d, writes=[pscale])
    S.dma("sp", invc[:].rearrange("p a b c -> p (a b c)"), invc_d, writes=[invc])
    sink_sb = S.sb("sink", [1, 8], F32); esink1 = S.sb("esink1", [1, 8], F32); esink = S.sb("esink", [1, 2, 512], BF16)
    sinksel = S.sb("sinksel", [1, 2, 128], BF16)
    S.dma("sp", sink_sb[:], sink_d, writes=[sink_sb])
    S.op("act", lambda e: e.activation(out=esink1[:], in_=sink_sb[:], func=AF.Exp), reads=[sink_sb], writes=[esink1])
    for h in range(8):
        S.op("dve", lambda e, h=h: e.tensor_copy(out=esink[0:1, h // 4, (h % 4) * 128:(h % 4 + 1) * 128],
                                                 in_=esink1[0:1, h:h + 1].to_broadcast([1, 128])),
             reads=[esink1], writes=[esink])
    S.op("pool", lambda e: e.memset(sinksel[:], 0.0), writes=[sinksel])
    S.op("pool", lambda e: e.memset(sinksel[0:1, 0, 64:128], 1.0), writes=[sinksel])
    S.op("pool", lambda e: e.memset(sinksel[0:1, 1, 0:64], 1.0), writes=[sinksel])

    P2_ACT = S.sb_ptr
    csrep = S.sb("csrep", [128, 8, 128], F32)
    wadab = [S.sb(f"wada{i}", [128, 8, 512], F32) for i in range(2)]
    brow = [S.sb(f"brow{i}", [128, D], F32) for i in range(2)]
    n2row = S.sb("n2row", [128, D], F32)
    sh2row = S.sb("sh2row", [128, D], F32); a2row = S.sb("a2row", [128, D], F32); g2row = S.sb("g2row", [128, D], F32)
    if full:
        xs_t = nc.dram_tensor("xs", [NSLOT, D], BF16, kind="Internal"); xs_d = xs_t.ap(); xs_B = S.dram("xs")
    S.dma("sp", cs_in[:], csin_d, writes=[cs_in])
    S.dma("sp", badafm[:], badafm_d, writes=[badafm]); S.dma("sp", n1fm[:], n1fm_d, writes=[n1fm])
    S.dma("act", n2row[:], n2row_d.partition_broadcast(128), writes=[n2row])
    S.op("act", lambda e: e.activation(out=cs[:], in_=cs_in[:], func=AF.Silu), reads=[cs_in], writes=[cs])
    csv = cs[:].rearrange("p (k t) -> p k t", t=2)
    for k in range(8):
        S.op("dve", lambda e, k=k: e.tensor_copy(out=csrep[:, k, :], in_=cs[:, 2 * k:2 * k + 1].to_broadcast([128, 128])),
             reads=[cs], writes=[csrep])
    wview = wada_d.rearrange("(k p) n -> p k n", p=128)
    rows_dst = {2: g1row, 3: sh2row, 4: a2row, 5: g2row}
    for m2 in range(12):
        m, hf = m2 // 2, m2 % 2
        wb = wadab[m2 % 2]
        for k in range(8):
            S.dma("sp" if k % 2 == 0 else "act", wb[:, k, :], wview[:, k, m2 * 512:(m2 + 1) * 512], wshared=[wb])
        if m < 2:
            pa = PB[m2 % 2]
            for jj in range(4):
                for k in range(8):
                    S.op("pe", lambda e, pa=pa, wb=wb, jj=jj, k=k: e.matmul(
                        out=pa[:, 2 * jj:2 * jj + 2], lhsT=wb[:, k, jj * 128:(jj + 1) * 128],
                        rhs=cs[:, 2 * k:2 * k + 2], start=(k == 0), stop=(k == 7)),
                        reads=[wb, cs], writes=[pa])
            j0_ = m * 8 + hf * 4
            S.op("dve", lambda e, pa=pa, j0_=j0_: e.tensor_tensor(out=modx[:, j0_:j0_ + 4], in0=pa[:, 0:8].rearrange("p (j t) -> p j t", t=2)[:, :, 0],
                                                                  in1=badafm[:, j0_:j0_ + 4], op=ALU.add),
                 reads=[pa, badafm], wshared=[modx])
            S.op("dve", lambda e, pa=pa, j0_=j0_: e.tensor_tensor(out=modc[:, j0_:j0_ + 4], in0=pa[:, 0:8].rearrange("p (j t) -> p j t", t=2)[:, :, 1],
                                                                  in1=badafm[:, j0_:j0_ + 4], op=ALU.add),
                 reads=[pa, badafm], wshared=[modc])
        else:
            br = brow[m % 2]
            if hf == 0:
                S.dma("act", br[:], bada_d[:, m * 1024:(m + 1) * 1024].partition_broadcast(128), writes=[br])
            dst = rows_dst[m]
            pa = PB[2 + hf]
            for k in range(8):
                S.op("pe", lambda e, pa=pa, wb=wb, k=k: e.matmul(
                    out=pa[:], lhsT=csrep[:, k, :], rhs=wb[:, k, :],
                    start=(k == 0), stop=(k == 7)), reads=[wb, csrep], writes=[pa])
            S.op("dve", lambda e, pa=pa, hf=hf, dst=dst, br=br: e.tensor_tensor(
                out=dst[:, hf * 512:(hf + 1) * 512], in0=pa[:], in1=br[:, hf * 512:(hf + 1) * 512], op=ALU.add),
                reads=[pa, br], wshared=[dst])
            if m == 4 and hf == 1:
                S.op("dve", lambda e: e.scalar_tensor_tensor(out=a2row[:], in0=a2row[:], scalar=1.0, in1=n2row[:],
                                                             op0=ALU.add, op1=ALU.mult),
                     reads=[a2row, n2row], writes=[a2row])
    S.op("dve", lambda e: e.scalar_tensor_tensor(out=a1x[:], in0=modx[:, 8:16], scalar=1.0, in1=n1fm[:],
                                                 op0=ALU.add, op1=ALU.mult), reads=[modx, n1fm], writes=[a1x])
    S.op("dve", lambda e: e.scalar_tensor_tensor(out=a1c[:], in0=modc[:, 8:16], scalar=1.0, in1=n1fm[:],
                                                 op0=ALU.add, op1=ALU.mult), reads=[modc, n1fm], writes=[a1c])

    if stage == "P0":
        S.dma("sp", out_d[0:128, :], g1row[:], reads=[g1row], wshared=[out_B])
        S.dma("sp", out_d[128:256, :], a2row[:], reads=[a2row], wshared=[out_B])
        S.dma("sp", out_d[256:384, 0:16], modx[:], reads=[modx], wshared=[out_B])
        S.dma("sp", out_d[256:384, 16:32], modc[:], reads=[modc], wshared=[out_B])
        S.dma("sp", out_d[256:384, 32:40], a1x[:], reads=[a1x], wshared=[out_B])
        S.finalize_and_emit()
        return nc
    for ri, rr in enumerate((sh2row, a2row, g2row)):
        S.dma("sp", rowsave_d[ri:ri + 1, :], rr[0:1, :], reads=[rr], wshared=[rowsave_B])
    S.barrier("pool", lambda e: e.memset(ones_b[:, 0:1], 1.0))
    S.sb_ptr = P2_ACT

    import os
    if os.environ.get("DBG_STOP", "") == "p0end":
        S.finalize_and_emit()
        return nc
    xt = [S.sb(f"xt{i}", [128, D], F32) for i in range(2)]
    xnb = [S.sb(f"xnb{i}", [128, D], BF16) for i in range(2)]
    ssq = [S.sb(f"ssq{i}", [128, 1], F32) for i in range(2)]
    sdv = [S.sb(f"sdv{i}", [128, 1], F32) for i in range(2)]
    rstd = [S.sb(f"rstd{i}", [128, 1], F32) for i in range(2)]
    hT = [S.sb(f"hT{i}", [128, 8, CH], BF16) for i in range(2)]
    hcT = S.sb("hcT", [128, 8, 256], BF16)
    kcT = S.sb("kcT", [128, 256], BF16)
    vctx = S.sb("vctx", [128, 2, 2, 128], BF16)
    kring = [S.sb(f"kring{i}", [128, CH], BF16) for i in range(3)]
    vring = [S.sb(f"vring{i}", [128, 2, 2, 128], BF16) for i in range(3)]
    uring = [S.sb(f"uring{i}", [128, 4, CH + 16], F32) for i in range(2)]
    qT = S.sb("qT", [128, 4, CH], BF16)
    sg = S.sb("sg", [128, 16, CH], BF16)
    ropeC = [S.sb(f"ropeC{i}", [128, CH], F32) for i in range(2)]
    ropeS = [S.sb(f"ropeS{i}", [128, CH], F32) for i in range(2)]
    rt1 = S.sb("rt1", [128, CH], F32); rt2 = S.sb("rt2", [128, CH], F32)
    pT = [S.sb(f"pT{i}", [128, 512], BF16) for i in range(5)]
    recA = [S.sb(f"recA{i}", [128, 512], F32) for i in range(2)]; recB = [S.sb(f"recB{i}", [128, 512], F32) for i in range(2)]
    attnT = S.sb("attnT", [128, 4, CH], BF16)
    ps2 = S.sb("ps2", [128, CH + 16], F32); ps4 = S.sb("ps4", [128, CH + 16], F32); ps8 = S.sb("ps8", [128, CH + 16], F32)
    ps16 = S.sb("ps16", [128, CH + 16], F32)
    dT = S.sb("dT", [128, 4, CH], BF16); poolT = S.sb("poolT", [128, 4, CH], BF16)
    etmp = S.sb("etmp", [128, 8], F32)
    yT = S.sb("yT", [128, 8, CH], BF16); mt1 = S.sb("mt1", [128, CH], F32); mt2 = S.sb("mt2", [128, CH], F32)
    xr = [S.sb(f"xr{i}", [128, D], F32) for i in range(1)] * 2
    xmo = [S.sb(f"xmo{i}", [128, D], F32) for i in range(2)]
    for t in vring + [vctx]:
        S.op("pool", lambda e, t=t: e.memset(t[:], 1.0), writes=[t])
    for u in uring:
        S.op("pool", lambda e, u=u: e.memset(u[:], 0.0), writes=[u])

    if full:
        wedb_t = nc.dram_tensor("wedb", [E * 128, 2048], BF16, kind="Internal"); wedb_d = wedb_t.ap(); wedb_B = S.dram("wedb")
        conv_sem = S.newsem("convwd")

    def conv_wd(c):
        if not full:
            return
        per = E // NCH
        for ex in range(c * per, (c + 1) * per):
            S.dma("pool", wedb_d[ex * 128:(ex + 1) * 128, :], wed_d[ex * 128:(ex + 1) * 128, :], wshared=[wedb_B], sem=conv_sem)

    tile_ctr = [0]
    import os
    if os.environ.get("DBG_STOP", "") == "setup":
        S.finalize_and_emit()
        return nc

    def make_hT(src_rows_ap, avec, shvec, dst, dst_off):
        i = tile_ctr[0] % 2
        tile_ctr[0] += 1
        x_, xn_, ss_, sd_, rs_ = xt[i], xnb[i], ssq[i], sdv[i], rstd[i]
        S.dma("sp", x_[:], src_rows_ap, writes=[x_])
        S.op("act", lambda e: e.activation(out=xn_[:], in_=x_[:], func=AF.Square, accum_out=ss_[:]),
             reads=[x_], writes=[xn_, ss_])
        S.op("pool", lambda e: e.tensor_scalar(out=sd_[:], in0=ss_[:], scalar1=1.0 / D, scalar2=EPS, op0=ALU.mult, op1=ALU.add),
             reads=[ss_], writes=[sd_])
        S.op("pool", lambda e: e.tensor_tensor(out=rs_[:], in0=sd_[:], in1=neghalf[:], op=ALU.pow), reads=[sd_, neghalf], writes=[rs_])
        S.op("dve", lambda e: e.tensor_scalar(out=xn_[:], in0=x_[:], scalar1=rs_[:], scalar2=None, op0=ALU.mult),
             reads=[x_, rs_], writes=[xn_])
        if os.environ.get("DBG_STOP", "") == "h1":
            return
        for k in range(8):
            S.op("pe", lambda e, k=k: e.transpose(out=PTb[:, k * 128:(k + 1) * 128], in_=xn_[:, k * 128:(k + 1) * 128],
                                                  identity=ident_b[:]), reads=[xn_, ident_b], writes=[PTb])
        if os.environ.get("DBG_STOP", "") == "h2":
            return
        EV = os.environ.get("DBG_EVAC", "")
        for k in range(8):
            if i == 0:
                S.op("act", lambda e, k=k: e.activation(out=dst[:, k, dst_off:dst_off + 128], in_=PTb[:, k * 128:(k + 1) * 128],
                                                        func=AF.Identity, scale=avec[:, k:k + 1], bias=shvec[:, k:k + 1]),
                     reads=[PTb, avec, shvec], wshared=[dst])
            else:
                S.op("dve", lambda e, k=k: e.tensor_scalar(out=dst[:, k, dst_off:dst_off + 128], in0=PTb[:, k * 128:(k + 1) * 128],
                                                           scalar1=avec[:, k:k + 1], scalar2=shvec[:, k:k + 1],
                                                           op0=ALU.mult, op1=ALU.add),
                     reads=[PTb, avec, shvec], wshared=[dst])

    pb_rr = [0]

    def next_pb():
        p = PB[pb_rr[0] % 2]
        pb_rr[0] += 1
        return p

    def proj_fm(hsrc, blk, ncols):
        p = next_pb()
        for k in range(8):
            S.op("pe", lambda e, k=k, p=p: e.matmul(out=p[:, 0:ncols], lhsT=WX[:, k, blk * 128:(blk + 1) * 128],
                                                    rhs=hsrc[:, k, 0:ncols], start=(k == 0), stop=(k == 7)),
                 reads=[WX, hsrc], writes=[p])
        return p

    for t in range(2):
        make_hT(ctx_d[t * 128:(t + 1) * 128, :], a1c, modc, hcT, t * 128)
    if os.environ.get("DBG_STOP", "") in ("h1", "h2", "h3"):
        S.finalize_and_emit()
        return nc
    p = proj_fm(hcT, KB, 256)
    S.op("act", lambda e, p=p: e.activation(out=kcT[:], in_=p[:, 0:256], func=AF.Copy), reads=[p], writes=[kcT])

    def proj_v(hsrc, tok_off, dst4, blk):
        p = next_pb()
        for k in range(8):
            S.op("pe", lambda e, k=k, p=p: e.matmul(out=p[:, 0:128], lhsT=hsrc[:, k, tok_off:tok_off + 128],
                                                    rhs=WX[:, k, VB * 128:(VB + 1) * 128], start=(k == 0), stop=(k == 7)),
                 reads=[WX, hsrc], writes=[p])
        S.op("act", lambda e, p=p: e.activation(out=dst4[:, blk, 0, 0:64], in_=p[:, 0:64], func=AF.Copy),
             reads=[p], wshared=[dst4])
        S.op("act", lambda e, p=p: e.activation(out=dst4[:, blk, 1, 64:128], in_=p[:, 64:128], func=AF.Copy),
             reads=[p], wshared=[dst4])

    for t in range(2):
        proj_v(hcT, t * 128, vctx, t)

    def stage_H(c):
        for t in range(2):
            make_hT(x_d[c * CH + t * 128:c * CH + (t + 1) * 128, :], a1x, modx, hT[c % 2], t * 128)

    def load_rope(c):
        S.dma("sp", ropeC[c % 2][:], ropec_d[:, c * CH:(c + 1) * CH], writes=[ropeC[c % 2]])
        S.dma("sp", ropeS[c % 2][:], ropes_d[:, c * CH:(c + 1) * CH], writes=[ropeS[c % 2]])

    def rope_evac(pa, pr, c, dst_ap, dstB):
        rc, rs_ = ropeC[c % 2], ropeS[c % 2]
        S.op("dve", lambda e: e.tensor_tensor(out=rt1[:], in0=pa[:, 0:CH], in1=rc[:], op=ALU.mult),
             reads=[pa, rc], writes=[rt1])
        S.op("dve", lambda e: e.tensor_tensor(out=rt2[:], in0=pr[:, 0:CH], in1=rs_[:], op=ALU.mult),
             reads=[pr, rs_], writes=[rt2])
        S.op("pool", lambda e: e.tensor_tensor(out=dst_ap, in0=rt1[:], in1=rt2[:], op=ALU.add),
             reads=[rt1, rt2], wshared=[dstB])

    def kvu_units(c):
        h = hT[c % 2]
        u = uring[c % 2]
        units = []

        def unit_k():
            pa = proj_fm(h, KB, CH); pr = proj_fm(h, KR, CH)
            rope_evac(pa, pr, c, kring[c % 3][:], kring[c % 3])
        units.append(unit_k)
        for t in range(2):
            units.append(lambda t=t: proj_v(h, t * 128, vring[c % 3], t))
        for g in range(4):
            def unit_u(g=g):
                p = proj_fm(h, UB + g, CH)
                S.op("act", lambda e, p=p, g=g: e.activation(out=u[:, g, 8:8 + CH], in_=p[:, 0:CH], func=AF.Copy),
                     reads=[p], wshared=[u])
            units.append(unit_u)

        def unit_halo():
            if c > 0:
                up = uring[(c - 1) % 2]
                S.op("pool", lambda e: e.tensor_copy(out=up[:, :, 8 + CH:16 + CH], in_=u[:, :, 8:16]), reads=[u], wshared=[up])
                S.op("pool", lambda e: e.tensor_copy(out=u[:, :, 0:8], in_=up[:, :, CH:8 + CH]), reads=[up], wshared=[u])
            else:
                S.op("pool", lambda e: e.memset(u[:, :, 0:8], 0.0), wshared=[u])
            if c == NCH - 1:
                S.op("pool", lambda e: e.memset(u[:, :, 8 + CH:16 + CH], 0.0), wshared=[u])
        units.append(unit_halo)
        return units

    def stage_KVU(c):
        for f in kvu_units(c):
            f()

    def stage_QG(c):
        h = hT[c % 2]
        for q in range(4):
            pa = proj_fm(h, QB + q, CH); pr = proj_fm(h, QR + q, CH)
            rope_evac(pa, pr, c, qT[:, q, :], qT)
        for j in range(16):
            p = proj_fm(h, GA + j, CH)
            S.op("act", lambda e, p=p, j=j: e.activation(out=sg[:, j, :], in_=p[:, 0:CH], func=AF.Sigmoid),
                 reads=[p], wshared=[sg])

    pt_rr = [0]
    PSC = [PB[2], PB[3], PB[6], PB[0]]

    def attention(c):
        steps = []
        groups = []
        for i in range(2):
            n = 2 * c + i
            for g in range(2):
                gs = slice(g * 64, (g + 1) * 64)
                keys = []
                if n > 0:
                    cc, bb = (n - 1) // 2, (n - 1) % 2
                    keys.append((kring[cc % 3], kring[cc % 3][gs, bb * 128:(bb + 1) * 128], vring[cc % 3], vring[cc % 3][:, bb, g, :], 0))
                keys.append((kring[c % 3], kring[c % 3][gs, i * 128:(i + 1) * 128], vring[c % 3], vring[c % 3][:, i, g, :], None))
                if n < NT - 1:
                    cc, bb = (n + 1) // 2, (n + 1) % 2
                    keys.append((kring[cc % 3], kring[cc % 3][gs, bb * 128:(bb + 1) * 128], vring[cc % 3], vring[cc % 3][:, bb, g, :], 1))
                for t in range(2):
                    keys.append((kcT, kcT[gs, t * 128:(t + 1) * 128], vctx, vctx[:, t, g, :], None))
                gi = len(groups)
                groups.append(dict(i=i, g=g, po=PB[4 + gi % 2], nk=len(keys), qap=qT[gs, :, i * 128:(i + 1) * 128],
                                   rA=recA[gi % 2], rB=recB[gi % 2]))
                for ki, key in enumerate(keys):
                    steps.append((gi, ki, key))
        bufs = {}

        def emit_qk_exp(s):
            gi, ki, (kB, kap, vB, vap, mk) = steps[s]
            psc = PSC[pt_rr[0] % len(PSC)]
            pt = pT[pt_rr[0] % len(pT)]
            pt_rr[0] += 1
            bufs[s] = pt
            qap = groups[gi]["qap"]
            S.op("pe", lambda e, psc=psc, kap=kap, qap=qap, mk=mk: e.matmul(out=psc[:].rearrange("p (a b) -> p a b", a=4), lhsT=kap, rhs=qap,
                                                                            start=True, stop=(mk is None)), reads=[kB, qT], writes=[psc])
            if mk is not None:
                S.op("pe", lambda e, psc=psc, mk=mk: e.matmul(out=psc[:], lhsT=ident_b[:], rhs=masks[:, mk, :], start=False, stop=True),
                     reads=[ident_b, masks], writes=[psc])
            S.op("act", lambda e, psc=psc, pt=pt: e.activation(out=pt[:], in_=psc[:], func=AF.Exp, scale=0.125),
                 reads=[psc], writes=[pt])

        def emit_pv(s):
            gi, ki, (kB, kap, vB, vap, mk) = steps[s]
            G_ = groups[gi]; po = G_["po"]; pt = bufs[s]; g = G_["g"]
            S.op("pe", lambda e, po=po, vap=vap, pt=pt, ki=ki: e.matmul(out=po[:], lhsT=vap, rhs=pt[:], start=(ki == 0), stop=False),
                 reads=[vB, pt], writes=[po])
            if ki == G_["nk"] - 1:
                S.op("pe", lambda e, po=po, g=g: e.matmul(out=po[:], lhsT=sinksel[0:1, g, :], rhs=esink[0:1, g, :], start=False, stop=True),
                     reads=[sinksel, esink], writes=[po])

        def emit_norm_a(gi):
            G_ = groups[gi]; po = G_["po"]; rA = G_["rA"]
            ds = slice(64, 128) if G_["g"] == 0 else slice(0, 64)
            S.op("dve", lambda e, po=po, ds=ds, rA=rA: e.reciprocal(out=rA[ds, :], in_=po[ds, :]), reads=[po], writes=[rA])

        def emit_norm_b(gi):
            G_ = groups[gi]; po = G_["po"]; rA = G_["rA"]; rB = G_["rB"]; i = G_["i"]
            ns = slice(0, 64) if G_["g"] == 0 else slice(64, 128)
            ds = slice(64, 128) if G_["g"] == 0 else slice(0, 64)
            S.op("act", lambda e, ns=ns, ds=ds, rA=rA, rB=rB: e.activation(out=rB[ns, :], in_=rA[ds, :], func=AF.Copy),
                 reads=[rA], writes=[rB])
            S.op("dve", lambda e, po=po, ns=ns, i=i, rB=rB: e.tensor_tensor(
                out=attnT[ns, :, i * 128:(i + 1) * 128], in0=po[ns, :].rearrange("p (a b) -> p a b", a=4),
                in1=rB[ns, :].rearrange("p (a b) -> p a b", a=4), op=ALU.mult), reads=[po, rB], wshared=[attnT])

        nsteps = len(steps)
        pend = []
        LA = 3
        for s0 in range(min(LA, nsteps)):
            emit_qk_exp(s0)
        for s in range(nsteps):
            if s + LA < nsteps:
                emit_qk_exp(s + LA)
            emit_pv(s)
            gi, ki, _ = steps[s]
            if ki == groups[gi]["nk"] - 1:
                emit_norm_a(gi)
                pend.append((s + 3, gi))
            while pend and pend[0][0] <= s:
                emit_norm_b(pend.pop(0)[1])
        for _, gi in pend:
            emit_norm_b(gi)

    def poolmix_a(c):
        u = uring[c % 2]
        W = CH + 16
        for g in range(4):
            ug = u[:, g, :]
            S.op("pool", lambda e, ug=ug: e.tensor_tensor(out=ps2[:, 1:W], in0=ug[:, 0:W - 1], in1=ug[:, 1:W], op=ALU.add),
                 reads=[u], writes=[ps2])
            src = ps2
            if g >= 1:
                S.op("pool", lambda e: e.tensor_tensor(out=ps4[:, 2:W - 1], in0=ps2[:, 1:W - 2], in1=ps2[:, 3:W], op=ALU.add),
                     reads=[ps2], writes=[ps4])
                src = ps4
            if g >= 2:
                S.op("pool", lambda e: e.tensor_tensor(out=ps8[:, 4:W - 3], in0=ps4[:, 2:W - 5], in1=ps4[:, 6:W - 1], op=ALU.add),
                     reads=[ps4], writes=[ps8])
                src = ps8
            if g >= 3:
                S.op("pool", lambda e: e.tensor_tensor(out=ps16[:, 8:W - 7], in0=ps8[:, 4:W - 11], in1=ps8[:, 12:W - 3], op=ALU.add),
                     reads=[ps8], writes=[ps16])
                src = ps16
            w = 2 ** (g + 1)
            S.op("dve", lambda e, src=src, g=g, w=w, ug=ug: e.scalar_tensor_tensor(
                out=dT[:, g, :], in0=src[:, 8:8 + CH], scalar=1.0 / w, in1=ug[:, 8:8 + CH], op0=ALU.mult, op1=ALU.subtract),
                reads=[src, u], wshared=[dT])
            for (cond, side, col) in ((c == 0, 0, 0), (c == NCH - 1, 1, CH - 8)):
                if cond:
                    S.op("dve", lambda e, src=src, g=g, side=side, col=col: e.tensor_tensor(
                        out=etmp[:], in0=src[:, 8 + col:16 + col], in1=invc[:, g, side, :], op=ALU.mult),
                        reads=[src, invc], writes=[etmp])
                    S.op("dve", lambda e, g=g, col=col, ug=ug: e.tensor_tensor(
                        out=dT[:, g, col:col + 8], in0=etmp[:], in1=ug[:, 8 + col:16 + col], op=ALU.subtract),
                        reads=[etmp, u, dT], wshared=[dT])

    def poolmix_b(c):
        for g in range(4):
            p = next_pb()
            S.op("pe", lambda e, p=p, g=g: e.matmul(out=p[:, 0:CH], lhsT=wpool[:, g, :], rhs=dT[:, g, :], start=True, stop=True),
                 reads=[wpool, dT], writes=[p])
            S.op("act", lambda e, p=p, g=g: e.activation(out=poolT[:, g, :], in_=p[:, 0:CH], func=AF.Identity, scale=pscale[:, g:g + 1]),
                 reads=[p, pscale], wshared=[poolT])

    def merge(c, extra_units=()):
        for oc in range(8):
            pa = PB[2 + oc % 2]; pb_ = (PB[6], PB[0], PB[1])[oc % 3]
            for q in range(4):
                S.op("pe", lambda e, q=q, oc=oc, pa=pa: e.matmul(out=pa[:, 0:CH], lhsT=wua[:, q, oc * 128:(oc + 1) * 128], rhs=attnT[:, q, :],
                                                          start=(q == 0), stop=(q == 3)), reads=[wua, attnT], writes=[pa])
            for g in range(4):
                S.op("pe", lambda e, g=g, oc=oc, pb_=pb_: e.matmul(out=pb_[:, 0:CH], lhsT=wup[:, g, oc * 128:(oc + 1) * 128], rhs=poolT[:, g, :],
                                                                   start=(g == 0), stop=(g == 3)), reads=[wup, poolT], writes=[pb_])
            ma, mb = (mt1, mt2) if oc % 2 == 0 else (rt1, rt2)
            S.op("dve", lambda e, oc=oc, pa=pa, ma=ma: e.tensor_tensor(out=ma[:], in0=pa[:, 0:CH], in1=sg[:, oc, :], op=ALU.mult),
                 reads=[pa, sg], writes=[ma])
            S.op("dve", lambda e, oc=oc, pb_=pb_, mb=mb: e.tensor_tensor(out=mb[:], in0=pb_[:, 0:CH], in1=sg[:, 8 + oc, :], op=ALU.mult),
                 reads=[pb_, sg], writes=[mb])
            S.op("pool", lambda e, oc=oc, ma=ma, mb=mb: e.tensor_tensor(out=yT[:, oc, :], in0=ma[:], in1=mb[:], op=ALU.add),
                 reads=[ma, mb], wshared=[yT])
        mix_units = []
        for t in range(2):
            tok0 = c * CH + t * 128
            xr_ = xr[t]; xo_ = xmo[t]
            for n in range(2):
                def unit_mm(t=t, n=n, xr_=xr_, xo_=xo_, tok0=tok0):
                    if n == 0:
                        S.dma("sp", xr_[:], x_d[tok0:tok0 + 128, :], writes=[xr_])
                    pm = PB[4 + n]
                    for oc in range(8):
                        S.op("pe", lambda e, pm=pm, oc=oc, t=t, n=n: e.matmul(out=pm[:], lhsT=yT[:, oc, t * 128:(t + 1) * 128],
                                                                              rhs=wout[:, oc, n * 512:(n + 1) * 512], start=(oc == 0), stop=(oc == 7)),
                             reads=[yT, wout], writes=[pm])
                    S.op("dve", lambda e, pm=pm, n=n, xo_=xo_: e.tensor_tensor(out=xo_[:, n * 512:(n + 1) * 512], in0=pm[:],
                                                                              in1=g1row[:, n * 512:(n + 1) * 512], op=ALU.mult),
                         reads=[pm, g1row], wshared=[xo_])
                mix_units.append(unit_mm)

            def unit_st(xr_=xr_, xo_=xo_, tok0=tok0):
                S.op("dve", lambda e, xo_=xo_, xr_=xr_: e.tensor_tensor(out=xo_[:], in0=xo_[:], in1=xr_[:], op=ALU.add),
                     reads=[xo_, xr_], writes=[xo_])
                dstd = out_d if stage == "A" else xmid_d
                S.dma("sp", dstd[tok0:tok0 + 128, :], xo_[:], reads=[xo_], wshared=[out_B if stage == "A" else xmid_B])
            mix_units.append(unit_st)
        extra = list(extra_units)
        while mix_units or extra:
            if mix_units:
                mix_units.pop(0)()
            if extra:
                extra.pop(0)()
            if extra and len(extra) > len(mix_units):
                extra.pop(0)()

    stage_H(0); load_rope(0); stage_KVU(0)
    if NCH > 1:
        stage_H(1); load_rope(1); stage_KVU(1)
    for c in range(NCH):
        stage_QG(c)
        poolmix_a(c)
        conv_wd(c)
        attention(c)
        poolmix_b(c)
        extra = ()
        if c + 2 < NCH:
            stage_H(c + 2); load_rope(c + 2)
            extra = kvu_units(c + 2)
        merge(c, extra)

    if stage == "A":
        S.finalize_and_emit()
        return nc
    PHASE3(S, locals())
    S.finalize_and_emit()
    return nc


def PHASE3(S, ns):
    nc = S.nc
    LB = 64
    PB = ns["PB"]; PTb = ns["PTb"]
    ident_f = ns["ident_f"]; ident_b = ns["ident_b"]; ustrict = ns["ustrict"]; ones_b = ns["ones_b"]
    iotae = ns["iotae"]; iotap = ns["iotap"]
    rowsave_d = ns["rowsave_d"]; rowsave_B = ns["rowsave_B"]; fgrow_d = ns["fgrow_d"]
    wr_d = ns["wr_d"]; rbias_d = ns["rbias_d"]; wsg_d = ns["wsg_d"]; wsu_d = ns["wsu_d"]; wsd_d = ns["wsd_d"]
    weg_d = ns["weg_d"]; weu_d = ns["weu_d"]; wed_d = ns["wed_d"]
    xmid_d = ns["xmid_d"]; xmid_B = ns["xmid_B"]; out_d = ns["out_d"]; out_B = ns["out_B"]
    xs_d = ns["xs_d"]; xs_B = ns["xs_B"]
    ys_t = nc.dram_tensor("ys", [NSLOT, D], BF16, kind="Internal"); ys_d = ys_t.ap(); ys_B = S.dram("ys")
    x2_t = nc.dram_tensor("x2", [T, D], F32, kind="Internal"); x2_d = x2_t.ap(); x2_B = S.dram("x2")

    S.barrier("pool", lambda e: e.memset(ones_b[:, 0:1], 1.0))
    S.sb_ptr = ns["P0_KEEP"]
    g2row = S.sb("g2row3", [128, D], F32); fgrow = S.sb("fgrow", [128, D], F32)
    w8all = S.sb("w8all", [128, NT, 8], F32); sloti = S.sb("sloti", [128, NT, 8], I32)
    idxall = S.sb("idxall", [128, 512], I32)
    P3_KEEP = S.sb_ptr
    sh2row = S.sb("sh2row3", [128, D], F32); a2row = S.sb("a2row3", [128, D], F32)
    S.dma("sp", sh2row[:], rowsave_d[0:1, :].partition_broadcast(128), reads=[rowsave_B], writes=[sh2row])
    S.dma("sp", a2row[:], rowsave_d[1:2, :].partition_broadcast(128), reads=[rowsave_B], writes=[a2row])
    S.dma("sp", g2row[:], rowsave_d[2:3, :].partition_broadcast(128), reads=[rowsave_B], writes=[g2row])
    S.dma("sp", fgrow[:], fgrow_d.partition_broadcast(128), writes=[fgrow])
    rbias = S.sb("rbias", [128, 256], F32)
    S.dma("act", rbias[:], rbias_d.partition_broadcast(128), writes=[rbias])
    wr = S.sb("wr", [128, 8, 256], F32)
    S.dma("sp", wr[:].rearrange("p a b -> p (a b)"), wr_d, writes=[wr])
    wsg = S.sb("wsg", [128, 8, 256], BF16); wsu = S.sb("wsu", [128, 8, 256], BF16); wsd = S.sb("wsd", [128, 2, 1024], BF16)
    S.dma("pool", wsg[:].rearrange("p a b -> p (a b)"), wsg_d, writes=[wsg])
    S.dma("pool", wsu[:].rearrange("p a b -> p (a b)"), wsu_d, writes=[wsu])
    S.dma("pool", wsd[:].rearrange("p a b -> p (a b)"), wsd_d, writes=[wsd])
    maskall = S.sb("maskall", [128, NT, 256], BF16)
    h2ball = S.sb("h2ball", [128, NT, D], BF16)
    eidxu = S.sb("eidxu", [128, NT, 8], U32); eidxf = S.sb("eidxf", [128, NT, 8], F32); slotf = S.sb("slotf", [128, NT, 8], F32)
    xmb = [S.sb(f"xm{i}", [128, D], F32) for i in range(2)]
    h2fs = [S.sb(f"h2f{j}", [128, D], F32) for j in range(3)]
    h2T32s = [S.sb(f"h2T32{j}", [128, 8, 128], F32) for j in range(3)]
    iotab = S.sb("iotab", [128, 512], F32); pendsT = S.sb("pendsT", [128, 2], F32)
    Mh = [S.sb(f"Mh{j}", [128, 512], BF16) for j in range(2)]
    S.dma("sp", iotab[:], ns["iotab_d"], writes=[iotab])
    ss = S.sb("ss3", [128, 1], F32); sd = S.sb("sd3", [128, 1], F32); rs = S.sb("rs3", [128, 1], F32)
    sv = S.sb("sv", [128, 256], F32); sel = S.sb("sel", [128, 256], F32); selm = S.sb("selm", [128, 256], F32)
    sw = S.sb("sw", [128, 256], F32); G = S.sb("G", [128, 256], F32); junk256 = S.sb("junk256", [128, 256], F32)
    top8g = S.sb("top8g", [128, 8, 8], F32); gs = S.sb("gs", [128, 8], F32); gsort = S.sb("gsort", [128, 8], F32)
    gmask = S.sb("gmask", [128, 8], F32); negb = S.sb("negb", [128, 8], F32); top8 = S.sb("top8", [128, 8], F32)
    ssum = S.sb("ssum", [128, 1], F32); rs2 = S.sb("rs2", [128, 1], F32)
    sgs = S.sb("sgs", [128, 256], F32); actsh = S.sb("actsh", [128, 256], BF16)
    x2t = [S.sb(f"x2t{i}", [128, D], F32) for i in range(2)]
    cnt = S.sb("cnt", [128, 256], F32); tq = S.sb("tq", [128, 256], F32); nbi = S.sb("nbi", [128, 256], I32)
    padded = S.sb("padded", [128, 256], F32); fix = S.sb("fix", [128, 256], F32)
    pends = S.sb("pends", [128, 256], F32); pstart = S.sb("pstart", [128, 256], F32); ones256 = S.sb("ones256", [128, 256], F32)
    eb = S.sb("eb", [128, 512], F32); idxf = S.sb("idxf", [128, 512], F32)
    ebs = S.sb("ebs", [128, 512], F32); same = S.sb("same", [128, 512], F32)
    macc = S.sb("macc", [128, 256], BF16); slotm = S.sb("slotm", [128, 256], F32)
    S.op("pool", lambda e: e.memset(macc[:], 0.0), writes=[macc])
    S.op("pool", lambda e: e.memset(ones256[:], 1.0), writes=[ones256])
    S.op("pool", lambda e: e.memset(eb[:], 256.0), writes=[eb])

    neghalf = ns["neghalf"]

    def load_xm(i):
        S.dma("sp", xmb[i % 2][:], xmid_d[i * 128:(i + 1) * 128, :], reads=[xmid_B], writes=[xmb[i % 2]])

    def stage_A(i):
        xm = xmb[i % 2]; h2f = h2fs[i % 3]; h2T32 = h2T32s[i % 3]
        if i + 1 < NT:
            load_xm(i + 1)
        S.op("act", lambda e: e.activation(out=h2f[:], in_=xm[:], func=AF.Square, accum_out=ss[:]), reads=[xm], writes=[h2f, ss])
        S.op("pool", lambda e: e.tensor_scalar(out=sd[:], in0=ss[:], scalar1=1.0 / D, scalar2=EPS, op0=ALU.mult, op1=ALU.add), reads=[ss], writes=[sd])
        S.op("pool", lambda e: e.tensor_tensor(out=rs[:], in0=sd[:], in1=neghalf[:], op=ALU.pow), reads=[sd, neghalf], writes=[rs])
        S.op("dve", lambda e: e.scalar_tensor_tensor(out=h2f[:], in0=xm[:], scalar=rs[:, 0:1], in1=a2row[:], op0=ALU.mult, op1=ALU.mult),
             reads=[xm, rs, a2row], writes=[h2f])
        S.op("dve", lambda e: e.tensor_tensor(out=h2f[:], in0=h2f[:], in1=sh2row[:], op=ALU.add), reads=[h2f, sh2row], writes=[h2f])
        S.op("act", lambda e: e.activation(out=h2ball[:, i, :], in_=h2f[:], func=AF.Copy), reads=[h2f], wshared=[h2ball])
        for k in range(8):
            pb = PB[0] if k < 4 else PB[1]
            S.op("pe", lambda e, k=k, pb=pb: e.transpose(out=pb[:, (k % 4) * 128:(k % 4 + 1) * 128], in_=h2f[:, k * 128:(k + 1) * 128], identity=ident_f[:]),
                 reads=[h2f, ident_f], writes=[pb])
        S.op("act", lambda e: e.activation(out=h2T32[:, 0:4, :].rearrange("p a b -> p (a b)"), in_=PB[0][:], func=AF.Copy), reads=[PB[0]], wshared=[h2T32])
        S.op("act", lambda e: e.activation(out=h2T32[:, 4:8, :].rearrange("p a b -> p (a b)"), in_=PB[1][:], func=AF.Copy), reads=[PB[1]], wshared=[h2T32])

    def stage_B1(i):
        h2T32 = h2T32s[i % 3]
        for k in range(8):
            S.op("pe", lambda e, k=k: e.matmul(out=PB[2][:, 0:256], lhsT=h2T32[:, k, :], rhs=wr[:, k, :], start=(k == 0), stop=(k == 7)),
                 reads=[h2T32, wr], writes=[PB[2]])
        S.op("act", lambda e: e.activation(out=sv[:], in_=PB[2][:, 0:256], func=AF.Sigmoid), reads=[PB[2]], writes=[sv])

    def stage_B(i):
        S.op("dve", lambda e: e.tensor_tensor(out=sel[:], in0=sv[:], in1=rbias[:], op=ALU.add), reads=[sv, rbias], writes=[sel])
        for g in range(8):
            S.op("dve", lambda e, g=g: e.max(out=top8g[:, g, :], in_=sel[:, g * 32:(g + 1) * 32]), reads=[sel], wshared=[top8g])
        S.op("dve", lambda e: e.tensor_tensor(out=gs[:], in0=top8g[:, :, 0], in1=top8g[:, :, 1], op=ALU.add), reads=[top8g], writes=[gs])
        S.op("dve", lambda e: e.max(out=gsort[:], in_=gs[:]), reads=[gs], writes=[gsort])
        S.op("dve", lambda e: e.tensor_scalar(out=gmask[:], in0=gs[:], scalar1=gsort[:, 3:4], scalar2=None, op0=ALU.is_ge), reads=[gs, gsort], writes=[gmask])
        S.op("dve", lambda e: e.tensor_scalar(out=negb[:], in0=gmask[:], scalar1=-1.0, scalar2=1e30, op0=ALU.add, op1=ALU.mult), reads=[gmask], writes=[negb])
        for g in range(8):
            S.op("dve", lambda e, g=g: e.tensor_scalar(out=selm[:, g * 32:(g + 1) * 32], in0=sel[:, g * 32:(g + 1) * 32],
                                                       scalar1=gmask[:, g:g + 1], scalar2=negb[:, g:g + 1], op0=ALU.mult, op1=ALU.add),
                 reads=[sel, gmask, negb], wshared=[selm])
        S.op("dve", lambda e: e.max(out=top8[:], in_=selm[:]), reads=[selm], writes=[top8])
        S.op("dve", lambda e: e.tensor_scalar(out=maskall[:, i, :], in0=selm[:], scalar1=top8[:, 7:8], scalar2=None, op0=ALU.is_ge),
             reads=[selm, top8], wshared=[maskall])
        S.op("dve", lambda e: e.scalar_tensor_tensor(out=sw[:], in0=sv[:], scalar=1.0, in1=maskall[:, i, :], op0=ALU.mult, op1=ALU.mult, accum_out=ssum[:]),
             reads=[sv, maskall], writes=[sw, ssum])
        S.op("dve", lambda e: e.reciprocal(out=rs2[:], in_=ssum[:]), reads=[ssum], writes=[rs2])
        S.op("dve", lambda e: e.tensor_scalar(out=G[:], in0=sw[:], scalar1=rs2[:, 0:1], scalar2=2.5, op0=ALU.mult, op1=ALU.mult), reads=[sw, rs2], writes=[G])
        S.op("dve", lambda e: e.max(out=w8all[:, i, :], in_=G[:]), reads=[G], wshared=[w8all])
        S.op("dve", lambda e: e.max_index(out=eidxu[:, i, :], in_max=w8all[:, i, :], in_values=G[:]), reads=[G, w8all], wshared=[eidxu])
        S.op("pe", lambda e: e.matmul(out=PB[3][:, 0:256], lhsT=ones_b[:], rhs=maskall[:, i, :], start=(i == 0), stop=(i == NT - 1)),
             reads=[ones_b, maskall], writes=[PB[3]])

    load_xm(0)
    stage_A(0)
    stage_A(1)
    for i in range(NT):
        stage_B1(i)
        if i + 2 < NT:
            stage_A(i + 2)
        stage_B(i)

    S.op("dve", lambda e: e.tensor_copy(out=cnt[:], in_=PB[3][:, 0:256]), reads=[PB[3]], writes=[cnt])
    S.op("dve", lambda e: e.tensor_scalar(out=tq[:], in0=cnt[:], scalar1=127.0, scalar2=1.0 / 128, op0=ALU.add, op1=ALU.mult), reads=[cnt], writes=[tq])
    S.op("dve", lambda e: e.tensor_scalar(out=nbi[:], in0=tq[:], scalar1=-0.49609375, scalar2=None, op0=ALU.add), reads=[tq], writes=[nbi])
    S.op("dve", lambda e: e.tensor_copy(out=padded[:], in_=nbi[:]), reads=[nbi], writes=[padded])
    S.op("dve", lambda e: e.tensor_scalar(out=padded[:], in0=padded[:], scalar1=128.0, scalar2=None, op0=ALU.mult), reads=[padded], writes=[padded])
    S.op("dve", lambda e: e.tensor_tensor(out=fix[:], in0=padded[:], in1=cnt[:], op=ALU.is_lt), reads=[padded, cnt], writes=[fix])
    S.op("dve", lambda e: e.scalar_tensor_tensor(out=padded[:], in0=fix[:], scalar=128.0, in1=padded[:], op0=ALU.mult, op1=ALU.add), reads=[fix, padded], writes=[padded])
    S.op("dve", lambda e: e.tensor_scalar(out=tq[:], in0=padded[:], scalar1=-128.0, scalar2=None, op0=ALU.add), reads=[padded], writes=[tq])
    S.op("dve", lambda e: e.tensor_tensor(out=fix[:], in0=tq[:], in1=cnt[:], op=ALU.is_ge), reads=[tq, cnt], writes=[fix])
    S.op("dve", lambda e: e.scalar_tensor_tensor(out=padded[:], in0=fix[:], scalar=-128.0, in1=padded[:], op0=ALU.mult, op1=ALU.add), reads=[fix, padded], writes=[padded])
    S.op("dve", lambda e: e.tensor_tensor_scan(out=pends[:], data0=ones256[:], data1=padded[:], initial=0.0, op0=ALU.mult, op1=ALU.add),
         reads=[ones256, padded], writes=[pends])
    S.op("dve", lambda e: e.tensor_tensor(out=pstart[:], in0=pends[:], in1=padded[:], op=ALU.subtract), reads=[pends, padded], writes=[pstart])
    for h in range(2):
        S.op("pe", lambda e, h=h: e.transpose(out=PB[4][:, h * 128:(h + 1) * 128], in_=pends[:, h * 128:(h + 1) * 128], identity=ident_f[:]),
             reads=[pends, ident_f], writes=[PB[4]])
    S.op("dve", lambda e: e.tensor_copy(out=pendsT[:], in_=PB[4][:, 0:256].rearrange("p (h c) -> p h c", h=2)[:, :, 0]), reads=[PB[4]], writes=[pendsT])
    for h in range(2):
        S.op("dve", lambda e, h=h: e.tensor_scalar(out=Mh[h][:], in0=iotab[:], scalar1=pendsT[:, h:h + 1], scalar2=None, op0=ALU.is_ge),
             reads=[iotab, pendsT], writes=[Mh[h]])
        S.op("pe", lambda e, h=h: e.matmul(out=PB[5][:], lhsT=ones_b[:], rhs=Mh[h][:], start=(h == 0), stop=(h == 1)),
             reads=[ones_b, Mh[h]], writes=[PB[5]])
    S.op("dve", lambda e: e.tensor_copy(out=eb[:], in_=PB[5][:]), reads=[PB[5]], writes=[eb])
    S.op("dve", lambda e: e.memset(eb[:, 511:512], 256.0), reads=[eb], writes=[eb])
    S.op("pool", lambda e: e.memset(ebs[:, 0:1], -1.0), wshared=[ebs])
    S.op("dve", lambda e: e.tensor_copy(out=ebs[:, 1:512], in_=eb[:, 0:511]), reads=[eb], wshared=[ebs])
    S.op("dve", lambda e: e.tensor_tensor(out=same[:], in0=eb[:], in1=ebs[:], op=ALU.is_equal), reads=[eb, ebs], writes=[same])
    S.op("dve", lambda e: e.memset(same[:].rearrange("p (l m) -> p l m", m=LB)[:, :, 0:1], 0.0), reads=[same], writes=[same])
    S.op("dve", lambda e: e.tensor_scalar(out=idxf[:], in0=eb[:], scalar1=128.0, scalar2=iotap[:, 0:1], op0=ALU.mult, op1=ALU.add), reads=[eb, iotap], writes=[idxf])
    S.op("dve", lambda e: e.scalar_tensor_tensor(out=idxf[:], in0=same[:], scalar=1.0e6, in1=idxf[:], op0=ALU.mult, op1=ALU.add), reads=[same, idxf], writes=[idxf])
    S.op("dve", lambda e: e.tensor_copy(out=idxall[:], in_=idxf[:]), reads=[idxf], writes=[idxall])

    h2Tb2 = S.sb("h2Tb2", [128, D], BF16)
    tsh = S.sb("tsh", [128, 256], F32)
    load_xm(0)
    for i in range(NT):
        xm = xmb[i % 2]
        if i + 1 < NT:
            load_xm(i + 1)
        S.op("pe", lambda e, i=i: e.matmul(out=PB[0][:, 0:256], lhsT=ustrict[:], rhs=maskall[:, i, :], start=True, stop=False), reads=[ustrict, maskall], writes=[PB[0]])
        S.op("pe", lambda e: e.matmul(out=PB[0][:, 0:256], lhsT=ones_b[:], rhs=macc[:], start=False, stop=True), reads=[ones_b, macc], writes=[PB[0]])
        S.op("dve", lambda e: e.tensor_tensor(out=slotm[:], in0=PB[0][:, 0:256], in1=pstart[:], op=ALU.add), reads=[PB[0], pstart], writes=[slotm])
        S.op("dve", lambda e, i=i: e.tensor_tensor(out=macc[:], in0=macc[:], in1=maskall[:, i, :], op=ALU.add), reads=[macc, maskall], writes=[macc])
        S.op("dve", lambda e, i=i: e.tensor_copy(out=eidxf[:, i, :], in_=eidxu[:, i, :]), reads=[eidxu], wshared=[eidxf])
        for k in range(8):
            S.op("dve", lambda e, i=i, k=k: e.scalar_tensor_tensor(out=junk256[:], in0=iotae[:], scalar=eidxf[:, i, k:k + 1], in1=slotm[:],
                                                                   op0=ALU.is_equal, op1=ALU.mult, accum_out=slotf[:, i, k:k + 1]),
                 reads=[iotae, eidxf, slotm], writes=[junk256], wshared=[slotf])
        S.op("dve", lambda e, i=i: e.tensor_copy(out=sloti[:, i, :], in_=slotf[:, i, :]), reads=[slotf], wshared=[sloti])
        for k in range(8):
            S.dma_fn("pool", lambda e, i=i, k=k: e.indirect_dma_start(
                out=xs_d, out_offset=bass.IndirectOffsetOnAxis(ap=sloti[:, i, k:k + 1], axis=0),
                in_=h2ball[:, i, :], in_offset=None, bounds_check=S.reg(e, NSLOT - 1), oob_is_err=False),
                reads=[h2ball, sloti], wshared=[xs_B])
        for k in range(8):
            S.op("pe", lambda e, i=i, k=k: e.transpose(out=PTb[:, k * 128:(k + 1) * 128], in_=h2ball[:, i, k * 128:(k + 1) * 128], identity=ident_b[:]),
                 reads=[h2ball, ident_b], writes=[PTb])
        S.op("act", lambda e: e.activation(out=h2Tb2[:], in_=PTb[:], func=AF.Copy), reads=[PTb], writes=[h2Tb2])
        for fc in range(2):
            for (wsrc, off) in ((wsg, 0), (wsu, 256)):
                for k in range(8):
                    S.op("pe", lambda e, fc=fc, wsrc=wsrc, off=off, k=k: e.matmul(out=PB[4][:, off + fc * 128:off + (fc + 1) * 128],
                                                                                   lhsT=wsrc[:, k, fc * 128:(fc + 1) * 128], rhs=h2Tb2[:, k * 128:(k + 1) * 128],
                                                                                   start=(k == 0), stop=(k == 7)),
                         reads=[wsrc, h2Tb2], writes=[PB[4]])
        S.op("act", lambda e: e.activation(out=sgs[:], in_=PB[4][:, 0:256], func=AF.Sigmoid), reads=[PB[4]], writes=[sgs])
        S.op("dve", lambda e: e.tensor_tensor(out=tsh[:], in0=sgs[:], in1=PB[4][:, 0:256], op=ALU.mult), reads=[sgs, PB[4]], writes=[tsh])
        S.op("dve", lambda e: e.tensor_tensor(out=actsh[:], in0=tsh[:], in1=PB[4][:, 256:512], op=ALU.mult), reads=[tsh, PB[4]], writes=[actsh])
        xo = x2t[i % 2]
        for n in range(2):
            for fc in range(2):
                S.op("pe", lambda e, n=n, fc=fc: e.matmul(out=PB[5 + n][:], lhsT=actsh[:, fc * 128:(fc + 1) * 128], rhs=wsd[:, fc, n * 512:(n + 1) * 512],
                                                          start=(fc == 0), stop=(fc == 1)), reads=[actsh, wsd], writes=[PB[5 + n]])
            S.op("dve", lambda e, n=n, xo=xo: e.tensor_tensor(out=xo[:, n * 512:(n + 1) * 512], in0=PB[5 + n][:], in1=g2row[:, n * 512:(n + 1) * 512], op=ALU.mult),
                 reads=[PB[5 + n], g2row], wshared=[xo])
        S.op("dve", lambda e, xo=xo, xm=xm: e.tensor_tensor(out=xo[:], in0=xo[:], in1=xm[:], op=ALU.add), reads=[xo, xm], writes=[xo])
        S.dma("sp", x2_d[i * 128:(i + 1) * 128, :], xo[:], reads=[xo], wshared=[x2_B])

    S.barrier("pool", lambda e: e.memset(ones_b[:, 0:1], 1.0))
    S.sb_ptr = P3_KEEP
    NL = 8
    wg = [S.sb(f"wg{j}", [128, 2048], BF16) for j in range(NL)]
    wu = [S.sb(f"wu{j}", [128, 2048], BF16) for j in range(NL)]
    wd = [S.sb(f"wd{j}", [128, 2048], BF16) for j in range(NL)]
    xsb = [S.sb(f"xsb{j}", [128, D], BF16) for j in range(3)]
    xsT = [S.sb(f"xsT{j}", [128, D], BF16) for j in range(2)]
    sgt = S.sb("sgt", [128, 256], F32)
    actT = [S.sb(f"actT{j}", [128, 256], BF16) for j in range(2)]
    ysb = [S.sb(f"ysb{j}", [128, D], BF16) for j in range(2)]
    order = [l * LB + m for m in range(LB) for l in range(NL) if l * LB + m < NBLK]

    def load_xs(n):
        b = order[n]
        S.dma("sp", xsb[n % 3][:], xs_d[b * 128:(b + 1) * 128, :], reads=[xs_B], writes=[xsb[n % 3]])

    def load_w(b):
        l = b // LB
        for (wt, src, extra) in ((wg[l], weg_d, []), (wu[l], weu_d, []), (wd[l], ns["wedb_d"], [ns["wedb_B"]])):
            S.dma_fn("pool", lambda e, wt=wt, src=src, b=b: e.indirect_dma_start(
                out=wt[:], out_offset=None, in_=src, in_offset=bass.IndirectOffsetOnAxis(ap=idxall[:, b:b + 1], axis=0),
                bounds_check=S.reg(e, E * 128 - 1), oob_is_err=False), reads=[idxall] + extra, writes=[wt])

    for l in range(NL):
        load_w(l * LB)
    load_xs(0); load_xs(1)
    for n, b in enumerate(order):
        l = b // LB
        if n + 2 < len(order):
            load_xs(n + 2)
        xb = xsb[n % 3]; xT = xsT[n % 2]; aT = actT[n % 2]; yb = ysb[n % 2]
        for k in range(8):
            S.op("pe", lambda e, k=k, xb=xb: e.transpose(out=PTb[:, k * 128:(k + 1) * 128], in_=xb[:, k * 128:(k + 1) * 128], identity=ident_b[:]),
                 reads=[xb, ident_b], writes=[PTb])
        if n % 2 == 0:
            S.op("act", lambda e, xT=xT: e.activation(out=xT[:], in_=PTb[:], func=AF.Copy), reads=[PTb], writes=[xT])
        else:
            S.op("dve", lambda e, xT=xT: e.tensor_copy(out=xT[:], in_=PTb[:]), reads=[PTb], writes=[xT])
        pg = PB[n % 2]
        for fc in range(2):
            for (wt, off) in ((wg[l], 0), (wu[l], 256)):
                for k in range(8):
                    S.op("pe", lambda e, fc=fc, wt=wt, off=off, k=k, pg=pg, xT=xT: e.matmul(
                        out=pg[:, off + fc * 128:off + (fc + 1) * 128], lhsT=wt[:, k * 256 + fc * 128:k * 256 + (fc + 1) * 128],
                        rhs=xT[:, k * 128:(k + 1) * 128], start=(k == 0), stop=(k == 7)), reads=[wt, xT], writes=[pg])
        S.op("act", lambda e, pg=pg: e.activation(out=sgt[:], in_=pg[:, 0:256], func=AF.Silu), reads=[pg], writes=[sgt])
        S.op("dve", lambda e, pg=pg, aT=aT: e.tensor_tensor(out=aT[:], in0=sgt[:], in1=pg[:, 256:512], op=ALU.mult), reads=[sgt, pg], writes=[aT])
        for nn in range(2):
            py = PB[2 + 2 * (n % 2) + nn]
            for fc in range(2):
                S.op("pe", lambda e, nn=nn, fc=fc, py=py, aT=aT, l=l: e.matmul(out=py[:], lhsT=aT[:, fc * 128:(fc + 1) * 128],
                                                                              rhs=wd[l][:, fc * 1024 + nn * 512:fc * 1024 + (nn + 1) * 512],
                                                                              start=(fc == 0), stop=(fc == 1)), reads=[aT, wd[l]], writes=[py])
            S.op("dve", lambda e, py=py, yb=yb, nn=nn: e.tensor_tensor(out=yb[:, nn * 512:(nn + 1) * 512], in0=py[:], in1=g2row[:, nn * 512:(nn + 1) * 512], op=ALU.mult),
                 reads=[py, g2row], wshared=[yb])
        S.dma("sp", ys_d[b * 128:(b + 1) * 128, :], yb[:], reads=[yb], wshared=[ys_B])
        if b + 1 < NBLK and (b + 1) % LB != 0:
            load_w(b + 1)

    S.barrier("pool", lambda e: e.memset(ones_b[:, 0:1], 1.0))
    S.sb_ptr = P3_KEEP
    gk = [S.sb(f"gk{j}", [128, 8, D], BF16) for j in range(2)]
    x2l = [S.sb(f"x2l{j}", [128, D], F32) for j in range(2)]
    acc = [S.sb(f"acc{j}", [128, D], F32) for j in range(2)]
    ot = [S.sb(f"ot{j}", [128, D], F32) for j in range(2)]
    ss4 = S.sb("ss4", [128, 1], F32); sd4 = S.sb("sd4", [128, 1], F32); rs4 = S.sb("rs4", [128, 1], F32)
    def load_c(i):
        gkt = gk[i % 2]; xl = x2l[i % 2]
        for k in range(8):
            S.dma_fn("pool", lambda e, i=i, k=k, gkt=gkt: e.indirect_dma_start(
                out=gkt[:, k, :], out_offset=None, in_=ys_d, in_offset=bass.IndirectOffsetOnAxis(ap=sloti[:, i, k:k + 1], axis=0),
                bounds_check=S.reg(e, NSLOT - 1), oob_is_err=False), reads=[ys_B, sloti], wshared=[gkt])
        S.dma("sp", xl[:], x2_d[i * 128:(i + 1) * 128, :], reads=[x2_B], writes=[xl])
    load_c(0)
    for i in range(NT):
        gkt = gk[i % 2]; xl = x2l[i % 2]; ac = acc[i % 2]; o_ = ot[i % 2]
        if i + 1 < NT:
            load_c(i + 1)
        S.op("dve", lambda e, i=i, gkt=gkt, ac=ac, xl=xl: e.scalar_tensor_tensor(out=ac[:], in0=gkt[:, 0, :], scalar=w8all[:, i, 0:1], in1=xl[:],
                                                                            op0=ALU.mult, op1=ALU.add), reads=[gkt, w8all, xl], writes=[ac])
        for k in range(1, 8):
            S.op("dve", lambda e, i=i, k=k, gkt=gkt, ac=ac: e.scalar_tensor_tensor(out=ac[:], in0=gkt[:, k, :], scalar=w8all[:, i, k:k + 1], in1=ac[:],
                                                                                   op0=ALU.mult, op1=ALU.add), reads=[gkt, w8all, ac], writes=[ac])
        S.op("act", lambda e, ac=ac, o_=o_: e.activation(out=o_[:], in_=ac[:], func=AF.Square, accum_out=ss4[:]), reads=[ac], writes=[o_, ss4])
        S.op("pool", lambda e: e.tensor_scalar(out=sd4[:], in0=ss4[:], scalar1=1.0 / D, scalar2=EPS, op0=ALU.mult, op1=ALU.add), reads=[ss4], writes=[sd4])
        S.op("pool", lambda e: e.tensor_tensor(out=rs4[:], in0=sd4[:], in1=neghalf[:], op=ALU.pow), reads=[sd4, neghalf], writes=[rs4])
        S.op("dve", lambda e, ac=ac, o_=o_: e.scalar_tensor_tensor(out=o_[:], in0=ac[:], scalar=rs4[:, 0:1], in1=fgrow[:], op0=ALU.mult, op1=ALU.mult),
             reads=[ac, rs4, fgrow], writes=[o_])
        S.dma("sp", out_d[i * 128:(i + 1) * 128, :], o_[:], reads=[o_], wshared=[out_B])

def _fm(v, nk):
    return np.ascontiguousarray(v.reshape(nk, 128).T)


def _kp(w):
    K = w.shape[0] // 128
    return np.ascontiguousarray(w.reshape(K, 128, w.shape[1]).transpose(1, 0, 2).reshape(128, K * w.shape[1]))


def _consts():
    c = {}
    c["ident"] = np.eye(128, dtype=np.float32)
    j = np.arange(128)[:, None]; i = np.arange(128)[None, :]
    NEG = np.float32(-240000.0)
    mprev = np.where(j >= i, np.float32(0.0), NEG).astype(np.float32); mnext = np.where(j <= i, np.float32(0.0), NEG).astype(np.float32)
    c["masks"] = np.concatenate([np.tile(mprev, (1, 4)), np.tile(mnext, (1, 4))], axis=1)
    c["ustrict"] = (j < i).astype(np.float32)
    L = T
    invc = np.zeros((4, 2, 8), np.float32)
    for g, w in enumerate((2, 4, 8, 16)):
        for side in range(2):
            for jj in range(8):
                t = jj if side == 0 else L - 8 + jj
                lo = min(max(t - w // 2, 0), L); hi = min(max(t + w // 2, 0), L)
                invc[g, side, jj] = 1.0 / float(hi - lo)
    c["invc"] = np.tile(invc.reshape(1, 64), (128, 1))
    c["iotae"] = np.tile(np.arange(256, dtype=np.float32)[None, :], (128, 1))
    c["iotap"] = np.arange(128, dtype=np.float32)[:, None].copy()
    c["iotab"] = np.tile((128.0 * np.arange(512, dtype=np.float32))[None, :], (128, 1))
    rows = T // 64
    row = np.repeat(np.arange(rows), 64).astype(np.float32)
    col = np.tile(np.arange(64), rows).astype(np.float32)
    nf = 16
    inv = (np.float32(10000.0) ** (-np.arange(nf, dtype=np.float32) / np.float32(nf))).astype(np.float32)
    ang = np.stack([row[:, None] * inv, col[:, None] * inv], axis=1)
    cs_, sn_ = np.cos(ang).astype(np.float32), np.sin(ang).astype(np.float32)
    C = np.zeros((64, T), np.float32); Sg = np.zeros((64, T), np.float32)
    for d in range(64):
        axis, ab, f = d // 32, (d % 32) // 16, d % 16
        C[d] = cs_[:, axis, f]
        Sg[d] = (-sn_[:, axis, f]) if ab == 0 else sn_[:, axis, f]
    c["ropec"] = np.concatenate([C, C], axis=0); c["ropes"] = np.concatenate([Sg, Sg], axis=0)
    return c


def _partner(cols):
    d = cols % 64
    ab = (d % 32) // 16
    return np.where(ab == 0, cols + 16, cols - 16)


def _prep_shared(inp, full=True):
    sh = dict(_consts())
    w_in = inp["w_in"][0]
    qcols = np.concatenate([np.concatenate([np.arange(c * 64, (c + 1) * 64), np.arange((4 + c) * 64, (5 + c) * 64)]) for c in range(4)])
    kcols = np.arange(512, 640)
    cols = np.concatenate([qcols, (_partner(qcols)), kcols, 512 + _partner(kcols - 512),
                           np.arange(768, 1280), np.arange(1280, 3328), np.arange(640, 768)])
    assert cols.shape[0] == NXC
    sh["wx"] = np.ascontiguousarray(w_in[:, cols])
    sh["wada"] = inp["w_ada"][0]; sh["bada"] = inp["b_ada"][0][None, :].copy(); sh["badafm"] = _fm(inp["b_ada"][0], 48)
    sh["n1fm"] = _fm(inp["norm1_g"][0], 8); sh["n2row"] = inp["norm2_g"][0][None, :].copy(); sh["fgrow"] = inp["final_g"][None, :].copy()
    sh["sink"] = inp["attn_sink"][0][None, :].copy()
    sh["wpool"] = np.ascontiguousarray(inp["w_pool"][0].transpose(1, 0, 2).reshape(128, 512))
    sh["pscale"] = _fm(inp["pool_scale"][0], 4)
    wua = inp["w_up_attn"][0]
    rows_ = np.concatenate([np.concatenate([np.arange(c * 64, (c + 1) * 64), np.arange((4 + c) * 64, (5 + c) * 64)]) for c in range(4)])
    sh["wua"] = _kp(wua[rows_]); sh["wup"] = _kp(inp["w_up_pool"][0]); sh["wout"] = _kp(inp["w_out"][0])
    if full:
        sh["wr"] = _kp(inp["w_router"][0]); sh["rbias"] = inp["router_bias"][0][None, :].copy()
        sh["wsg"] = _kp(inp["w_sh_gate"][0]); sh["wsu"] = _kp(inp["w_sh_up"][0]); sh["wsd"] = _kp(inp["w_sh_down"][0])
        def ek(w):
            Ee, R, N = w.shape
            K = R // 128
            return np.ascontiguousarray(w.reshape(Ee, K, 128, N).transpose(0, 2, 1, 3)).reshape(Ee * 128, K * N)
        sh["weg"] = ek(inp["w_exp_gate"][0]); sh["weu"] = ek(inp["w_exp_up"][0]); sh["wed"] = ek(inp["w_exp_down"][0])
    return sh


def _prep_core(inp, b):
    m = {}
    m["x"] = np.ascontiguousarray(inp["x"][b]); m["ctx"] = np.ascontiguousarray(inp["ctx"][b])
    cs = np.stack([_fm(inp["c"][b], 8), _fm(inp["c_ctx"], 8)], axis=2)
    m["csin"] = np.ascontiguousarray(cs.reshape(128, 16))
    return m


def kernel(**inputs):
    inp = {k: np.asarray(v, dtype=np.float32) for k, v in inputs.items()}
    nc = bass.Bass("TRN2", target_bir_lowering=False)
    build(nc, "full")
    sh = _prep_shared(inp, True)
    in_maps = []
    for b in range(8):
        m = dict(sh); m.update(_prep_core(inp, b)); in_maps.append(m)
    res = run_bass_kernel_spmd(nc, in_maps, core_ids=list(range(8)))
    return np.stack([res.results[b]["out"] for b in range(8)], axis=0).astype(np.float32)
```

```python
import numpy as np
import concourse.bass as bass
import concourse.mybir as mybir
from concourse.bass_utils import run_bass_kernel_spmd

ENGS = ("sp", "act", "dve", "pool", "pe")
SB_LO = 16512
SB_HI = 229376


class Ev:
    __slots__ = ("op", "shared")

    def __init__(self, op, shared=False):
        self.op = op
        self.shared = shared


class Buf:
    def __init__(self, S, name, t=None, is_dram=False):
        self.S = S
        self.name = name
        self.t = t
        self.is_dram = is_dram
        self.writers = {}
        self.readers = {}
        self.shared_keys = set()
        self.wsem = None
        self.rsem = None
        if S.epoch_op is not None:
            self.writers[S.epoch_op.semkey()] = S.epoch_op
        S.bufs.append(self)

    def __getitem__(self, k):
        return self.t[k]


class Op:
    __slots__ = ("eng", "fn", "deps", "is_dma", "sem", "val", "marked", "idx", "raw_same")

    def __init__(self, eng, fn, is_dma):
        self.eng = eng
        self.fn = fn
        self.deps = []
        self.is_dma = is_dma
        self.sem = None
        self.val = None
        self.marked = False

    def semkey(self):
        return ("dma", id(self.sem)) if self.is_dma else ("eng", self.eng)


class DmaSem:
    def __init__(self, S, name):
        self.h = S.nc.alloc_semaphore(name)
        self.count = 0


class Sched:
    def __init__(self, nc):
        self.nc = nc
        self.ops = {e: [] for e in ENGS}
        self.bufs = []
        self.epoch_op = None
        self.sb_ptr = SB_LO
        self.nsem = 0
        self.eng_sem = {}
        self.uid = 0
        self.psum_used = 0

    def sb(self, name, shape, dtype, buf=True):
        nbytes = 1
        for s in shape[1:]:
            nbytes *= s
        nbytes *= mybir.dt.size(dtype)
        nbytes = (nbytes + 31) // 32 * 32
        off = self.sb_ptr
        assert off + nbytes <= SB_HI, f"SBUF overflow allocating {name}: {off}+{nbytes}"
        self.sb_ptr += nbytes
        self.uid += 1
        t = self.nc.alloc_sbuf_tensor_at(f"{name}_{self.uid}", list(shape), dtype, offset=off)
        return Buf(self, name, t) if buf else t

    def ps(self, name, shape, dtype):
        self.uid += 1
        t = self.nc.alloc_psum_tensor(f"{name}_{self.uid}", list(shape), dtype)
        return Buf(self, name, t)

    def dram(self, name):
        return Buf(self, name, None, True)

    def reg(self, e, val):
        if not hasattr(self, "_regs"):
            self._regs = {}
        if val not in self._regs:
            self._regs[val] = e.to_reg(val)
        return self._regs[val]

    def newsem(self, name):
        self.nsem += 1
        return DmaSem(self, f"{name}_{self.nsem}")

    def _add(self, eng, fn, reads, writes, wshared, is_dma, sem):
        op = Op(eng, fn, is_dma)
        op.sem = sem
        deps = {}

        def add(d, kind):
            if d is op:
                return
            if (not d.is_dma) and (not is_dma) and d.eng == eng and kind != "raw":
                return
            if (not d.is_dma) and (not is_dma) and d.eng == eng == "pe":
                return
            k = d.semkey()
            cur = deps.get(k)
            if cur is None or d.idx > cur.idx:
                deps[k] = d

        for b in reads:
            for d in b.writers.values():
                add(d, "raw")
        for b in writes:
            for d in b.writers.values():
                add(d, "waw")
            for d in b.readers.values():
                add(d, "war")
        for b in wshared:
            for kk, d in b.writers.items():
                if kk not in b.shared_keys:
                    add(d, "waw")
            for d in b.readers.values():
                add(d, "war")
        op.deps = list(deps.values())
        op.idx = len(self.ops[eng])
        if is_dma:
            sem.count += 16
            op.val = sem.count
        if is_dma:
            op.idx = op.val
        self.ops[eng].append(op)
        k = op.semkey()
        for b in reads:
            b.readers[k] = op
        for b in writes:
            b.writers = {k: op}
            b.readers = {}
            b.shared_keys = set()
        for b in wshared:
            b.writers[k] = op
            b.shared_keys.add(k)
        return op

    def op(self, eng, fn, reads=(), writes=(), wshared=()):
        return self._add(eng, fn, list(reads), list(writes), list(wshared), False, None)

    def dma(self, eng, out_ap, in_ap, reads=(), writes=(), wshared=(), sem=None, cast=False, **kw):
        reads, writes, wshared = list(reads), list(writes), list(wshared)
        if sem is None:
            sem = self._pick_sem(reads, writes, wshared)

        def fn(e, out_ap=out_ap, in_ap=in_ap, kw=kw):
            return e.dma_start(out=out_ap, in_=in_ap, **kw)

        return self._add(eng, fn, reads, writes, wshared, True, sem)

    def _pick_sem(self, reads, writes, wshared):
        for b in writes + wshared:
            if not b.is_dram:
                if b.wsem is None:
                    b.wsem = self.newsem("w" + b.name)
                return b.wsem
        for b in reads:
            if not b.is_dram:
                if b.rsem is None:
                    b.rsem = self.newsem("r" + b.name)
                return b.rsem
        raise ValueError("dma needs explicit sem")

    def dma_fn(self, eng, fn, reads=(), writes=(), wshared=(), sem=None):
        reads, writes, wshared = list(reads), list(writes), list(wshared)
        if sem is None:
            sem = self._pick_sem(reads, writes, wshared)
        return self._add(eng, fn, reads, writes, wshared, True, sem)

    def barrier(self, eng="pool", fn=None):
        assert fn is not None
        op = self._add(eng, fn, [], list(self.bufs), [], False, None)
        self.epoch_op = op
        return op

    def finalize_and_emit(self):
        nc = self.nc
        for e in ENGS:
            for op in self.ops[e]:
                for d in op.deps:
                    d.marked = True
        for e in ENGS:
            c = 0
            for op in self.ops[e]:
                if not op.is_dma and op.marked:
                    c += 1
                    op.val = c
        for e in ENGS:
            self.eng_sem[e] = nc.alloc_semaphore(f"eng_{e}")
        engobj = {"sp": None, "act": None, "dve": None, "pool": None, "pe": None}
        S = self
        nwaits = {e: 0 for e in ENGS}

        def run(ename, eng):
            seen = {}
            mysems = {}
            for op in S.ops[ename]:
                for d in op.deps:
                    if d.is_dma:
                        key, h, v = ("d", id(d.sem)), d.sem.h, d.val
                    else:
                        key, h, v = ("e", d.eng), S.eng_sem[d.eng], d.val
                    if seen.get(key, 0) >= v:
                        continue
                    seen[key] = v
                    eng.wait_ge(h, v)
                    nwaits[ename] += 1
                ins = op.fn(eng)
                if op.is_dma:
                    ins.then_inc(op.sem.h, 16)
                    mysems[id(op.sem)] = op.sem
                elif op.marked:
                    ins.then_inc(S.eng_sem[ename], 1)
            for sm in mysems.values():
                if seen.get(("d", id(sm)), 0) < sm.count:
                    eng.wait_ge(sm.h, sm.count)

        with nc.Block() as block:
            @block.sync
            def _(e):
                run("sp", e)

            @block.scalar
            def _(e):
                run("act", e)

            @block.vector
            def _(e):
                run("dve", e)

            @block.gpsimd
            def _(e):
                run("pool", e)

            @block.tensor
            def _(e):
                run("pe", e)
        self.nwaits = nwaits

F32 = mybir.dt.float32
BF16 = mybir.dt.bfloat16
I32 = mybir.dt.int32
U32 = mybir.dt.uint32
AF = mybir.ActivationFunctionType
ALU = mybir.AluOpType
AX = mybir.AxisListType

T = 4096
D = 1024
NT = T // 128
CH = 256
NCH = T // CH
NXC = 3968
E = 256
NBLK = 511
NSLOT = NBLK * 128
EPS = 1e-6

QB, QR, KB, KR, UB, GA, GP, VB = 0, 4, 8, 9, 10, 14, 22, 30


def build(nc, stage="full"):
    S = Sched(nc)
    dt_in = lambda name, shape, dt=F32: nc.dram_tensor(name, list(shape), dt, kind="ExternalInput").ap()
    x_d = dt_in("x", [T, D]); ctx_d = dt_in("ctx", [256, D]); csin_d = dt_in("csin", [128, 16])
    wada_d = dt_in("wada", [D, 6144]); bada_d = dt_in("bada", [1, 6144]); badafm_d = dt_in("badafm", [128, 48])
    n1fm_d = dt_in("n1fm", [128, 8]); n2row_d = dt_in("n2row", [1, D]); fgrow_d = dt_in("fgrow", [1, D])
    wx_d = dt_in("wx", [D, NXC]); ropec_d = dt_in("ropec", [128, T]); ropes_d = dt_in("ropes", [128, T])
    sink_d = dt_in("sink", [1, 8]); wpool_d = dt_in("wpool", [128, 512]); pscale_d = dt_in("pscale", [128, 4])
    wua_d = dt_in("wua", [128, 4096]); wup_d = dt_in("wup", [128, 4096]); wout_d = dt_in("wout", [128, 8192])
    ident_d = dt_in("ident", [128, 128]); masks_d = dt_in("masks", [128, 1024]); ustrict_d = dt_in("ustrict", [128, 128])
    invc_d = dt_in("invc", [128, 64]); iotae_d = dt_in("iotae", [128, 256]); iotap_d = dt_in("iotap", [128, 1])
    iotab_d = dt_in("iotab", [128, 512])
    full = stage == "full"
    if full:
        wr_d = dt_in("wr", [128, 2048]); rbias_d = dt_in("rbias", [1, 256])
        wsg_d = dt_in("wsg", [128, 2048]); wsu_d = dt_in("wsu", [128, 2048]); wsd_d = dt_in("wsd", [128, 2048])
        weg_d = dt_in("weg", [E * 128, 2048]); weu_d = dt_in("weu", [E * 128, 2048]); wed_d = dt_in("wed", [E * 128, 2048])
    out_d = nc.dram_tensor("out", [T, D], F32, kind="ExternalOutput").ap()
    xmid_t = nc.dram_tensor("xmid", [T, D], F32, kind="Internal")
    xmid_d = xmid_t.ap()
    xmid_B = S.dram("xmid")
    out_B = S.dram("outd")

    def bcast(ap_row, n):
        return ap_row.partition_broadcast(128) if hasattr(ap_row, "partition_broadcast") else ap_row

    ident_f = S.sb("identf", [128, 128], F32); ident_b = S.sb("identb", [128, 128], BF16)
    masks = S.sb("masks", [128, 2, 512], BF16)
    ustrict = S.sb("ustrict", [128, 128], BF16); ones_b = S.sb("onesb", [128, 128], BF16)
    iotae = S.sb("iotae", [128, 256], F32); iotap = S.sb("iotap", [128, 1], F32)
    g1row = S.sb("g1row", [128, D], F32)
    rowsave_t = nc.dram_tensor("rowsave", [3, D], F32, kind="Internal")
    rowsave_d = rowsave_t.ap()
    rowsave_B = S.dram("rowsave")
    S.dma("sp", ident_f[:], ident_d, writes=[ident_f])
    S.dma("pool", ident_b[:], ident_d, writes=[ident_b])
    S.dma("pool", masks[:].rearrange("p a b -> p (a b)"), masks_d, writes=[masks])
    S.dma("pool", ustrict[:], ustrict_d, writes=[ustrict])
    S.dma("sp", iotae[:], iotae_d, writes=[iotae]); S.dma("sp", iotap[:], iotap_d, writes=[iotap])
    S.op("pool", lambda e: e.memset(ones_b[:], 1.0), writes=[ones_b])
    neghalf = S.sb("neghalf", [128, 1], F32)
    S.op("pool", lambda e: e.memset(neghalf[:], -0.5), writes=[neghalf])
    P2_BASE = S.sb_ptr

    PB = [S.ps(f"pb{i}", [128, 512], F32) for i in range(7)]
    PTb = S.ps("ptb", [128, 1024], BF16)

    cs_in = S.sb("csin", [128, 16], F32); cs = S.sb("cs", [128, 16], F32)
    badafm = S.sb("badafm", [128, 48], F32); n1fm = S.sb("n1fm", [128, 8], F32)
    modx = S.sb("modx", [128, 16], F32); modc = S.sb("modc", [128, 16], F32)
    a1x = S.sb("a1x", [128, 8], F32); a1c = S.sb("a1c", [128, 8], F32)
    P0_KEEP = S.sb_ptr
    WX = S.sb("WX", [128, 8, NXC], BF16)
    wxv = wx_d.rearrange("(k p) n -> p k n", p=128)
    for k in range(8):
        for hh in range(2):
            S.dma("pool", WX[:, k, hh * 1984:(hh + 1) * 1984], wxv[:, k, hh * 1984:(hh + 1) * 1984], wshared=[WX])
    wua = S.sb("wua", [128, 4, D], BF16); wup = S.sb("wup", [128, 4, D], BF16); wout = S.sb("wout", [128, 8, D], BF16)
    wpool = S.sb("wpool", [128, 4, 128], BF16); pscale = S.sb("pscale", [128, 4], F32)
    invc = S.sb("invc", [128, 4, 2, 8], F32)
    for g in range(4):
        S.dma("pool", wua[:, g, :], wua_d[:, g * 1024:(g + 1) * 1024], wshared=[wua])
        S.dma("pool", wup[:, g, :], wup_d[:, g * 1024:(g + 1) * 1024], wshared=[wup])
    for g in range(8):
        S.dma("pool", wout[:, g, :], wout_d[:, g * 1024:(g + 1) * 1024], wshared=[wout])
    S.dma("pool", wpool[:].rearrange("p a b -> p (a b)"), wpool_d, writes=[wpool])
    S.dma("sp", pscale[:], pscale_d, writes=[pscale])
    S.dma("sp", invc[:].rearrange("p a b c -> p (a b c)"), invc_d, writes=[invc])
    sink_sb = S.sb("sink", [1, 8], F32); esink1 = S.sb("esink1", [1, 8], F32); esink = S.sb("esink", [1, 2, 512], BF16)
    sinksel = S.sb("sinksel", [1, 2, 128], BF16)
    S.dma("sp", sink_sb[:], sink_d, writes=[sink_sb])
    S.op("act", lambda e: e.activation(out=esink1[:], in_=sink_sb[:], func=AF.Exp), reads=[sink_sb], writes=[esink1])
    for h in range(8):
        S.op("dve", lambda e, h=h: e.tensor_copy(out=esink[0:1, h // 4, (h % 4) * 128:(h % 4 + 1) * 128],
                                                 in_=esink1[0:1, h:h + 1].to_broadcast([1, 128])),
             reads=[esink1], writes=[esink])
    S.op("pool", lambda e: e.memset(sinksel[:], 0.0), writes=[sinksel])
    S.op("pool", lambda e: e.memset(sinksel[0:1, 0, 64:128], 1.0), writes=[sinksel])
    S.op("pool", lambda e: e.memset(sinksel[0:1, 1, 0:64], 1.0), writes=[sinksel])

    P2_ACT = S.sb_ptr
    csrep = S.sb("csrep", [128, 8, 128], F32)
    wadab = [S.sb(f"wada{i}", [128, 8, 512], F32) for i in range(2)]
    brow = [S.sb(f"brow{i}", [128, D], F32) for i in range(2)]
    n2row = S.sb("n2row", [128, D], F32)
    sh2row = S.sb("sh2row", [128, D], F32); a2row = S.sb("a2row", [128, D], F32); g2row = S.sb("g2row", [128, D], F32)
    if full:
        xs_t = nc.dram_tensor("xs", [NSLOT, D], BF16, kind="Internal"); xs_d = xs_t.ap(); xs_B = S.dram("xs")
    S.dma("sp", cs_in[:], csin_d, writes=[cs_in])
    S.dma("sp", badafm[:], badafm_d, writes=[badafm]); S.dma("sp", n1fm[:], n1fm_d, writes=[n1fm])
    S.dma("act", n2row[:], n2row_d.partition_broadcast(128), writes=[n2row])
    S.op("act", lambda e: e.activation(out=cs[:], in_=cs_in[:], func=AF.Silu), reads=[cs_in], writes=[cs])
    csv = cs[:].rearrange("p (k t) -> p k t", t=2)
    for k in range(8):
        S.op("dve", lambda e, k=k: e.tensor_copy(out=csrep[:, k, :], in_=cs[:, 2 * k:2 * k + 1].to_broadcast([128, 128])),
             reads=[cs], writes=[csrep])
    wview = wada_d.rearrange("(k p) n -> p k n", p=128)
    rows_dst = {2: g1row, 3: sh2row, 4: a2row, 5: g2row}
    for m2 in range(12):
        m, hf = m2 // 2, m2 % 2
        wb = wadab[m2 % 2]
        for k in range(8):
            S.dma("sp" if k % 2 == 0 else "act", wb[:, k, :], wview[:, k, m2 * 512:(m2 + 1) * 512], wshared=[wb])
        if m < 2:
            pa = PB[m2 % 2]
            for jj in range(4):
                for k in range(8):
                    S.op("pe", lambda e, pa=pa, wb=wb, jj=jj, k=k: e.matmul(
                        out=pa[:, 2 * jj:2 * jj + 2], lhsT=wb[:, k, jj * 128:(jj + 1) * 128],
                        rhs=cs[:, 2 * k:2 * k + 2], start=(k == 0), stop=(k == 7)),
                        reads=[wb, cs], writes=[pa])
            j0_ = m * 8 + hf * 4
            S.op("dve", lambda e, pa=pa, j0_=j0_: e.tensor_tensor(out=modx[:, j0_:j0_ + 4], in0=pa[:, 0:8].rearrange("p (j t) -> p j t", t=2)[:, :, 0],
                                                                  in1=badafm[:, j0_:j0_ + 4], op=ALU.add),
                 reads=[pa, badafm], wshared=[modx])
            S.op("dve", lambda e, pa=pa, j0_=j0_: e.tensor_tensor(out=modc[:, j0_:j0_ + 4], in0=pa[:, 0:8].rearrange("p (j t) -> p j t", t=2)[:, :, 1],
                                                                  in1=badafm[:, j0_:j0_ + 4], op=ALU.add),
                 reads=[pa, badafm], wshared=[modc])
        else:
            br = brow[m % 2]
            if hf == 0:
                S.dma("act", br[:], bada_d[:, m * 1024:(m + 1) * 1024].partition_broadcast(128), writes=[br])
            dst = rows_dst[m]
            pa = PB[2 + hf]
            for k in range(8):
                S.op("pe", lambda e, pa=pa, wb=wb, k=k: e.matmul(
                    out=pa[:], lhsT=csrep[:, k, :], rhs=wb[:, k, :],
                    start=(k == 0), stop=(k == 7)), reads=[wb, csrep], writes=[pa])
            S.op("dve", lambda e, pa=pa, hf=hf, dst=dst, br=br: e.tensor_tensor(
                out=dst[:, hf * 512:(hf + 1) * 512], in0=pa[:], in1=br[:, hf * 512:(hf + 1) * 512], op=ALU.add),
                reads=[pa, br], wshared=[dst])
            if m == 4 and hf == 1:
                S.op("dve", lambda e: e.scalar_tensor_tensor(out=a2row[:], in0=a2row[:], scalar=1.0, in1=n2row[:],
                                                             op0=ALU.add, op1=ALU.mult),
                     reads=[a2row, n2row], writes=[a2row])
    S.op("dve", lambda e: e.scalar_tensor_tensor(out=a1x[:], in0=modx[:, 8:16], scalar=1.0, in1=n1fm[:],
                                                 op0=ALU.add, op1=ALU.mult), reads=[modx, n1fm], writes=[a1x])
    S.op("dve", lambda e: e.scalar_tensor_tensor(out=a1c[:], in0=modc[:, 8:16], scalar=1.0, in1=n1fm[:],
                                                 op0=ALU.add, op1=ALU.mult), reads=[modc, n1fm], writes=[a1c])

    if stage == "P0":
        S.dma("sp", out_d[0:128, :], g1row[:], reads=[g1row], wshared=[out_B])
        S.dma("sp", out_d[128:256, :], a2row[:], reads=[a2row], wshared=[out_B])
        S.dma("sp", out_d[256:384, 0:16], modx[:], reads=[modx], wshared=[out_B])
        S.dma("sp", out_d[256:384, 16:32], modc[:], reads=[modc], wshared=[out_B])
        S.dma("sp", out_d[256:384, 32:40], a1x[:], reads=[a1x], wshared=[out_B])
        S.finalize_and_emit()
        return nc
    for ri, rr in enumerate((sh2row, a2row, g2row)):
        S.dma("sp", rowsave_d[ri:ri + 1, :], rr[0:1, :], reads=[rr], wshared=[rowsave_B])
    S.barrier("pool", lambda e: e.memset(ones_b[:, 0:1], 1.0))
    S.sb_ptr = P2_ACT

    import os
    if os.environ.get("DBG_STOP", "") == "p0end":
        S.finalize_and_emit()
        return nc
    xt = [S.sb(f"xt{i}", [128, D], F32) for i in range(2)]
    xnb = [S.sb(f"xnb{i}", [128, D], BF16) for i in range(2)]
    ssq = [S.sb(f"ssq{i}", [128, 1], F32) for i in range(2)]
    sdv = [S.sb(f"sdv{i}", [128, 1], F32) for i in range(2)]
    rstd = [S.sb(f"rstd{i}", [128, 1], F32) for i in range(2)]
    hT = [S.sb(f"hT{i}", [128, 8, CH], BF16) for i in range(2)]
    hcT = S.sb("hcT", [128, 8, 256], BF16)
    kcT = S.sb("kcT", [128, 256], BF16)
    vctx = S.sb("vctx", [128, 2, 2, 128], BF16)
    kring = [S.sb(f"kring{i}", [128, CH], BF16) for i in range(3)]
    vring = [S.sb(f"vring{i}", [128, 2, 2, 128], BF16) for i in range(3)]
    uring = [S.sb(f"uring{i}", [128, 4, CH + 16], F32) for i in range(2)]
    qT = S.sb("qT", [128, 4, CH], BF16)
    sg = S.sb("sg", [128, 16, CH], BF16)
    ropeC = [S.sb(f"ropeC{i}", [128, CH], F32) for i in range(2)]
    ropeS = [S.sb(f"ropeS{i}", [128, CH], F32) for i in range(2)]
    rt1 = S.sb("rt1", [128, CH], F32); rt2 = S.sb("rt2", [128, CH], F32)
    pT = [S.sb(f"pT{i}", [128, 512], BF16) for i in range(5)]
    recA = [S.sb(f"recA{i}", [128, 512], F32) for i in range(2)]; recB = [S.sb(f"recB{i}", [128, 512], F32) for i in range(2)]
    attnT = S.sb("attnT", [128, 4, CH], BF16)
    ps2 = S.sb("ps2", [128, CH + 16], F32); ps4 = S.sb("ps4", [128, CH + 16], F32); ps8 = S.sb("ps8", [128, CH + 16], F32)
    ps16 = S.sb("ps16", [128, CH + 16], F32)
    dT = S.sb("dT", [128, 4, CH], BF16); poolT = S.sb("poolT", [128, 4, CH], BF16)
    etmp = S.sb("etmp", [128, 8], F32)
    yT = S.sb("yT", [128, 8, CH], BF16); mt1 = S.sb("mt1", [128, CH], F32); mt2 = S.sb("mt2", [128, CH], F32)
    xr = [S.sb(f"xr{i}", [128, D], F32) for i in range(1)] * 2
    xmo = [S.sb(f"xmo{i}", [128, D], F32) for i in range(2)]
    for t in vring + [vctx]:
        S.op("pool", lambda e, t=t: e.memset(t[:], 1.0), writes=[t])
    for u in uring:
        S.op("pool", lambda e, u=u: e.memset(u[:], 0.0), writes=[u])

    if full:
        wedb_t = nc.dram_tensor("wedb", [E * 128, 2048], BF16, kind="Internal"); wedb_d = wedb_t.ap(); wedb_B = S.dram("wedb")
        conv_sem = S.newsem("convwd")

    def conv_wd(c):
        if not full:
            return
        per = E // NCH
        for ex in range(c * per, (c + 1) * per):
            S.dma("pool", wedb_d[ex * 128:(ex + 1) * 128, :], wed_d[ex * 128:(ex + 1) * 128, :], wshared=[wedb_B], sem=conv_sem)

    tile_ctr = [0]
    import os
    if os.environ.get("DBG_STOP", "") == "setup":
        S.finalize_and_emit()
        return nc

    def make_hT(src_rows_ap, avec, shvec, dst, dst_off):
        i = tile_ctr[0] % 2
        tile_ctr[0] += 1
        x_, xn_, ss_, sd_, rs_ = xt[i], xnb[i], ssq[i], sdv[i], rstd[i]
        S.dma("sp", x_[:], src_rows_ap, writes=[x_])
        S.op("act", lambda e: e.activation(out=xn_[:], in_=x_[:], func=AF.Square, accum_out=ss_[:]),
             reads=[x_], writes=[xn_, ss_])
        S.op("pool", lambda e: e.tensor_scalar(out=sd_[:], in0=ss_[:], scalar1=1.0 / D, scalar2=EPS, op0=ALU.mult, op1=ALU.add),
             reads=[ss_], writes=[sd_])
        S.op("pool", lambda e: e.tensor_tensor(out=rs_[:], in0=sd_[:], in1=neghalf[:], op=ALU.pow), reads=[sd_, neghalf], writes=[rs_])
        S.op("dve", lambda e: e.tensor_scalar(out=xn_[:], in0=x_[:], scalar1=rs_[:], scalar2=None, op0=ALU.mult),
             reads=[x_, rs_], writes=[xn_])
        if os.environ.get("DBG_STOP", "") == "h1":
            return
        for k in range(8):
            S.op("pe", lambda e, k=k: e.transpose(out=PTb[:, k * 128:(k + 1) * 128], in_=xn_[:, k * 128:(k + 1) * 128],
                                                  identity=ident_b[:]), reads=[xn_, ident_b], writes=[PTb])
        if os.environ.get("DBG_STOP", "") == "h2":
            return
        EV = os.environ.get("DBG_EVAC", "")
        for k in range(8):
            if i == 0:
                S.op("act", lambda e, k=k: e.activation(out=dst[:, k, dst_off:dst_off + 128], in_=PTb[:, k * 128:(k + 1) * 128],
                                                        func=AF.Identity, scale=avec[:, k:k + 1], bias=shvec[:, k:k + 1]),
                     reads=[PTb, avec, shvec], wshared=[dst])
            else:
                S.op("dve", lambda e, k=k: e.tensor_scalar(out=dst[:, k, dst_off:dst_off + 128], in0=PTb[:, k * 128:(k + 1) * 128],
                                                           scalar1=avec[:, k:k + 1], scalar2=shvec[:, k:k + 1],
                                                           op0=ALU.mult, op1=ALU.add),
                     reads=[PTb, avec, shvec], wshared=[dst])

    pb_rr = [0]

    def next_pb():
        p = PB[pb_rr[0] % 2]
        pb_rr[0] += 1
        return p

    def proj_fm(hsrc, blk, ncols):
        p = next_pb()
        for k in range(8):
            S.op("pe", lambda e, k=k, p=p: e.matmul(out=p[:, 0:ncols], lhsT=WX[:, k, blk * 128:(blk + 1) * 128],
                                                    rhs=hsrc[:, k, 0:ncols], start=(k == 0), stop=(k == 7)),
                 reads=[WX, hsrc], writes=[p])
        return p

    for t in range(2):
        make_hT(ctx_d[t * 128:(t + 1) * 128, :], a1c, modc, hcT, t * 128)
    if os.environ.get("DBG_STOP", "") in ("h1", "h2", "h3"):
        S.finalize_and_emit()
        return nc
    p = proj_fm(hcT, KB, 256)
    S.op("act", lambda e, p=p: e.activation(out=kcT[:], in_=p[:, 0:256], func=AF.Copy), reads=[p], writes=[kcT])

    def proj_v(hsrc, tok_off, dst4, blk):
        p = next_pb()
        for k in range(8):
            S.op("pe", lambda e, k=k, p=p: e.matmul(out=p[:, 0:128], lhsT=hsrc[:, k, tok_off:tok_off + 128],
                                                    rhs=WX[:, k, VB * 128:(VB + 1) * 128], start=(k == 0), stop=(k == 7)),
                 reads=[WX, hsrc], writes=[p])
        S.op("act", lambda e, p=p: e.activation(out=dst4[:, blk, 0, 0:64], in_=p[:, 0:64], func=AF.Copy),
             reads=[p], wshared=[dst4])
        S.op("act", lambda e, p=p: e.activation(out=dst4[:, blk, 1, 64:128], in_=p[:, 64:128], func=AF.Copy),
             reads=[p], wshared=[dst4])

    for t in range(2):
        proj_v(hcT, t * 128, vctx, t)

    def stage_H(c):
        for t in range(2):
            make_hT(x_d[c * CH + t * 128:c * CH + (t + 1) * 128, :], a1x, modx, hT[c % 2], t * 128)

    def load_rope(c):
        S.dma("sp", ropeC[c % 2][:], ropec_d[:, c * CH:(c + 1) * CH], writes=[ropeC[c % 2]])
        S.dma("sp", ropeS[c % 2][:], ropes_d[:, c * CH:(c + 1) * CH], writes=[ropeS[c % 2]])

    def rope_evac(pa, pr, c, dst_ap, dstB):
        rc, rs_ = ropeC[c % 2], ropeS[c % 2]
        S.op("dve", lambda e: e.tensor_tensor(out=rt1[:], in0=pa[:, 0:CH], in1=rc[:], op=ALU.mult),
             reads=[pa, rc], writes=[rt1])
        S.op("dve", lambda e: e.tensor_tensor(out=rt2[:], in0=pr[:, 0:CH], in1=rs_[:], op=ALU.mult),
             reads=[pr, rs_], writes=[rt2])
        S.op("pool", lambda e: e.tensor_tensor(out=dst_ap, in0=rt1[:], in1=rt2[:], op=ALU.add),
             reads=[rt1, rt2], wshared=[dstB])

    def kvu_units(c):
        h = hT[c % 2]
        u = uring[c % 2]
        units = []

        def unit_k():
            pa = proj_fm(h, KB, CH); pr = proj_fm(h, KR, CH)
            rope_evac(pa, pr, c, kring[c % 3][:], kring[c % 3])
        units.append(unit_k)
        for t in range(2):
            units.append(lambda t=t: proj_v(h, t * 128, vring[c % 3], t))
        for g in range(4):
            def unit_u(g=g):
                p = proj_fm(h, UB + g, CH)
                S.op("act", lambda e, p=p, g=g: e.activation(out=u[:, g, 8:8 + CH], in_=p[:, 0:CH], func=AF.Copy),
                     reads=[p], wshared=[u])
            units.append(unit_u)

        def unit_halo():
            if c > 0:
                up = uring[(c - 1) % 2]
                S.op("pool", lambda e: e.tensor_copy(out=up[:, :, 8 + CH:16 + CH], in_=u[:, :, 8:16]), reads=[u], wshared=[up])
                S.op("pool", lambda e: e.tensor_copy(out=u[:, :, 0:8], in_=up[:, :, CH:8 + CH]), reads=[up], wshared=[u])
            else:
                S.op("pool", lambda e: e.memset(u[:, :, 0:8], 0.0), wshared=[u])
            if c == NCH - 1:
                S.op("pool", lambda e: e.memset(u[:, :, 8 + CH:16 + CH], 0.0), wshared=[u])
        units.append(unit_halo)
        return units

    def stage_KVU(c):
        for f in kvu_units(c):
            f()

    def stage_QG(c):
        h = hT[c % 2]
        for q in range(4):
            pa = proj_fm(h, QB + q, CH); pr = proj_fm(h, QR + q, CH)
            rope_evac(pa, pr, c, qT[:, q, :], qT)
        for j in range(16):
            p = proj_fm(h, GA + j, CH)
            S.op("act", lambda e, p=p, j=j: e.activation(out=sg[:, j, :], in_=p[:, 0:CH], func=AF.Sigmoid),
                 reads=[p], wshared=[sg])

    pt_rr = [0]
    PSC = [PB[2], PB[3], PB[6], PB[0]]

    def attention(c):
        steps = []
        groups = []
        for i in range(2):
            n = 2 * c + i
            for g in range(2):
                gs = slice(g * 64, (g + 1) * 64)
                keys = []
                if n > 0:
                    cc, bb = (n - 1) // 2, (n - 1) % 2
                    keys.append((kring[cc % 3], kring[cc % 3][gs, bb * 128:(bb + 1) * 128], vring[cc % 3], vring[cc % 3][:, bb, g, :], 0))
                keys.append((kring[c % 3], kring[c % 3][gs, i * 128:(i + 1) * 128], vring[c % 3], vring[c % 3][:, i, g, :], None))
                if n < NT - 1:
                    cc, bb = (n + 1) // 2, (n + 1) % 2
                    keys.append((kring[cc % 3], kring[cc % 3][gs, bb * 128:(bb + 1) * 128], vring[cc % 3], vring[cc % 3][:, bb, g, :], 1))
                for t in range(2):
                    keys.append((kcT, kcT[gs, t * 128:(t + 1) * 128], vctx, vctx[:, t, g, :], None))
                gi = len(groups)
                groups.append(dict(i=i, g=g, po=PB[4 + gi % 2], nk=len(keys), qap=qT[gs, :, i * 128:(i + 1) * 128],
                                   rA=recA[gi % 2], rB=recB[gi % 2]))
                for ki, key in enumerate(keys):
                    steps.append((gi, ki, key))
        bufs = {}

        def emit_qk_exp(s):
            gi, ki, (kB, kap, vB, vap, mk) = steps[s]
            psc = PSC[pt_rr[0] % len(PSC)]
            pt = pT[pt_rr[0] % len(pT)]
            pt_rr[0] += 1
            bufs[s] = pt
            qap = groups[gi]["qap"]
            S.op("pe", lambda e, psc=psc, kap=kap, qap=qap, mk=mk: e.matmul(out=psc[:].rearrange("p (a b) -> p a b", a=4), lhsT=kap, rhs=qap,
                                                                            start=True, stop=(mk is None)), reads=[kB, qT], writes=[psc])
            if mk is not None:
                S.op("pe", lambda e, psc=psc, mk=mk: e.matmul(out=psc[:], lhsT=ident_b[:], rhs=masks[:, mk, :], start=False, stop=True),
                     reads=[ident_b, masks], writes=[psc])
            S.op("act", lambda e, psc=psc, pt=pt: e.activation(out=pt[:], in_=psc[:], func=AF.Exp, scale=0.125),
                 reads=[psc], writes=[pt])

        def emit_pv(s):
            gi, ki, (kB, kap, vB, vap, mk) = steps[s]
            G_ = groups[gi]; po = G_["po"]; pt = bufs[s]; g = G_["g"]
            S.op("pe", lambda e, po=po, vap=vap, pt=pt, ki=ki: e.matmul(out=po[:], lhsT=vap, rhs=pt[:], start=(ki == 0), stop=False),
                 reads=[vB, pt], writes=[po])
            if ki == G_["nk"] - 1:
                S.op("pe", lambda e, po=po, g=g: e.matmul(out=po[:], lhsT=sinksel[0:1, g, :], rhs=esink[0:1, g, :], start=False, stop=True),
                     reads=[sinksel, esink], writes=[po])

        def emit_norm_a(gi):
            G_ = groups[gi]; po = G_["po"]; rA = G_["rA"]
            ds = slice(64, 128) if G_["g"] == 0 else slice(0, 64)
            S.op("dve", lambda e, po=po, ds=ds, rA=rA: e.reciprocal(out=rA[ds, :], in_=po[ds, :]), reads=[po], writes=[rA])

        def emit_norm_b(gi):
            G_ = groups[gi]; po = G_["po"]; rA = G_["rA"]; rB = G_["rB"]; i = G_["i"]
            ns = slice(0, 64) if G_["g"] == 0 else slice(64, 128)
            ds = slice(64, 128) if G_["g"] == 0 else slice(0, 64)
            S.op("act", lambda e, ns=ns, ds=ds, rA=rA, rB=rB: e.activation(out=rB[ns, :], in_=rA[ds, :], func=AF.Copy),
                 reads=[rA], writes=[rB])
            S.op("dve", lambda e, po=po, ns=ns, i=i, rB=rB: e.tensor_tensor(
                out=attnT[ns, :, i * 128:(i + 1) * 128], in0=po[ns, :].rearrange("p (a b) -> p a b", a=4),
                in1=rB[ns, :].rearrange("p (a b) -> p a b", a=4), op=ALU.mult), reads=[po, rB], wshared=[attnT])

        nsteps = len(steps)
        pend = []
        LA = 3
        for s0 in range(min(LA, nsteps)):
            emit_qk_exp(s0)
        for s in range(nsteps):
            if s + LA < nsteps:
                emit_qk_exp(s + LA)
            emit_pv(s)
            gi, ki, _ = steps[s]
            if ki == groups[gi]["nk"] - 1:
                emit_norm_a(gi)
                pend.append((s + 3, gi))
            while pend and pend[0][0] <= s:
                emit_norm_b(pend.pop(0)[1])
        for _, gi in pend:
            emit_norm_b(gi)

    def poolmix_a(c):
        u = uring[c % 2]
        W = CH + 16
        for g in range(4):
            ug = u[:, g, :]
            S.op("pool", lambda e, ug=ug: e.tensor_tensor(out=ps2[:, 1:W], in0=ug[:, 0:W - 1], in1=ug[:, 1:W], op=ALU.add),
                 reads=[u], writes=[ps2])
            src = ps2
            if g >= 1:
                S.op("pool", lambda e: e.tensor_tensor(out=ps4[:, 2:W - 1], in0=ps2[:, 1:W - 2], in1=ps2[:, 3:W], op=ALU.add),
                     reads=[ps2], writes=[ps4])
                src = ps4
            if g >= 2:
                S.op("pool", lambda e: e.tensor_tensor(out=ps8[:, 4:W - 3], in0=ps4[:, 2:W - 5], in1=ps4[:, 6:W - 1], op=ALU.add),
                     reads=[ps4], writes=[ps8])
                src = ps8
            if g >= 3:
                S.op("pool", lambda e: e.tensor_tensor(out=ps16[:, 8:W - 7], in0=ps8[:, 4:W - 11], in1=ps8[:, 12:W - 3], op=ALU.add),
                     reads=[ps8], writes=[ps16])
                src = ps16
            w = 2 ** (g + 1)
            S.op("dve", lambda e, src=src, g=g, w=w, ug=ug: e.scalar_tensor_tensor(
                out=dT[:, g, :], in0=src[:, 8:8 + CH], scalar=1.0 / w, in1=ug[:, 8:8 + CH], op0=ALU.mult, op1=ALU.subtract),
                reads=[src, u], wshared=[dT])
            for (cond, side, col) in ((c == 0, 0, 0), (c == NCH - 1, 1, CH - 8)):
                if cond:
                    S.op("dve", lambda e, src=src, g=g, side=side, col=col: e.tensor_tensor(
                        out=etmp[:], in0=src[:, 8 + col:16 + col], in1=invc[:, g, side, :], op=ALU.mult),
                        reads=[src, invc], writes=[etmp])
                    S.op("dve", lambda e, g=g, col=col, ug=ug: e.tensor_tensor(
                        out=dT[:, g, col:col + 8], in0=etmp[:], in1=ug[:, 8 + col:16 + col], op=ALU.subtract),
                        reads=[etmp, u, dT], wshared=[dT])

    def poolmix_b(c):
        for g in range(4):
            p = next_pb()
            S.op("pe", lambda e, p=p, g=g: e.matmul(out=p[:, 0:CH], lhsT=wpool[:, g, :], rhs=dT[:, g, :], start=True, stop=True),
                 reads=[wpool, dT], writes=[p])
            S.op("act", lambda e, p=p, g=g: e.activation(out=poolT[:, g, :], in_=p[:, 0:CH], func=AF.Identity, scale=pscale[:, g:g + 1]),
                 reads=[p, pscale], wshared=[poolT])

    def merge(c, extra_units=()):
        for oc in range(8):
            pa = PB[2 + oc % 2]; pb_ = (PB[6], PB[0], PB[1])[oc % 3]
            for q in range(4):
                S.op("pe", lambda e, q=q, oc=oc, pa=pa: e.matmul(out=pa[:, 0:CH], lhsT=wua[:, q, oc * 128:(oc + 1) * 128], rhs=attnT[:, q, :],
                                                          start=(q == 0), stop=(q == 3)), reads=[wua, attnT], writes=[pa])
            for g in range(4):
                S.op("pe", lambda e, g=g, oc=oc, pb_=pb_: e.matmul(out=pb_[:, 0:CH], lhsT=wup[:, g, oc * 128:(oc + 1) * 128], rhs=poolT[:, g, :],
                                                                   start=(g == 0), stop=(g == 3)), reads=[wup, poolT], writes=[pb_])
            ma, mb = (mt1, mt2) if oc % 2 == 0 else (rt1, rt2)
            S.op("dve", lambda e, oc=oc, pa=pa, ma=ma: e.tensor_tensor(out=ma[:], in0=pa[:, 0:CH], in1=sg[:, oc, :], op=ALU.mult),
                 reads=[pa, sg], writes=[ma])
            S.op("dve", lambda e, oc=oc, pb_=pb_, mb=mb: e.tensor_tensor(out=mb[:], in0=pb_[:, 0:CH], in1=sg[:, 8 + oc, :], op=ALU.mult),
                 reads=[pb_, sg], writes=[mb])
            S.op("pool", lambda e, oc=oc, ma=ma, mb=mb: e.tensor_tensor(out=yT[:, oc, :], in0=ma[:], in1=mb[:], op=ALU.add),
                 reads=[ma, mb], wshared=[yT])
        mix_units = []
        for t in range(2):
            tok0 = c * CH + t * 128
            xr_ = xr[t]; xo_ = xmo[t]
            for n in range(2):
                def unit_mm(t=t, n=n, xr_=xr_, xo_=xo_, tok0=tok0):
                    if n == 0:
                        S.dma("sp", xr_[:], x_d[tok0:tok0 + 128, :], writes=[xr_])
                    pm = PB[4 + n]
                    for oc in range(8):
                        S.op("pe", lambda e, pm=pm, oc=oc, t=t, n=n: e.matmul(out=pm[:], lhsT=yT[:, oc, t * 128:(t + 1) * 128],
                                                                              rhs=wout[:, oc, n * 512:(n + 1) * 512], start=(oc == 0), stop=(oc == 7)),
                             reads=[yT, wout], writes=[pm])
                    S.op("dve", lambda e, pm=pm, n=n, xo_=xo_: e.tensor_tensor(out=xo_[:, n * 512:(n + 1) * 512], in0=pm[:],
                                                                              in1=g1row[:, n * 512:(n + 1) * 512], op=ALU.mult),
                         reads=[pm, g1row], wshared=[xo_])
                mix_units.append(unit_mm)

            def unit_st(xr_=xr_, xo_=xo_, tok0=tok0):
                S.op("dve", lambda e, xo_=xo_, xr_=xr_: e.tensor_tensor(out=xo_[:], in0=xo_[:], in1=xr_[:], op=ALU.add),
                     reads=[xo_, xr_], writes=[xo_])
                dstd = out_d if stage == "A" else xmid_d
                S.dma("sp", dstd[tok0:tok0 + 128, :], xo_[:], reads=[xo_], wshared=[out_B if stage == "A" else xmid_B])
            mix_units.append(unit_st)
        extra = list(extra_units)
        while mix_units or extra:
            if mix_units:
                mix_units.pop(0)()
            if extra:
                extra.pop(0)()
            if extra and len(extra) > len(mix_units):
                extra.pop(0)()

    stage_H(0); load_rope(0); stage_KVU(0)
    if NCH > 1:
        stage_H(1); load_rope(1); stage_KVU(1)
    for c in range(NCH):
        stage_QG(c)
        poolmix_a(c)
        conv_wd(c)
        attention(c)
        poolmix_b(c)
        extra = ()
        if c + 2 < NCH:
            stage_H(c + 2); load_rope(c + 2)
            extra = kvu_units(c + 2)
        merge(c, extra)

    if stage == "A":
        S.finalize_and_emit()
        return nc
    PHASE3(S, locals())
    S.finalize_and_emit()
    return nc


def PHASE3(S, ns):
    nc = S.nc
    LB = 64
    PB = ns["PB"]; PTb = ns["PTb"]
    ident_f = ns["ident_f"]; ident_b = ns["ident_b"]; ustrict = ns["ustrict"]; ones_b = ns["ones_b"]
    iotae = ns["iotae"]; iotap = ns["iotap"]
    rowsave_d = ns["rowsave_d"]; rowsave_B = ns["rowsave_B"]; fgrow_d = ns["fgrow_d"]
    wr_d = ns["wr_d"]; rbias_d = ns["rbias_d"]; wsg_d = ns["wsg_d"]; wsu_d = ns["wsu_d"]; wsd_d = ns["wsd_d"]
    weg_d = ns["weg_d"]; weu_d = ns["weu_d"]; wed_d = ns["wed_d"]
    xmid_d = ns["xmid_d"]; xmid_B = ns["xmid_B"]; out_d = ns["out_d"]; out_B = ns["out_B"]
    xs_d = ns["xs_d"]; xs_B = ns["xs_B"]
    ys_t = nc.dram_tensor("ys", [NSLOT, D], BF16, kind="Internal"); ys_d = ys_t.ap(); ys_B = S.dram("ys")
    x2_t = nc.dram_tensor("x2", [T, D], F32, kind="Internal"); x2_d = x2_t.ap(); x2_B = S.dram("x2")

    S.barrier("pool", lambda e: e.memset(ones_b[:, 0:1], 1.0))
    S.sb_ptr = ns["P0_KEEP"]
    g2row = S.sb("g2row3", [128, D], F32); fgrow = S.sb("fgrow", [128, D], F32)
    w8all = S.sb("w8all", [128, NT, 8], F32); sloti = S.sb("sloti", [128, NT, 8], I32)
    idxall = S.sb("idxall", [128, 512], I32)
    P3_KEEP = S.sb_ptr
    sh2row = S.sb("sh2row3", [128, D], F32); a2row = S.sb("a2row3", [128, D], F32)
    S.dma("sp", sh2row[:], rowsave_d[0:1, :].partition_broadcast(128), reads=[rowsave_B], writes=[sh2row])
    S.dma("sp", a2row[:], rowsave_d[1:2, :].partition_broadcast(128), reads=[rowsave_B], writes=[a2row])
    S.dma("sp", g2row[:], rowsave_d[2:3, :].partition_broadcast(128), reads=[rowsave_B], writes=[g2row])
    S.dma("sp", fgrow[:], fgrow_d.partition_broadcast(128), writes=[fgrow])
    rbias = S.sb("rbias", [128, 256], F32)
    S.dma("act", rbias[:], rbias_d.partition_broadcast(128), writes=[rbias])
    wr = S.sb("wr", [128, 8, 256], F32)
    S.dma("sp", wr[:].rearrange("p a b -> p (a b)"), wr_d, writes=[wr])
    wsg = S.sb("wsg", [128, 8, 256], BF16); wsu = S.sb("wsu", [128, 8, 256], BF16); wsd = S.sb("wsd", [128, 2, 1024], BF16)
    S.dma("pool", wsg[:].rearrange("p a b -> p (a b)"), wsg_d, writes=[wsg])
    S.dma("pool", wsu[:].rearrange("p a b -> p (a b)"), wsu_d, writes=[wsu])
    S.dma("pool", wsd[:].rearrange("p a b -> p (a b)"), wsd_d, writes=[wsd])
    maskall = S.sb("maskall", [128, NT, 256], BF16)
    h2ball = S.sb("h2ball", [128, NT, D], BF16)
    eidxu = S.sb("eidxu", [128, NT, 8], U32); eidxf = S.sb("eidxf", [128, NT, 8], F32); slotf = S.sb("slotf", [128, NT, 8], F32)
    xmb = [S.sb(f"xm{i}", [128, D], F32) for i in range(2)]
    h2fs = [S.sb(f"h2f{j}", [128, D], F32) for j in range(3)]
    h2T32s = [S.sb(f"h2T32{j}", [128, 8, 128], F32) for j in range(3)]
    iotab = S.sb("iotab", [128, 512], F32); pendsT = S.sb("pendsT", [128, 2], F32)
    Mh = [S.sb(f"Mh{j}", [128, 512], BF16) for j in range(2)]
    S.dma("sp", iotab[:], ns["iotab_d"], writes=[iotab])
    ss = S.sb("ss3", [128, 1], F32); sd = S.sb("sd3", [128, 1], F32); rs = S.sb("rs3", [128, 1], F32)
    sv = S.sb("sv", [128, 256], F32); sel = S.sb("sel", [128, 256], F32); selm = S.sb("selm", [128, 256], F32)
    sw = S.sb("sw", [128, 256], F32); G = S.sb("G", [128, 256], F32); junk256 = S.sb("junk256", [128, 256], F32)
    top8g = S.sb("top8g", [128, 8, 8], F32); gs = S.sb("gs", [128, 8], F32); gsort = S.sb("gsort", [128, 8], F32)
    gmask = S.sb("gmask", [128, 8], F32); negb = S.sb("negb", [128, 8], F32); top8 = S.sb("top8", [128, 8], F32)
    ssum = S.sb("ssum", [128, 1], F32); rs2 = S.sb("rs2", [128, 1], F32)
    sgs = S.sb("sgs", [128, 256], F32); actsh = S.sb("actsh", [128, 256], BF16)
    x2t = [S.sb(f"x2t{i}", [128, D], F32) for i in range(2)]
    cnt = S.sb("cnt", [128, 256], F32); tq = S.sb("tq", [128, 256], F32); nbi = S.sb("nbi", [128, 256], I32)
    padded = S.sb("padded", [128, 256], F32); fix = S.sb("fix", [128, 256], F32)
    pends = S.sb("pends", [128, 256], F32); pstart = S.sb("pstart", [128, 256], F32); ones256 = S.sb("ones256", [128, 256], F32)
    eb = S.sb("eb", [128, 512], F32); idxf = S.sb("idxf", [128, 512], F32)
    ebs = S.sb("ebs", [128, 512], F32); same = S.sb("same", [128, 512], F32)
    macc = S.sb("macc", [128, 256], BF16); slotm = S.sb("slotm", [128, 256], F32)
    S.op("pool", lambda e: e.memset(macc[:], 0.0), writes=[macc])
    S.op("pool", lambda e: e.memset(ones256[:], 1.0), writes=[ones256])
    S.op("pool", lambda e: e.memset(eb[:], 256.0), writes=[eb])

    neghalf = ns["neghalf"]

    def load_xm(i):
        S.dma("sp", xmb[i % 2][:], xmid_d[i * 128:(i + 1) * 128, :], reads=[xmid_B], writes=[xmb[i % 2]])

    def stage_A(i):
        xm = xmb[i % 2]; h2f = h2fs[i % 3]; h2T32 = h2T32s[i % 3]
        if i + 1 < NT:
            load_xm(i + 1)
        S.op("act", lambda e: e.activation(out=h2f[:], in_=xm[:], func=AF.Square, accum_out=ss[:]), reads=[xm], writes=[h2f, ss])
        S.op("pool", lambda e: e.tensor_scalar(out=sd[:], in0=ss[:], scalar1=1.0 / D, scalar2=EPS, op0=ALU.mult, op1=ALU.add), reads=[ss], writes=[sd])
        S.op("pool", lambda e: e.tensor_tensor(out=rs[:], in0=sd[:], in1=neghalf[:], op=ALU.pow), reads=[sd, neghalf], writes=[rs])
        S.op("dve", lambda e: e.scalar_tensor_tensor(out=h2f[:], in0=xm[:], scalar=rs[:, 0:1], in1=a2row[:], op0=ALU.mult, op1=ALU.mult),
             reads=[xm, rs, a2row], writes=[h2f])
        S.op("dve", lambda e: e.tensor_tensor(out=h2f[:], in0=h2f[:], in1=sh2row[:], op=ALU.add), reads=[h2f, sh2row], writes=[h2f])
        S.op("act", lambda e: e.activation(out=h2ball[:, i, :], in_=h2f[:], func=AF.Copy), reads=[h2f], wshared=[h2ball])
        for k in range(8):
            pb = PB[0] if k < 4 else PB[1]
            S.op("pe", lambda e, k=k, pb=pb: e.transpose(out=pb[:, (k % 4) * 128:(k % 4 + 1) * 128], in_=h2f[:, k * 128:(k + 1) * 128], identity=ident_f[:]),
                 reads=[h2f, ident_f], writes=[pb])
        S.op("act", lambda e: e.activation(out=h2T32[:, 0:4, :].rearrange("p a b -> p (a b)"), in_=PB[0][:], func=AF.Copy), reads=[PB[0]], wshared=[h2T32])
        S.op("act", lambda e: e.activation(out=h2T32[:, 4:8, :].rearrange("p a b -> p (a b)"), in_=PB[1][:], func=AF.Copy), reads=[PB[1]], wshared=[h2T32])

    def stage_B1(i):
        h2T32 = h2T32s[i % 3]
        for k in range(8):
            S.op("pe", lambda e, k=k: e.matmul(out=PB[2][:, 0:256], lhsT=h2T32[:, k, :], rhs=wr[:, k, :], start=(k == 0), stop=(k == 7)),
                 reads=[h2T32, wr], writes=[PB[2]])
        S.op("act", lambda e: e.activation(out=sv[:], in_=PB[2][:, 0:256], func=AF.Sigmoid), reads=[PB[2]], writes=[sv])

    def stage_B(i):
        S.op("dve", lambda e: e.tensor_tensor(out=sel[:], in0=sv[:], in1=rbias[:], op=ALU.add), reads=[sv, rbias], writes=[sel])
        for g in range(8):
            S.op("dve", lambda e, g=g: e.max(out=top8g[:, g, :], in_=sel[:, g * 32:(g + 1) * 32]), reads=[sel], wshared=[top8g])
        S.op("dve", lambda e: e.tensor_tensor(out=gs[:], in0=top8g[:, :, 0], in1=top8g[:, :, 1], op=ALU.add), reads=[top8g], writes=[gs])
        S.op("dve", lambda e: e.max(out=gsort[:], in_=gs[:]), reads=[gs], writes=[gsort])
        S.op("dve", lambda e: e.tensor_scalar(out=gmask[:], in0=gs[:], scalar1=gsort[:, 3:4], scalar2=None, op0=ALU.is_ge), reads=[gs, gsort], writes=[gmask])
        S.op("dve", lambda e: e.tensor_scalar(out=negb[:], in0=gmask[:], scalar1=-1.0, scalar2=1e30, op0=ALU.add, op1=ALU.mult), reads=[gmask], writes=[negb])
        for g in range(8):
            S.op("dve", lambda e, g=g: e.tensor_scalar(out=selm[:, g * 32:(g + 1) * 32], in0=sel[:, g * 32:(g + 1) * 32],
                                                       scalar1=gmask[:, g:g + 1], scalar2=negb[:, g:g + 1], op0=ALU.mult, op1=ALU.add),
                 reads=[sel, gmask, negb], wshared=[selm])
        S.op("dve", lambda e: e.max(out=top8[:], in_=selm[:]), reads=[selm], writes=[top8])
        S.op("dve", lambda e: e.tensor_scalar(out=maskall[:, i, :], in0=selm[:], scalar1=top8[:, 7:8], scalar2=None, op0=ALU.is_ge),
             reads=[selm, top8], wshared=[maskall])
        S.op("dve", lambda e: e.scalar_tensor_tensor(out=sw[:], in0=sv[:], scalar=1.0, in1=maskall[:, i, :], op0=ALU.mult, op1=ALU.mult, accum_out=ssum[:]),
             reads=[sv, maskall], writes=[sw, ssum])
        S.op("dve", lambda e: e.reciprocal(out=rs2[:], in_=ssum[:]), reads=[ssum], writes=[rs2])
        S.op("dve", lambda e: e.tensor_scalar(out=G[:], in0=sw[:], scalar1=rs2[:, 0:1], scalar2=2.5, op0=ALU.mult, op1=ALU.mult), reads=[sw, rs2], writes=[G])
        S.op("dve", lambda e: e.max(out=w8all[:, i, :], in_=G[:]), reads=[G], wshared=[w8all])
        S.op("dve", lambda e: e.max_index(out=eidxu[:, i, :], in_max=w8all[:, i, :], in_values=G[:]), reads=[G, w8all], wshared=[eidxu])
        S.op("pe", lambda e: e.matmul(out=PB[3][:, 0:256], lhsT=ones_b[:], rhs=maskall[:, i, :], start=(i == 0), stop=(i == NT - 1)),
             reads=[ones_b, maskall], writes=[PB[3]])

    load_xm(0)
    stage_A(0)
    stage_A(1)
    for i in range(NT):
        stage_B1(i)
        if i + 2 < NT:
            stage_A(i + 2)
        stage_B(i)

    S.op("dve", lambda e: e.tensor_copy(out=cnt[:], in_=PB[3][:, 0:256]), reads=[PB[3]], writes=[cnt])
    S.op("dve", lambda e: e.tensor_scalar(out=tq[:], in0=cnt[:], scalar1=127.0, scalar2=1.0 / 128, op0=ALU.add, op1=ALU.mult), reads=[cnt], writes=[tq])
    S.op("dve", lambda e: e.tensor_scalar(out=nbi[:], in0=tq[:], scalar1=-0.49609375, scalar2=None, op0=ALU.add), reads=[tq], writes=[nbi])
    S.op("dve", lambda e: e.tensor_copy(out=padded[:], in_=nbi[:]), reads=[nbi], writes=[padded])
    S.op("dve", lambda e: e.tensor_scalar(out=padded[:], in0=padded[:], scalar1=128.0, scalar2=None, op0=ALU.mult), reads=[padded], writes=[padded])
    S.op("dve", lambda e: e.tensor_tensor(out=fix[:], in0=padded[:], in1=cnt[:], op=ALU.is_lt), reads=[padded, cnt], writes=[fix])
    S.op("dve", lambda e: e.scalar_tensor_tensor(out=padded[:], in0=fix[:], scalar=128.0, in1=padded[:], op0=ALU.mult, op1=ALU.add), reads=[fix, padded], writes=[padded])
    S.op("dve", lambda e: e.tensor_scalar(out=tq[:], in0=padded[:], scalar1=-128.0, scalar2=None, op0=ALU.add), reads=[padded], writes=[tq])
    S.op("dve", lambda e: e.tensor_tensor(out=fix[:], in0=tq[:], in1=cnt[:], op=ALU.is_ge), reads=[tq, cnt], writes=[fix])
    S.op("dve", lambda e: e.scalar_tensor_tensor(out=padded[:], in0=fix[:], scalar=-128.0, in1=padded[:], op0=ALU.mult, op1=ALU.add), reads=[fix, padded], writes=[padded])
    S.op("dve", lambda e: e.tensor_tensor_scan(out=pends[:], data0=ones256[:], data1=padded[:], initial=0.0, op0=ALU.mult, op1=ALU.add),
         reads=[ones256, padded], writes=[pends])
    S.op("dve", lambda e: e.tensor_tensor(out=pstart[:], in0=pends[:], in1=padded[:], op=ALU.subtract), reads=[pends, padded], writes=[pstart])
    for h in range(2):
        S.op("pe", lambda e, h=h: e.transpose(out=PB[4][:, h * 128:(h + 1) * 128], in_=pends[:, h * 128:(h + 1) * 128], identity=ident_f[:]),
             reads=[pends, ident_f], writes=[PB[4]])
    S.op("dve", lambda e: e.tensor_copy(out=pendsT[:], in_=PB[4][:, 0:256].rearrange("p (h c) -> p h c", h=2)[:, :, 0]), reads=[PB[4]], writes=[pendsT])
    for h in range(2):
        S.op("dve", lambda e, h=h: e.tensor_scalar(out=Mh[h][:], in0=iotab[:], scalar1=pendsT[:, h:h + 1], scalar2=None, op0=ALU.is_ge),
             reads=[iotab, pendsT], writes=[Mh[h]])
        S.op("pe", lambda e, h=h: e.matmul(out=PB[5][:], lhsT=ones_b[:], rhs=Mh[h][:], start=(h == 0), stop=(h == 1)),
             reads=[ones_b, Mh[h]], writes=[PB[5]])
    S.op("dve", lambda e: e.tensor_copy(out=eb[:], in_=PB[5][:]), reads=[PB[5]], writes=[eb])
    S.op("dve", lambda e: e.memset(eb[:, 511:512], 256.0), reads=[eb], writes=[eb])
    S.op("pool", lambda e: e.memset(ebs[:, 0:1], -1.0), wshared=[ebs])
    S.op("dve", lambda e: e.tensor_copy(out=ebs[:, 1:512], in_=eb[:, 0:511]), reads=[eb], wshared=[ebs])
    S.op("dve", lambda e: e.tensor_tensor(out=same[:], in0=eb[:], in1=ebs[:], op=ALU.is_equal), reads=[eb, ebs], writes=[same])
    S.op("dve", lambda e: e.memset(same[:].rearrange("p (l m) -> p l m", m=LB)[:, :, 0:1], 0.0), reads=[same], writes=[same])
    S.op("dve", lambda e: e.tensor_scalar(out=idxf[:], in0=eb[:], scalar1=128.0, scalar2=iotap[:, 0:1], op0=ALU.mult, op1=ALU.add), reads=[eb, iotap], writes=[idxf])
    S.op("dve", lambda e: e.scalar_tensor_tensor(out=idxf[:], in0=same[:], scalar=1.0e6, in1=idxf[:], op0=ALU.mult, op1=ALU.add), reads=[same, idxf], writes=[idxf])
    S.op("dve", lambda e: e.tensor_copy(out=idxall[:], in_=idxf[:]), reads=[idxf], writes=[idxall])

    h2Tb2 = S.sb("h2Tb2", [128, D], BF16)
    tsh = S.sb("tsh", [128, 256], F32)
    load_xm(0)
    for i in range(NT):
        xm = xmb[i % 2]
        if i + 1 < NT:
            load_xm(i + 1)
        S.op("pe", lambda e, i=i: e.matmul(out=PB[0][:, 0:256], lhsT=ustrict[:], rhs=maskall[:, i, :], start=True, stop=False), reads=[ustrict, maskall], writes=[PB[0]])
        S.op("pe", lambda e: e.matmul(out=PB[0][:, 0:256], lhsT=ones_b[:], rhs=macc[:], start=False, stop=True), reads=[ones_b, macc], writes=[PB[0]])
        S.op("dve", lambda e: e.tensor_tensor(out=slotm[:], in0=PB[0][:, 0:256], in1=pstart[:], op=ALU.add), reads=[PB[0], pstart], writes=[slotm])
        S.op("dve", lambda e, i=i: e.tensor_tensor(out=macc[:], in0=macc[:], in1=maskall[:, i, :], op=ALU.add), reads=[macc, maskall], writes=[macc])
        S.op("dve", lambda e, i=i: e.tensor_copy(out=eidxf[:, i, :], in_=eidxu[:, i, :]), reads=[eidxu], wshared=[eidxf])
        for k in range(8):
            S.op("dve", lambda e, i=i, k=k: e.scalar_tensor_tensor(out=junk256[:], in0=iotae[:], scalar=eidxf[:, i, k:k + 1], in1=slotm[:],
                                                                   op0=ALU.is_equal, op1=ALU.mult, accum_out=slotf[:, i, k:k + 1]),
                 reads=[iotae, eidxf, slotm], writes=[junk256], wshared=[slotf])
        S.op("dve", lambda e, i=i: e.tensor_copy(out=sloti[:, i, :], in_=slotf[:, i, :]), reads=[slotf], wshared=[sloti])
        for k in range(8):
            S.dma_fn("pool", lambda e, i=i, k=k: e.indirect_dma_start(
                out=xs_d, out_offset=bass.IndirectOffsetOnAxis(ap=sloti[:, i, k:k + 1], axis=0),
                in_=h2ball[:, i, :], in_offset=None, bounds_check=S.reg(e, NSLOT - 1), oob_is_err=False),
                reads=[h2ball, sloti], wshared=[xs_B])
        for k in range(8):
            S.op("pe", lambda e, i=i, k=k: e.transpose(out=PTb[:, k * 128:(k + 1) * 128], in_=h2ball[:, i, k * 128:(k + 1) * 128], identity=ident_b[:]),
                 reads=[h2ball, ident_b], writes=[PTb])
        S.op("act", lambda e: e.activation(out=h2Tb2[:], in_=PTb[:], func=AF.Copy), reads=[PTb], writes=[h2Tb2])
        for fc in range(2):
            for (wsrc, off) in ((wsg, 0), (wsu, 256)):
                for k in range(8):
                    S.op("pe", lambda e, fc=fc, wsrc=wsrc, off=off, k=k: e.matmul(out=PB[4][:, off + fc * 128:off + (fc + 1) * 128],
                                                                                   lhsT=wsrc[:, k, fc * 128:(fc + 1) * 128], rhs=h2Tb2[:, k * 128:(k + 1) * 128],
                                                                                   start=(k == 0), stop=(k == 7)),
                         reads=[wsrc, h2Tb2], writes=[PB[4]])
        S.op("act", lambda e: e.activation(out=sgs[:], in_=PB[4][:, 0:256], func=AF.Sigmoid), reads=[PB[4]], writes=[sgs])
        S.op("dve", lambda e: e.tensor_tensor(out=tsh[:], in0=sgs[:], in1=PB[4][:, 0:256], op=ALU.mult), reads=[sgs, PB[4]], writes=[tsh])
        S.op("dve", lambda e: e.tensor_tensor(out=actsh[:], in0=tsh[:], in1=PB[4][:, 256:512], op=ALU.mult), reads=[tsh, PB[4]], writes=[actsh])
        xo = x2t[i % 2]
        for n in range(2):
            for fc in range(2):
                S.op("pe", lambda e, n=n, fc=fc: e.matmul(out=PB[5 + n][:], lhsT=actsh[:, fc * 128:(fc + 1) * 128], rhs=wsd[:, fc, n * 512:(n + 1) * 512],
                                                          start=(fc == 0), stop=(fc == 1)), reads=[actsh, wsd], writes=[PB[5 + n]])
            S.op("dve", lambda e, n=n, xo=xo: e.tensor_tensor(out=xo[:, n * 512:(n + 1) * 512], in0=PB[5 + n][:], in1=g2row[:, n * 512:(n + 1) * 512], op=ALU.mult),
                 reads=[PB[5 + n], g2row], wshared=[xo])
        S.op("dve", lambda e, xo=xo, xm=xm: e.tensor_tensor(out=xo[:], in0=xo[:], in1=xm[:], op=ALU.add), reads=[xo, xm], writes=[xo])
        S.dma("sp", x2_d[i * 128:(i + 1) * 128, :], xo[:], reads=[xo], wshared=[x2_B])

    S.barrier("pool", lambda e: e.memset(ones_b[:, 0:1], 1.0))
    S.sb_ptr = P3_KEEP
    NL = 8
    wg = [S.sb(f"wg{j}", [128, 2048], BF16) for j in range(NL)]
    wu = [S.sb(f"wu{j}", [128, 2048], BF16) for j in range(NL)]
    wd = [S.sb(f"wd{j}", [128, 2048], BF16) for j in range(NL)]
    xsb = [S.sb(f"xsb{j}", [128, D], BF16) for j in range(3)]
    xsT = [S.sb(f"xsT{j}", [128, D], BF16) for j in range(2)]
    sgt = S.sb("sgt", [128, 256], F32)
    actT = [S.sb(f"actT{j}", [128, 256], BF16) for j in range(2)]
    ysb = [S.sb(f"ysb{j}", [128, D], BF16) for j in range(2)]
    order = [l * LB + m for m in range(LB) for l in range(NL) if l * LB + m < NBLK]

    def load_xs(n):
        b = order[n]
        S.dma("sp", xsb[n % 3][:], xs_d[b * 128:(b + 1) * 128, :], reads=[xs_B], writes=[xsb[n % 3]])

    def load_w(b):
        l = b // LB
        for (wt, src, extra) in ((wg[l], weg_d, []), (wu[l], weu_d, []), (wd[l], ns["wedb_d"], [ns["wedb_B"]])):
            S.dma_fn("pool", lambda e, wt=wt, src=src, b=b: e.indirect_dma_start(
                out=wt[:], out_offset=None, in_=src, in_offset=bass.IndirectOffsetOnAxis(ap=idxall[:, b:b + 1], axis=0),
                bounds_check=S.reg(e, E * 128 - 1), oob_is_err=False), reads=[idxall] + extra, writes=[wt])

    for l in range(NL):
        load_w(l * LB)
    load_xs(0); load_xs(1)
    for n, b in enumerate(order):
        l = b // LB
        if n + 2 < len(order):
            load_xs(n + 2)
        xb = xsb[n % 3]; xT = xsT[n % 2]; aT = actT[n % 2]; yb = ysb[n % 2]
        for k in range(8):
            S.op("pe", lambda e, k=k, xb=xb: e.transpose(out=PTb[:, k * 128:(k + 1) * 128], in_=xb[:, k * 128:(k + 1) * 128], identity=ident_b[:]),
                 reads=[xb, ident_b], writes=[PTb])
        if n % 2 == 0:
            S.op("act", lambda e, xT=xT: e.activation(out=xT[:], in_=PTb[:], func=AF.Copy), reads=[PTb], writes=[xT])
        else:
            S.op("dve", lambda e, xT=xT: e.tensor_copy(out=xT[:], in_=PTb[:]), reads=[PTb], writes=[xT])
        pg = PB[n % 2]
        for fc in range(2):
            for (wt, off) in ((wg[l], 0), (wu[l], 256)):
                for k in range(8):
                    S.op("pe", lambda e, fc=fc, wt=wt, off=off, k=k, pg=pg, xT=xT: e.matmul(
                        out=pg[:, off + fc * 128:off + (fc + 1) * 128], lhsT=wt[:, k * 256 + fc * 128:k * 256 + (fc + 1) * 128],
                        rhs=xT[:, k * 128:(k + 1) * 128], start=(k == 0), stop=(k == 7)), reads=[wt, xT], writes=[pg])
        S.op("act", lambda e, pg=pg: e.activation(out=sgt[:], in_=pg[:, 0:256], func=AF.Silu), reads=[pg], writes=[sgt])
        S.op("dve", lambda e, pg=pg, aT=aT: e.tensor_tensor(out=aT[:], in0=sgt[:], in1=pg[:, 256:512], op=ALU.mult), reads=[sgt, pg], writes=[aT])
        for nn in range(2):
            py = PB[2 + 2 * (n % 2) + nn]
            for fc in range(2):
                S.op("pe", lambda e, nn=nn, fc=fc, py=py, aT=aT, l=l: e.matmul(out=py[:], lhsT=aT[:, fc * 128:(fc + 1) * 128],
                                                                              rhs=wd[l][:, fc * 1024 + nn * 512:fc * 1024 + (nn + 1) * 512],
                                                                              start=(fc == 0), stop=(fc == 1)), reads=[aT, wd[l]], writes=[py])
            S.op("dve", lambda e, py=py, yb=yb, nn=nn: e.tensor_tensor(out=yb[:, nn * 512:(nn + 1) * 512], in0=py[:], in1=g2row[:, nn * 512:(nn + 1) * 512], op=ALU.mult),
                 reads=[py, g2row], wshared=[yb])
        S.dma("sp", ys_d[b * 128:(b + 1) * 128, :], yb[:], reads=[yb], wshared=[ys_B])
        if b + 1 < NBLK and (b + 1) % LB != 0:
            load_w(b + 1)

    S.barrier("pool", lambda e: e.memset(ones_b[:, 0:1], 1.0))
    S.sb_ptr = P3_KEEP
    gk = [S.sb(f"gk{j}", [128, 8, D], BF16) for j in range(2)]
    x2l = [S.sb(f"x2l{j}", [128, D], F32) for j in range(2)]
    acc = [S.sb(f"acc{j}", [128, D], F32) for j in range(2)]
    ot = [S.sb(f"ot{j}", [128, D], F32) for j in range(2)]
    ss4 = S.sb("ss4", [128, 1], F32); sd4 = S.sb("sd4", [128, 1], F32); rs4 = S.sb("rs4", [128, 1], F32)
    whb = S.sb("whb", [128, 8], BF16); whf = S.sb("whf", [128, 8], F32); wlf = S.sb("wlf", [128, 8], F32)
    dg = [S.sb(f"dg{j}", [128, 16, 128], BF16) for j in range(2)]
    def load_c(i):
        gkt = gk[i % 2]; xl = x2l[i % 2]
        for k in range(8):
            S.dma_fn("pool", lambda e, i=i, k=k, gkt=gkt: e.indirect_dma_start(
                out=gkt[:, k, :], out_offset=None, in_=ys_d, in_offset=bass.IndirectOffsetOnAxis(ap=sloti[:, i, k:k + 1], axis=0),
                bounds_check=S.reg(e, NSLOT - 1), oob_is_err=False), reads=[ys_B, sloti], wshared=[gkt])
        S.dma("sp", xl[:], x2_d[i * 128:(i + 1) * 128, :], reads=[x2_B], writes=[xl])
    load_c(0)
    for i in range(NT):
        gkt = gk[i % 2]; xl = x2l[i % 2]; ac = acc[i % 2]; o_ = ot[i % 2]
        if i + 1 < NT:
            load_c(i + 1)
        dgt = dg[i % 2]
        S.op("dve", lambda e, i=i: e.tensor_copy(out=whb[:], in_=w8all[:, i, :]), reads=[w8all], writes=[whb])
        S.op("dve", lambda e: e.tensor_copy(out=whf[:], in_=whb[:]), reads=[whb], writes=[whf])
        S.op("dve", lambda e, i=i: e.tensor_tensor(out=wlf[:], in0=w8all[:, i, :], in1=whf[:], op=ALU.subtract), reads=[w8all, whf], writes=[wlf])
        for k in range(8):
            S.op("dve", lambda e, k=k, dgt=dgt: e.tensor_scalar(out=dgt[:, 2 * k, :], in0=ident_b[:], scalar1=whf[:, k:k + 1], scalar2=None, op0=ALU.mult),
                 reads=[ident_b, whf], wshared=[dgt])
            S.op("dve", lambda e, k=k, dgt=dgt: e.tensor_scalar(out=dgt[:, 2 * k + 1, :], in0=ident_b[:], scalar1=wlf[:, k:k + 1], scalar2=None, op0=ALU.mult),
                 reads=[ident_b, wlf], wshared=[dgt])
        for n in range(2):
            pc = PB[(2 * i + n) % 4]
            for j in range(16):
                S.op("pe", lambda e, pc=pc, j=j, n=n, dgt=dgt, gkt=gkt: e.matmul(out=pc[:], lhsT=dgt[:, j, :], rhs=gkt[:, j // 2, n * 512:(n + 1) * 512],
                                                                              start=(j == 0), stop=(j == 15)), reads=[dgt, gkt], writes=[pc])
            S.op("dve", lambda e, pc=pc, n=n, ac=ac, xl=xl: e.tensor_tensor(out=ac[:, n * 512:(n + 1) * 512], in0=pc[:], in1=xl[:, n * 512:(n + 1) * 512], op=ALU.add),
                 reads=[pc, xl], wshared=[ac])
        S.op("act", lambda e, ac=ac, o_=o_: e.activation(out=o_[:], in_=ac[:], func=AF.Square, accum_out=ss4[:]), reads=[ac], writes=[o_, ss4])
        S.op("pool", lambda e: e.tensor_scalar(out=sd4[:], in0=ss4[:], scalar1=1.0 / D, scalar2=EPS, op0=ALU.mult, op1=ALU.add), reads=[ss4], writes=[sd4])
        S.op("pool", lambda e: e.tensor_tensor(out=rs4[:], in0=sd4[:], in1=neghalf[:], op=ALU.pow), reads=[sd4, neghalf], writes=[rs4])
        S.op("dve", lambda e, ac=ac, o_=o_: e.scalar_tensor_tensor(out=o_[:], in0=ac[:], scalar=rs4[:, 0:1], in1=fgrow[:], op0=ALU.mult, op1=ALU.mult),
             reads=[ac, rs4, fgrow], writes=[o_])
        S.dma("sp", out_d[i * 128:(i + 1) * 128, :], o_[:], reads=[o_], wshared=[out_B])

def _fm(v, nk):
    return np.ascontiguousarray(v.reshape(nk, 128).T)


def _kp(w):
    K = w.shape[0] // 128
    return np.ascontiguousarray(w.reshape(K, 128, w.shape[1]).transpose(1, 0, 2).reshape(128, K * w.shape[1]))


def _consts():
    c = {}
    c["ident"] = np.eye(128, dtype=np.float32)
    j = np.arange(128)[:, None]; i = np.arange(128)[None, :]
    NEG = np.float32(-240000.0)
    mprev = np.where(j >= i, np.float32(0.0), NEG).astype(np.float32); mnext = np.where(j <= i, np.float32(0.0), NEG).astype(np.float32)
    c["masks"] = np.concatenate([np.tile(mprev, (1, 4)), np.tile(mnext, (1, 4))], axis=1)
    c["ustrict"] = (j < i).astype(np.float32)
    L = T
    invc = np.zeros((4, 2, 8), np.float32)
    for g, w in enumerate((2, 4, 8, 16)):
        for side in range(2):
            for jj in range(8):
                t = jj if side == 0 else L - 8 + jj
                lo = min(max(t - w // 2, 0), L); hi = min(max(t + w // 2, 0), L)
                invc[g, side, jj] = 1.0 / float(hi - lo)
    c["invc"] = np.tile(invc.reshape(1, 64), (128, 1))
    c["iotae"] = np.tile(np.arange(256, dtype=np.float32)[None, :], (128, 1))
    c["iotap"] = np.arange(128, dtype=np.float32)[:, None].copy()
    c["iotab"] = np.tile((128.0 * np.arange(512, dtype=np.float32))[None, :], (128, 1))
    rows = T // 64
    row = np.repeat(np.arange(rows), 64).astype(np.float32)
    col = np.tile(np.arange(64), rows).astype(np.float32)
    nf = 16
    inv = (np.float32(10000.0) ** (-np.arange(nf, dtype=np.float32) / np.float32(nf))).astype(np.float32)
    ang = np.stack([row[:, None] * inv, col[:, None] * inv], axis=1)
    cs_, sn_ = np.cos(ang).astype(np.float32), np.sin(ang).astype(np.float32)
    C = np.zeros((64, T), np.float32); Sg = np.zeros((64, T), np.float32)
    for d in range(64):
        axis, ab, f = d // 32, (d % 32) // 16, d % 16
        C[d] = cs_[:, axis, f]
        Sg[d] = (-sn_[:, axis, f]) if ab == 0 else sn_[:, axis, f]
    c["ropec"] = np.concatenate([C, C], axis=0); c["ropes"] = np.concatenate([Sg, Sg], axis=0)
    return c


def _partner(cols):
    d = cols % 64
    ab = (d % 32) // 16
    return np.where(ab == 0, cols + 16, cols - 16)


def _prep_shared(inp, full=True):
    sh = dict(_consts())
    w_in = inp["w_in"][0]
    qcols = np.concatenate([np.concatenate([np.arange(c * 64, (c + 1) * 64), np.arange((4 + c) * 64, (5 + c) * 64)]) for c in range(4)])
    kcols = np.arange(512, 640)
    cols = np.concatenate([qcols, (_partner(qcols)), kcols, 512 + _partner(kcols - 512),
                           np.arange(768, 1280), np.arange(1280, 3328), np.arange(640, 768)])
    assert cols.shape[0] == NXC
    sh["wx"] = np.ascontiguousarray(w_in[:, cols])
    sh["wada"] = inp["w_ada"][0]; sh["bada"] = inp["b_ada"][0][None, :].copy(); sh["badafm"] = _fm(inp["b_ada"][0], 48)
    sh["n1fm"] = _fm(inp["norm1_g"][0], 8); sh["n2row"] = inp["norm2_g"][0][None, :].copy(); sh["fgrow"] = inp["final_g"][None, :].copy()
    sh["sink"] = inp["attn_sink"][0][None, :].copy()
    sh["wpool"] = np.ascontiguousarray(inp["w_pool"][0].transpose(1, 0, 2).reshape(128, 512))
    sh["pscale"] = _fm(inp["pool_scale"][0], 4)
    wua = inp["w_up_attn"][0]
    rows_ = np.concatenate([np.concatenate([np.arange(c * 64, (c + 1) * 64), np.arange((4 + c) * 64, (5 + c) * 64)]) for c in range(4)])
    sh["wua"] = _kp(wua[rows_]); sh["wup"] = _kp(inp["w_up_pool"][0]); sh["wout"] = _kp(inp["w_out"][0])
    if full:
        sh["wr"] = _kp(inp["w_router"][0]); sh["rbias"] = inp["router_bias"][0][None, :].copy()
        sh["wsg"] = _kp(inp["w_sh_gate"][0]); sh["wsu"] = _kp(inp["w_sh_up"][0]); sh["wsd"] = _kp(inp["w_sh_down"][0])
        def ek(w):
            Ee, R, N = w.shape
            K = R // 128
            return np.ascontiguousarray(w.reshape(Ee, K, 128, N).transpose(0, 2, 1, 3)).reshape(Ee * 128, K * N)
        sh["weg"] = ek(inp["w_exp_gate"][0]); sh["weu"] = ek(inp["w_exp_up"][0]); sh["wed"] = ek(inp["w_exp_down"][0])
    return sh


def _prep_core(inp, b):
    m = {}
    m["x"] = np.ascontiguousarray(inp["x"][b]); m["ctx"] = np.ascontiguousarray(inp["ctx"][b])
    cs = np.stack([_fm(inp["c"][b], 8), _fm(inp["c_ctx"], 8)], axis=2)
    m["csin"] = np.ascontiguousarray(cs.reshape(128, 16))
    return m


def kernel(**inputs):
    inp = {k: np.asarray(v, dtype=np.float32) for k, v in inputs.items()}
    nc = bass.Bass("TRN2", target_bir_lowering=False)
    build(nc, "full")
    sh = _prep_shared(inp, True)
    in_maps = []
    for b in range(8):
        m = dict(sh); m.update(_prep_core(inp, b)); in_maps.append(m)
    res = run_bass_kernel_spmd(nc, in_maps, core_ids=list(range(8)))
    return np.stack([res.results[b]["out"] for b in range(8)], axis=0).astype(np.float32)
```
